# Optimizing a Trainium2 kernel written in Bass

```python
import math
import jax, jax.numpy as jnp
from jax import lax
import numpy as np

D_MODEL = 2048
BATCH = 4
SEQ = 4096
DEPTH = 1

NORM_EPS = 1e-6
ROPE_THETA = 500000.0
MAX_POS_OFFSET = 1024
ADALN_CHUNKS = 6

MLA_HEADS = 8
Q_LORA_RANK = 512
KV_LORA_RANK = 512
QK_NOPE_DIM = 128
QK_ROPE_DIM = 64
V_HEAD_DIM = 128
MLA_QK_DIM = QK_NOPE_DIM + QK_ROPE_DIM
Q_BLOCK = 128

DIL_PATTERNS = ((128, 1), (512, 4), (2048, 16))
DIL_GROUPS = len(DIL_PATTERNS)
DIL_HEADS_PER_GROUP = 4
DIL_HEADS = DIL_GROUPS * DIL_HEADS_PER_GROUP
DIL_HEAD_DIM = 128
DIL_ROT_DIM = DIL_HEAD_DIM // 4
DIL_BLOCK = max(w // d for (w, d) in DIL_PATTERNS)

IN_SIZES = (Q_LORA_RANK, KV_LORA_RANK + QK_ROPE_DIM, 3 * DIL_HEADS * DIL_HEAD_DIM, D_MODEL, D_MODEL)
IN_COLS = sum(IN_SIZES)
IN_SPLITS = [int(v) for v in np.cumsum(IN_SIZES)[:-1]]

N_EXPERTS = 64
N_EXPERT_GROUPS = 8
TOPK_GROUPS = 4
TOP_K = 6
EXPERT_DIM = 512
SHARED_DIM = 512
ROUTED_SCALE = 2.5
MOE_BLOCK = 256

kernel_name = "hybrid_mla_dilated_moe_block"


def rms_norm(x, g):
    xf = x.astype(jnp.float32)
    xf = xf * lax.rsqrt(jnp.mean(xf * xf, axis=-1, keepdims=True) + NORM_EPS)
    return (xf * g.astype(jnp.float32)).astype(x.dtype)


def rope_cos_sin(positions, dim):
    inv_freq = 1.0 / (ROPE_THETA ** (jnp.arange(0, dim, 2, dtype=jnp.float32) / dim))
    ang = positions.astype(jnp.float32)[..., None] * inv_freq
    return jnp.cos(ang)[:, :, None, :], jnp.sin(ang)[:, :, None, :]


def apply_rope(t, cos, sin):
    half = t.shape[-1] // 2
    t1 = t[..., :half].astype(jnp.float32)
    t2 = t[..., half:].astype(jnp.float32)
    return jnp.concatenate([t1 * cos - t2 * sin, t2 * cos + t1 * sin], axis=-1).astype(t.dtype)


def causal_block_attention(q, k, v, scale):
    B, S, H, Dq = q.shape
    Dv = v.shape[-1]
    nb = S // Q_BLOCK
    qb = q.reshape(B, nb, Q_BLOCK, H, Dq).swapaxes(0, 1)
    kpos = jnp.arange(S)

    def one_block(args):
        qi, i = args
        s = jnp.einsum('bqhd,bkhd->bhqk', qi, k, preferred_element_type=jnp.float32) * scale
        qpos = i * Q_BLOCK + jnp.arange(Q_BLOCK)
        s = jnp.where((kpos[None, :] <= qpos[:, None])[None, None], s, -jnp.inf)
        p = jax.nn.softmax(s, axis=-1).astype(v.dtype)
        return jnp.einsum('bhqk,bkhd->bqhd', p, v)

    o = lax.map(one_block, (qb, jnp.arange(nb)))
    return o.swapaxes(0, 1).reshape(B, S, H, Dv)


def mla_branch(q_a, kv_a, cos, sin, q_a_norm_g, w_q_up, kv_a_norm_g, w_kv_up, w_mla_o):
    B, S, _ = q_a.shape
    q = (rms_norm(q_a, q_a_norm_g) @ w_q_up).reshape(B, S, MLA_HEADS, MLA_QK_DIM)
    q_nope, q_rope = q[..., :QK_NOPE_DIM], q[..., QK_NOPE_DIM:]
    q = jnp.concatenate([q_nope, apply_rope(q_rope, cos, sin)], axis=-1)
    c_kv, k_rope = kv_a[..., :KV_LORA_RANK], kv_a[..., KV_LORA_RANK:]
    kv = (rms_norm(c_kv, kv_a_norm_g) @ w_kv_up).reshape(B, S, MLA_HEADS, QK_NOPE_DIM + V_HEAD_DIM)
    k_nope, v = kv[..., :QK_NOPE_DIM], kv[..., QK_NOPE_DIM:]
    k_rope = apply_rope(k_rope[:, :, None, :], cos, sin)
    k = jnp.concatenate([k_nope, jnp.broadcast_to(k_rope, (B, S, MLA_HEADS, QK_ROPE_DIM))], axis=-1)
    o = causal_block_attention(q, k, v, MLA_QK_DIM ** -0.5)
    return o.reshape(B, S, MLA_HEADS * V_HEAD_DIM) @ w_mla_o


def dilated_group_attention(q, k, v, window, dilation):
    B, S, H, Dh = q.shape
    L = S // dilation
    W = window // dilation
    blk = DIL_BLOCK
    nb = -(-L // blk)
    Lp = nb * blk

    def to_blocks(t):
        t = t.reshape(B, L, dilation, H, Dh).transpose(0, 2, 1, 3, 4).reshape(B * dilation, L, H, Dh)
        t = jnp.pad(t, ((0, 0), (0, Lp - L), (0, 0), (0, 0)))
        return t.reshape(B * dilation, nb, blk, H, Dh)

    def with_prev(t):
        prev = jnp.pad(t[:, :-1], ((0, 0), (1, 0), (0, 0), (0, 0), (0, 0)))
        return jnp.concatenate([prev, t], axis=2)

    qb = to_blocks(q)
    kw = with_prev(to_blocks(k))
    vw = with_prev(to_blocks(v))
    s = jnp.einsum('nbqhd,nbkhd->nbhqk', qb, kw, preferred_element_type=jnp.float32) * (Dh ** -0.5)
    qi = jnp.arange(blk)
    kj = jnp.arange(2 * blk)
    dist = qi[:, None] + blk - kj[None, :]
    band = (dist >= 0) & (dist <= W)
    key_exists = (jnp.arange(nb)[:, None] * blk + kj[None, :] - blk) >= 0
    mask = band[None] & key_exists[:, None, :]
    s = jnp.where(mask[None, :, None], s, -jnp.inf)
    lse = jax.nn.logsumexp(s, axis=-1)
    p = jnp.exp(s - lse[..., None]).astype(v.dtype)
    o = jnp.einsum('nbhqk,nbkhd->nbqhd', p, vw)
    o = o.reshape(B * dilation, Lp, H, Dh)[:, :L]
    o = o.reshape(B, dilation, L, H, Dh).transpose(0, 2, 1, 3, 4).reshape(B, S, H, Dh)
    lse = lse.transpose(0, 1, 3, 2).reshape(B * dilation, Lp, H)[:, :L]
    lse = lse.reshape(B, dilation, L, H).transpose(0, 2, 1, 3).reshape(B, S, H)
    return o, lse


def dilated_branch(dil, cos, sin, w_dil_o):
    B, S, _ = dil.shape
    q, k, v = [t.reshape(B, S, DIL_HEADS, DIL_HEAD_DIM) for t in jnp.split(dil, 3, axis=-1)]

    def partial_rope(t):
        return jnp.concatenate([apply_rope(t[..., :DIL_ROT_DIM], cos, sin), t[..., DIL_ROT_DIM:]], axis=-1)

    q, k = partial_rope(q), partial_rope(k)
    outs, lses = [], []
    for gi, (window, dilation) in enumerate(DIL_PATTERNS):
        sl = slice(gi * DIL_HEADS_PER_GROUP, (gi + 1) * DIL_HEADS_PER_GROUP)
        o, lse = dilated_group_attention(q[:, :, sl], k[:, :, sl], v[:, :, sl], window, dilation)
        outs.append(o)
        lses.append(lse)
    wts = jax.nn.softmax(jnp.stack(lses), axis=0).astype(dil.dtype)
    o = jnp.einsum('gbsh,gbshd->bshd', wts, jnp.stack(outs))
    return o.reshape(B, S, DIL_HEADS_PER_GROUP * DIL_HEAD_DIM) @ w_dil_o


def token_mixer(h, cos_m, sin_m, cos_d, sin_d, w_in, q_a_norm_g, w_q_up, kv_a_norm_g, w_kv_up,
                w_mla_o, w_dil_o, w_out):
    proj = h @ w_in
    q_a, kv_a, dil, g_a, g_b = jnp.split(proj, IN_SPLITS, axis=-1)
    y_a = mla_branch(q_a, kv_a, cos_m, sin_m, q_a_norm_g, w_q_up, kv_a_norm_g, w_kv_up, w_mla_o)
    y_b = dilated_branch(dil, cos_d, sin_d, w_dil_o)
    merged = jax.nn.sigmoid(g_a) * y_a + jax.nn.sigmoid(g_b) * y_b
    return merged @ w_out


def swiglu(t, wg, wu, wd):
    return (jax.nn.silu(t @ wg) * (t @ wu)) @ wd


def moe(h, w_router, router_bias, w_exp_gate, w_exp_up, w_exp_down, w_sh_gate, w_sh_up, w_sh_down):
    N, D = h.shape
    scores = jax.nn.sigmoid((h @ w_router).astype(jnp.float32))
    biased = scores + router_bias.astype(jnp.float32)
    per_group = N_EXPERTS // N_EXPERT_GROUPS
    grp_score = lax.top_k(biased.reshape(N, N_EXPERT_GROUPS, per_group), 2)[0].sum(-1)
    _, grp_idx = lax.top_k(grp_score, TOPK_GROUPS)
    grp_mask = jax.nn.one_hot(grp_idx, N_EXPERT_GROUPS, dtype=jnp.float32).sum(1) > 0
    sel = jnp.where(jnp.repeat(grp_mask, per_group, axis=1), biased, -jnp.inf)
    _, top_idx = lax.top_k(sel, TOP_K)
    top_w = jnp.take_along_axis(scores, top_idx, axis=-1)
    top_w = top_w / (top_w.sum(-1, keepdims=True) + 1e-20) * ROUTED_SCALE

    A = N * TOP_K
    blk = MOE_BLOCK
    flat_e = top_idx.reshape(-1).astype(jnp.int32)
    flat_tok = jnp.repeat(jnp.arange(N, dtype=jnp.int32), TOP_K)
    flat_w = top_w.reshape(-1)
    order = jnp.argsort(flat_e)
    se, st, sw = flat_e[order], flat_tok[order], flat_w[order]
    counts = jnp.bincount(flat_e, length=N_EXPERTS).astype(jnp.int32)
    starts = jnp.cumsum(counts) - counts
    pcounts = (counts + blk - 1) // blk * blk
    pends = jnp.cumsum(pcounts)
    pstarts = pends - pcounts
    dest = pstarts[se] + (jnp.arange(A, dtype=jnp.int32) - starts[se])
    nb = -(-(A + N_EXPERTS * (blk - 1)) // blk)
    P = nb * blk
    buf_tok = jnp.full((P,), N, jnp.int32).at[dest].set(st)
    buf_w = jnp.zeros((P,), jnp.float32).at[dest].set(sw)
    blk_e = jnp.minimum(jnp.searchsorted(pends, jnp.arange(nb, dtype=jnp.int32) * blk, side='right'),
                        N_EXPERTS - 1).astype(jnp.int32)
    h_pad = jnp.concatenate([h, jnp.zeros((1, D), h.dtype)], axis=0)

    def expert_block(args):
        tok, wt, e = args
        xi = h_pad[tok]
        y = swiglu(xi, w_exp_gate[e], w_exp_up[e], w_exp_down[e])
        return y * wt[:, None].astype(y.dtype)

    yb = lax.map(expert_block, (buf_tok.reshape(nb, blk), buf_w.reshape(nb, blk), blk_e))
    routed = jax.ops.segment_sum(yb.reshape(P, D), buf_tok, num_segments=N + 1)[:N]
    return routed + swiglu(h, w_sh_gate, w_sh_up, w_sh_down)


def setup_inputs(seed: int = 0) -> dict:
    key = jax.random.key(seed)
    ks = jax.random.split(key, 32)
    L = DEPTH

    def nrm(k, shape, scale):
        return jax.random.normal(k, shape, jnp.float32) * scale

    return {
        "x": nrm(ks[0], (BATCH, SEQ, D_MODEL), 1.0),
        "c": nrm(ks[1], (BATCH, D_MODEL), 1.0),
        "positions": (jnp.arange(SEQ, dtype=jnp.int32)[None, :]
                      + jax.random.randint(ks[2], (BATCH, 1), 0, MAX_POS_OFFSET, dtype=jnp.int32)),
        "w_ada": nrm(ks[3], (L, D_MODEL, ADALN_CHUNKS * D_MODEL), 0.5 * D_MODEL ** -0.5),
        "b_ada": nrm(ks[4], (L, ADALN_CHUNKS * D_MODEL), 0.02),
        "attn_pre_g": 1.0 + nrm(ks[5], (L, D_MODEL), 0.1),
        "w_in": nrm(ks[6], (L, D_MODEL, IN_COLS), D_MODEL ** -0.5),
        "q_a_norm_g": 1.0 + nrm(ks[7], (L, Q_LORA_RANK), 0.1),
        "w_q_up": nrm(ks[8], (L, Q_LORA_RANK, MLA_HEADS * MLA_QK_DIM), Q_LORA_RANK ** -0.5),
        "kv_a_norm_g": 1.0 + nrm(ks[9], (L, KV_LORA_RANK), 0.1),
        "w_kv_up": nrm(ks[10], (L, KV_LORA_RANK, MLA_HEADS * (QK_NOPE_DIM + V_HEAD_DIM)), KV_LORA_RANK ** -0.5),
        "w_mla_o": nrm(ks[11], (L, MLA_HEADS * V_HEAD_DIM, D_MODEL), (MLA_HEADS * V_HEAD_DIM) ** -0.5),
        "w_dil_o": nrm(ks[12], (L, DIL_HEADS_PER_GROUP * DIL_HEAD_DIM, D_MODEL),
                       (DIL_HEADS_PER_GROUP * DIL_HEAD_DIM) ** -0.5),
        "w_out": nrm(ks[13], (L, D_MODEL, D_MODEL), D_MODEL ** -0.5),
        "attn_post_g": 1.0 + nrm(ks[14], (L, D_MODEL), 0.1),
        "ffn_pre_g": 1.0 + nrm(ks[15], (L, D_MODEL), 0.1),
        "w_router": nrm(ks[16], (L, D_MODEL, N_EXPERTS), D_MODEL ** -0.5),
        "router_bias": nrm(ks[17], (L, N_EXPERTS), 0.01),
        "w_exp_gate": nrm(ks[18], (L, N_EXPERTS, D_MODEL, EXPERT_DIM), D_MODEL ** -0.5),
        "w_exp_up": nrm(ks[19], (L, N_EXPERTS, D_MODEL, EXPERT_DIM), D_MODEL ** -0.5),
        "w_exp_down": nrm(ks[20], (L, N_EXPERTS, EXPERT_DIM, D_MODEL), EXPERT_DIM ** -0.5),
        "w_sh_gate": nrm(ks[21], (L, D_MODEL, SHARED_DIM), D_MODEL ** -0.5),
        "w_sh_up": nrm(ks[22], (L, D_MODEL, SHARED_DIM), D_MODEL ** -0.5),
        "w_sh_down": nrm(ks[23], (L, SHARED_DIM, D_MODEL), SHARED_DIM ** -0.5),
        "ffn_post_g": 1.0 + nrm(ks[24], (L, D_MODEL), 0.1),
    }


def reference(x, c, positions, w_ada, b_ada, attn_pre_g, w_in, q_a_norm_g, w_q_up, kv_a_norm_g,
              w_kv_up, w_mla_o, w_dil_o, w_out, attn_post_g, ffn_pre_g, w_router, router_bias,
              w_exp_gate, w_exp_up, w_exp_down, w_sh_gate, w_sh_up, w_sh_down, ffn_post_g):
    B, S, D = x.shape
    cos_m, sin_m = rope_cos_sin(positions, QK_ROPE_DIM)
    cos_d, sin_d = rope_cos_sin(positions, DIL_ROT_DIM)
    for l in range(DEPTH):
        mod = (jax.nn.silu(c) @ w_ada[l] + b_ada[l])[:, None, :]
        shift_a, scale_a, gate_a, shift_f, scale_f, gate_f = jnp.split(mod, ADALN_CHUNKS, axis=-1)
        h = rms_norm(x, attn_pre_g[l]) * (1.0 + scale_a) + shift_a
        y = token_mixer(h, cos_m, sin_m, cos_d, sin_d, w_in[l], q_a_norm_g[l], w_q_up[l],
                        kv_a_norm_g[l], w_kv_up[l], w_mla_o[l], w_dil_o[l], w_out[l])
        x = x + gate_a * rms_norm(y, attn_post_g[l])
        h = rms_norm(x, ffn_pre_g[l]) * (1.0 + scale_f) + shift_f
        y = moe(h.reshape(B * S, D), w_router[l], router_bias[l], w_exp_gate[l], w_exp_up[l],
                w_exp_down[l], w_sh_gate[l], w_sh_up[l], w_sh_down[l]).reshape(B, S, D)
        x = x + gate_f * rms_norm(y, ffn_post_g[l])
    return x
```

```python
import math
import numpy as np
import ml_dtypes
from contextlib import ExitStack
import concourse.bass as bass
import concourse.mybir as mybir
from concourse.bass_utils import run_bass_kernel_spmd

F32 = mybir.dt.float32
BF16 = mybir.dt.bfloat16
I32 = mybir.dt.int32
U8 = mybir.dt.uint8
ALU = mybir.AluOpType
AF = mybir.ActivationFunctionType

SAME_ENGINE_SYNC = True

D = 2048
S = 4096
NT = 2048
NE = 64
IN_COLS = 9792
C_QA, C_CKV, C_KR, C_DQ, C_DK, C_DV, C_GA, C_GB = 0, 512, 1024, 1088, 2624, 4160, 5696, 7744
EPS = 1e-6
PI = math.pi
DIL = ((128, 1), (512, 4), (2048, 16))


class Buf:
    __slots__ = ("name", "w", "r", "excl")

    def __init__(self, name="", excl=False):
        self.name = name
        self.w = None
        self.r = {}
        self.excl = excl


class Op:
    __slots__ = ("eng", "fn", "deps", "signal", "semval", "sem", "is_dma", "guard", "idx")

    def __init__(self, eng, fn, is_dma):
        self.eng = eng
        self.fn = fn
        self.deps = []
        self.signal = False
        self.semval = None
        self.sem = None
        self.is_dma = is_dma
        self.guard = None


class Prog:
    ENGS = ("pe", "act", "dve", "pool", "sp")

    def __init__(self, nc, stack):
        self.nc = nc
        self.ops = {e: [] for e in self.ENGS}
        self.last = {e: None for e in self.ENGS}
        self.barrier_deps = []
        self.dma_since_barrier = []
        self.reg_requests = []
        self.regs = {}
        n_dma_sems = {"sp": 24, "pool": 12, "act": 4, "spz": 24}
        self.nobar_rings = {"spz"}
        self.csem = {}
        for e in ("pe", "act", "dve", "pool"):
            self.csem[e] = stack.enter_context(nc.semaphore("c_" + e))
        self.dsem, self.dsem_last, self.dsem_cnt, self.dsem_rr = {}, {}, {}, {}
        for e, n in n_dma_sems.items():
            self.dsem[e] = [stack.enter_context(nc.semaphore("d_%s%d" % (e, i))) for i in range(n)]
            self.dsem_last[e] = [None] * n
            self.dsem_cnt[e] = [0] * n
            self.dsem_rr[e] = 0

    def add(self, eng, fn, reads=(), writes=(), dma=False, ring=None):
        op = Op(eng, fn, dma)
        ring = ring or eng
        op.idx = len(self.ops[eng])
        best = {}
        dmas = {}
        ex = [b for b in reads if b.excl]
        if ex:
            reads = [b for b in reads if not b.excl]
            writes = list(writes) + [b for b in ex if b not in writes]

        def adddep(d):
            if d is None:
                return
            if d.is_dma:
                dmas[id(d)] = d
            else:
                b = best.get(d.eng)
                if b is None or d.idx > b.idx:
                    best[d.eng] = d

        for b in reads:
            adddep(b.w)
        for b in writes:
            adddep(b.w)
            for r in b.r.values():
                adddep(r)
        for d in self.barrier_deps:
            adddep(d)
        fdeps = []
        for d in list(best.values()) + list(dmas.values()):
            if not d.is_dma and d.eng == eng and not dma:
                if eng == "pe" or not SAME_ENGINE_SYNC:
                    continue
            fdeps.append(d)
            d.signal = True
        op.deps = fdeps
        for b in reads:
            b.r[("d", id(op)) if dma else eng] = op
        for b in writes:
            b.w = op
            b.r = {}
        if dma:
            i = self.dsem_rr[ring]
            n = len(self.dsem[ring])
            self.dsem_rr[ring] = (i + 1) % n
            op.sem = self.dsem[ring][i]
            op.guard = self.dsem_last[ring][i]
            self.dsem_cnt[ring][i] += 1
            op.semval = 16 * self.dsem_cnt[ring][i]
            self.dsem_last[ring][i] = op
            op.signal = True
            if ring not in self.nobar_rings:
                self.dma_since_barrier.append(op)
        else:
            self.last[eng] = op
        self.ops[eng].append(op)
        return op

    def barrier(self):
        deps = [o for o in self.last.values() if o is not None] + list(self.dma_since_barrier)
        for e in self.dsem:
            if e in self.nobar_rings:
                continue
            for o in self.dsem_last[e]:
                if o is not None and o not in deps:
                    deps.append(o)
        self.barrier_deps = deps
        self.dma_since_barrier = []

    def dma(self, out, in_, reads=(), writes=(), eng="sp", ring=None):
        return self.add(eng, lambda e: e.dma_start(out=out, in_=in_), reads, writes, dma=True, ring=ring)

    def emit(self):
        nc = self.nc
        final_deps = [o for o in self.last.values() if o is not None]
        for e in self.dsem:
            for o in self.dsem_last[e]:
                if o is not None:
                    final_deps.append(o)
        for d in final_deps:
            d.signal = True
        for e in ("pe", "act", "dve", "pool"):
            cnt = 0
            for op in self.ops[e]:
                if op.is_dma:
                    continue
                if op.signal:
                    cnt += 1
                    op.semval = cnt
                    op.sem = self.csem[e]
        stats = {}

        def run_engine(ename, eh, final=False):
            waited = {}
            nw = 0

            def wait_for(d):
                nonlocal nw
                key = id(d.sem)
                if waited.get(key, 0) >= d.semval:
                    return
                waited[key] = d.semval
                eh.wait_ge(d.sem, d.semval)
                nw += 1

            if ename == "pool":
                for v in self.reg_requests:
                    self.regs[v] = eh.to_reg(v)
            for op in self.ops[ename]:
                for d in op.deps:
                    wait_for(d)
                if op.guard is not None:
                    wait_for(op.guard)
                ins = op.fn(eh)
                if op.signal:
                    ins.then_inc(op.sem, 16 if op.is_dma else 1)
            if final:
                for d in final_deps:
                    wait_for(d)
            stats[ename] = (len(self.ops[ename]), nw)

        with nc.Block() as block:
            @block.tensor
            def _(t):
                run_engine("pe", t)

            @block.scalar
            def _(s):
                run_engine("act", s)

            @block.vector
            def _(v):
                run_engine("dve", v)

            @block.gpsimd
            def _(g):
                run_engine("pool", g)

            @block.sync
            def _(s):
                run_engine("sp", s, final=True)
        return stats


class StopBuild(Exception):
    pass


class Arena:
    def __init__(self, ap, size):
        self.ap = ap
        self.size = size
        self.off = 0

    def alloc(self, free_shape, dtype, parts=128):
        esz = {F32: 4, BF16: 2, I32: 4, U8: 1}[dtype]
        n = esz
        for s in free_shape:
            n *= s
        off = (self.off + 63) // 64 * 64
        assert off + n <= self.size, ("arena overflow", off, n, self.size)
        self.off = off + n
        a = self.ap[0:parts, off:off + n].bitcast(dtype)
        if len(free_shape) == 2:
            a = a.rearrange("p (a b) -> p a b", a=free_shape[0])
        elif len(free_shape) == 3:
            a = a.rearrange("p (a b c) -> p a b c", a=free_shape[0], b=free_shape[1])
        return a

    def mark(self):
        return self.off

    def release(self, m):
        self.off = m


def mask_table(p):
    masks = []
    idx = {}
    ki = np.arange(128)[:, None]
    qi = np.arange(128)[None, :]

    def add(key, fn):
        m = np.zeros((128, 512), np.float32)
        for a in range(4):
            m[:, a * 128:(a + 1) * 128] = fn(a)
        idx[key] = len(masks)
        masks.append(m)

    for c in range(4):
        add(("m", "own", c), lambda a, c=c: (np.ones((128, 128)) if a > c else ((ki <= qi) if a == c else np.zeros((128, 128)))))
        add(("m", "oth", c), lambda a, c=c: (np.ones((128, 128)) if a > c else (np.full((128, 128), float(p)) if a == c else np.zeros((128, 128)))))
    def dmask(pp, kind, rho, w, d):
        off = 0 if kind == "own" else 128 * (2 * pp - 1)
        ms = []
        for a in range(4):
            delta = 256 * (a + rho) + off + qi - ki
            ms.append(((delta >= 0) & (delta <= w) & (delta % d == 0)).astype(np.float32))
        return np.concatenate(ms, axis=1)

    for g, (w, d) in enumerate(DIL):
        for kind in ("own", "oth"):
            for rho in range(-3, 10):
                m0, m1 = dmask(0, kind, rho, w, d), dmask(1, kind, rho, w, d)
                if m0.any() or m1.any():
                    idx[("d", g, kind, rho)] = len(masks)
                    masks.append(m1 if p == 1 else m0)
    return np.stack(masks), idx


def build(debug=False, stop_after=None):
    nc = bass.Bass("TRN2", target_bir_lowering=False)
    _, midx = mask_table(0)
    _, midx1 = mask_table(1)
    assert midx == midx1
    NM = len(midx)

    def din(name, shape, dt=F32):
        return nc.dram_tensor(name, list(shape), dt, kind="ExternalInput").ap()

    def dscr(name, shape, dt):
        return nc.dram_tensor(name, list(shape), dt, kind=("ExternalOutput" if debug else "Internal")).ap()

    x_own = din("x_own", [NT, D])
    x_oth = din("x_oth", [NT, D])
    pos_in = din("pos", [2, NT], I32)
    colv = din("colv", [128, 128])
    rowv = din("rowv", [1, 7 * D + 64])
    cst_bf = din("cst_bf", [128, 640], BF16)
    cst_f = din("cst_f", [128, 128])
    masks_in = din("masks", [128, NM, 512], BF16)
    w_ada = din("w_ada", [D, 6 * D])
    w_in = din("w_in", [D, IN_COLS])
    w_q_up = din("w_q_up", [512, 1536])
    w_kv_up = din("w_kv_up", [512, 2048])
    w_mla_o = din("w_mla_o", [1024, D])
    w_dil_o = din("w_dil_o", [512, D])
    w_out = din("w_out", [D, D])
    w_router = din("w_router", [D, NE])
    w_eg = din("w_exp_gate", [NE * 128, 8192])
    w_eu = din("w_exp_up", [NE * 128, 8192])
    w_ed = din("w_exp_down", [NE * 128, 8192])
    w_sg = din("w_sh_gate", [D, 512])
    w_su = din("w_sh_up", [D, 512])
    w_sd = din("w_sh_down", [512, D])
    out_d = nc.dram_tensor("out", [NT, D], F32, kind="ExternalOutput").ap()

    QT_m = dscr("QT_m", [8, 192, NT], BF16)
    KT_m = dscr("KT_m", [8, 128, S], BF16)
    KRT = dscr("KRT", [64, S], BF16)
    V_m = dscr("V_m", [8, 128, 32, 128], BF16)
    QT_d = dscr("QT_d", [12, 128, NT], BF16)
    KT_d = dscr("KT_d", [12, 128, S], BF16)
    V_d = dscr("V_d", [12, 128, 32, 128], BF16)
    SG = dscr("SG", [32, 128, NT], BF16)
    OT_m = dscr("OT_m", [8, 128, NT], BF16)
    OT_d = dscr("OT_d", [4, 128, NT], BF16)
    X1 = dscr("X1", [NT, D], F32)
    H2T = dscr("H2T", [128, 16, NT], BF16)
    H2K = dscr("H2K", [NT, D], BF16)
    SHO = dscr("SHO", [NT, D], BF16)
    NBLK = 96
    NSLOT = NBLK * 256
    XG = dscr("XG", [NSLOT, D], BF16)
    YG = dscr("YG", [NSLOT, D], BF16)

    st = ExitStack()
    with st:
        P = Prog(nc, st)
        P.reg_requests = [96 * 256 - 1, NE * 128 - 1]
        ASZ = 206 * 1024
        arena_t = st.enter_context(nc.sbuf_tensor("arena", [128, ASZ], U8))
        A = Arena(arena_t[:, :], ASZ)
        PS = [st.enter_context(nc.psum_tensor("ps%d" % i, [128, 512], F32))[:] for i in range(8)]
        PSB = [Buf("ps%d" % i, excl=True) for i in range(8)]
        PSbf = [p.bitcast(BF16) for p in PS]

        cbf = A.alloc([640], BF16)
        Utri = cbf[:, 512:640]
        ident = cbf[:, 0:128]
        ones = cbf[:, 128:256]
        perm64 = cbf[0:64, 256:320]
        perm32 = cbf[0:32, 320:352]
        cf = A.alloc([128], F32)
        colv_sb = A.alloc([128], F32)
        rbias = A.alloc([NE], F32)
        AB = A.alloc([4, 16], F32)
        Gab = A.alloc([2, D], F32)
        Bc = Buf("consts")
        Bab = Buf("AB")
        Bg = Buf("G")
        P.dma(cbf, cst_bf, writes=[Bc])
        P.dma(cf, cst_f, writes=[Bc])
        P.dma(colv_sb, colv, writes=[Bc])
        P.dma(rbias, rowv[:, 4 * D:4 * D + NE].partition_broadcast(128), writes=[Bc])
        qg = colv_sb[:, 112:116]
        kvg = colv_sb[:, 116:120]

        def act(out, in_, func, reads, writes, **kw):
            return P.add("act", lambda e: e.activation(out=out, in_=in_, func=func, **kw), reads, writes)

        def tt(eng, out, in0, in1, op, reads, writes):
            return P.add(eng, lambda e: e.tensor_tensor(out=out, in0=in0, in1=in1, op=op), reads, writes)

        def ts(eng, out, in0, s1, s2, op0, op1, reads, writes):
            return P.add(eng, lambda e: e.tensor_scalar(out=out, in0=in0, scalar1=s1, scalar2=s2, op0=op0, op1=op1), reads, writes)

        def stt(eng, out, in0, scalar, in1, op0, op1, reads, writes):
            return P.add(eng, lambda e: e.scalar_tensor_tensor(out=out, in0=in0, scalar=scalar, in1=in1, op0=op0, op1=op1), reads, writes)

        def cp(eng, out, in_, reads, writes):
            return P.add(eng, lambda e: e.tensor_copy(out=out, in_=in_), reads, writes)

        def mm(out, lhsT, rhs, start, stop, reads, writes):
            return P.add("pe", lambda e: e.matmul(out, lhsT=lhsT, rhs=rhs, start=start, stop=stop), reads, writes)

        def rsqrt_chain(dst, src, scale, reads, writes, tmp):
            ts("dve", tmp, src, scale, EPS, ALU.mult, ALU.add, reads, writes)
            act(tmp, tmp, AF.Sqrt, writes, writes)
            P.add("dve", lambda e: e.reciprocal(out=dst, in_=tmp), writes, writes)

        zt = A.alloc([D], BF16)
        Bz = Buf("zero")
        P.add("pool", lambda e: e.memset(zt, 0.0), [], [Bz])
        XGz = XG.rearrange("(c p r) d -> c p r d", p=128, r=8)
        Bxgz = [Buf("xgz%d" % i) for i in range(NSLOT // 1024)]
        mA = A.mark()
        rows_sb = A.alloc([4 * D], F32)
        Brows = Buf("rows")
        P.dma(rows_sb, rowv[:, 0:4 * D].partition_broadcast(128), writes=[Brows])
        sc = A.alloc([16], BF16)
        scB = A.alloc([16, 128], BF16)
        Bsc = Buf("sc")
        act(sc, colv_sb[:, 0:16], AF.Silu, [Bc], [Bsc])
        for k in range(16):
            cp("dve", scB[:, k, :], sc[:, k:k + 1].to_broadcast([128, 128]), [Bsc], [Bsc])
        wada_v = w_ada.rearrange("(k p) n -> p k n", p=128)
        wa_t = [A.alloc([16, 512], BF16) for _ in range(2)]
        wa_b = [Buf("wa0"), Buf("wa1")]
        modT = A.alloc([64], F32)
        col_segs = [0, 1]
        gi = 0
        for si, seg in enumerate(col_segs):
            for n in range(4):
                wt, wb = wa_t[gi % 2], wa_b[gi % 2]
                gi += 1
                c0 = seg * D + n * 512
                P.dma(wt, wada_v[:, :, c0:c0 + 512], writes=[wb], eng="pool")
                for m in range(4):
                    col = si * 16 + n * 4 + m
                    for k in range(16):
                        mm(PS[0][:, col:col + 1], wt[:, k, m * 128:(m + 1) * 128], sc[:, k:k + 1], k == 0, k == 15,
                           [wb, Bsc], [PSB[0]])
        cp("dve", modT[:, 0:32], PS[0][:, 0:32], [PSB[0]], [Bab])
        tt("dve", modT[:, 0:32], modT[:, 0:32], colv_sb[:, 16:48], ALU.add, [Bab, Bc], [Bab])
        stt("dve", AB[:, 0, :], modT[:, 16:32], 1.0, colv_sb[:, 80:96], ALU.add, ALU.mult, [Bab, Bc], [Bab])
        cp("dve", AB[:, 1, :], modT[:, 0:16], [Bab], [Bab])
        for gsel, seg in enumerate([2, 5]):
            for n in range(4):
                wt, wb = wa_t[gi % 2], wa_b[gi % 2]
                gi += 1
                c0 = seg * D + n * 512
                P.dma(wt, wada_v[:, :, c0:c0 + 512], writes=[wb], eng="pool")
                bank = 1 + (n % 2)
                for k in range(16):
                    mm(PS[bank], scB[:, k, :], wt[:, k, :], k == 0, k == 15, [wb, Bsc], [PSB[bank]])
                dst = Gab[:, gsel, n * 512:(n + 1) * 512]
                tt("dve", dst, PS[bank], rows_sb[:, gsel * D + n * 512: gsel * D + (n + 1) * 512], ALU.add, [PSB[bank], Brows], [Bg])
                tt("dve", dst, dst, rows_sb[:, (2 + gsel) * D + n * 512:(2 + gsel) * D + (n + 1) * 512], ALU.mult, [Bg, Brows], [Bg])
        P.barrier()
        A.release(mA)
        if debug:
            dAB = nc.dram_tensor("dbgAB", [128, 64], F32, kind="ExternalOutput").ap()
            dG = nc.dram_tensor("dbgG", [128, 2 * D], F32, kind="ExternalOutput").ap()
            P.dma(dAB, AB, reads=[Bab])
            P.dma(dG, Gab, reads=[Bg])
        if stop_after == "A":
            return nc, P.emit()

        cosM = A.alloc([NT], BF16, parts=64)
        sinM = A.alloc([NT], BF16, parts=64)
        cosD = A.alloc([NT], BF16, parts=32)
        sinD = A.alloc([NT], BF16, parts=32)
        Brope = Buf("rope")

        hT = A.alloc([16, NT], BF16)
        BhT = [Buf("hT%d" % i) for i in range(16)]
        wq_sb = A.alloc([4, 1536], BF16)
        wkv_sb = A.alloc([4, 2048], BF16)
        Bwq, Bwkv = Buf("wq"), Buf("wkv")
        P.dma(wq_sb, w_q_up.rearrange("(k p) n -> p k n", p=128), writes=[Bwq], eng="pool")
        P.dma(wkv_sb, w_kv_up.rearrange("(k p) n -> p k n", p=128), writes=[Bwkv], eng="pool")
        stat = A.alloc([32, 4], F32)
        Bstat = [Buf("stat%d" % i) for i in range(32)]
        mBov = A.mark()
        xt = [A.alloc([D], F32) for _ in range(2)]
        Bxt = [Buf("xt0"), Buf("xt1")]
        xn = [A.alloc([D], BF16) for _ in range(2)]
        Bxn = [Buf("xn0"), Buf("xn1")]
        junk = A.alloc([D], BF16)
        Bjunk = Buf("junk")
        evt = [A.alloc([8, 128], F32) for _ in range(2)]
        Bevt = [Buf("evt0"), Buf("evt1")]
        mRt = A.mark()
        posi = A.alloc([NT], I32, parts=64)
        posf = A.alloc([NT], F32, parts=64)
        ang = A.alloc([NT], F32, parts=64)
        posf2 = A.alloc([NT], F32, parts=64)
        Bpos = Buf("pos")
        A.release(mRt)
        xt += [A.alloc([D], F32) for _ in range(2)]
        Bxt += [Buf("xt2"), Buf("xt3")]
        xn += [A.alloc([D], BF16) for _ in range(2)]
        Bxn += [Buf("xn2"), Buf("xn3")]
        A.release(mBov)
        wt_t = [A.alloc([16, 512], BF16) for _ in range(2)]
        wt_b = [Buf("wt0"), Buf("wt1")]
        lat = A.alloc([4, NT], BF16)
        Blat = [Buf("lat%d" % i) for i in range(4)]
        sq = [A.alloc([512], BF16) for _ in range(2)]
        Bsq = [Buf("sq0"), Buf("sq1")]
        rbc = A.alloc([512], F32)
        rtmp = A.alloc([512], F32)
        Brbc = Buf("rbc")
        NSTG = 6
        stg = [A.alloc([512], BF16) for _ in range(NSTG)]
        Bstg = [Buf("stg%d" % i) for i in range(NSTG)]
        rt = [A.alloc([512], F32) for _ in range(2)]
        Brt = [Buf("rt0"), Buf("rt1")]
        w_in_v = w_in.rearrange("(k p) n -> p k n", p=128)
        state = {"stg": 0, "wt": 0, "bank": 0, "rt": 0, "sq": 0, "ck": 0}

        def checkpoint():
            state["ck"] += 1
            if stop_after == "B2:%d" % state["ck"]:
                raise StopBuild()

        def next_stg():
            i = state["stg"]
            state["stg"] = (i + 1) % NSTG
            return stg[i], Bstg[i]

        def next_bank(lo=0, hi=6):
            i = state["bank"]
            state["bank"] = i + 1
            b = lo + i % (hi - lo)
            return PS[b], PSB[b], b

        def load_w(c0, ncols):
            i = state["wt"]
            state["wt"] = i + 1
            wt, wb = wt_t[i % 2], wt_b[i % 2]
            P.dma(wt[:, :, 0:ncols], w_in_v[:, :, c0:c0 + ncols], writes=[wb], eng="pool")
            return wt, wb

        def rope_rows(tile, Btile, rows, perm, ctab, stab, tok0):
            ps, pb, _ = next_bank(6, 8)
            mm(ps[0:rows, :], perm, tile[0:rows, :], True, True, [Btile, Bc], [pb])
            i = state["rt"]
            state["rt"] = i + 1
            r1, B1_ = rt[i % 2], Brt[i % 2]
            tt("dve", r1[0:rows, :], ps[0:rows, :], stab[0:rows, tok0:tok0 + 512], ALU.mult, [pb, Brope], [B1_])
            tt("dve", tile[0:rows, :], tile[0:rows, :], ctab[0:rows, tok0:tok0 + 512], ALU.mult, [Btile, Brope], [Btile])
            tt("dve", tile[0:rows, :], tile[0:rows, :], r1[0:rows, :], ALU.add, [Btile, B1_], [Btile])

        try:
          for tsi, xsrc in enumerate((x_own, x_oth)):
            own = tsi == 0
            tokbase = tsi * NT
            P.barrier()
            P.dma(posi, pos_in[tsi:tsi + 1, :].partition_broadcast(64), writes=[Bpos])
            cp("dve", posf, posi, [Bpos], [Bpos])
            for (rows, fcol, scol, ctab, stab) in ((64, 0, 1, cosM, sinM), (32, 2, 3, cosD, sinD)):
                for (shift, tab, signed) in ((0.0, stab, True), (0.5 * PI, ctab, False)):
                    a = ang[0:rows, :]
                    kf = posf2[0:rows, :]
                    ts("dve", a, posf[0:rows, :], cf[0:rows, fcol:fcol + 1], shift, ALU.mult, ALU.add, [Bpos, Bc], [Bpos])
                    ts("dve", kf, a, 1.0 / (2 * PI), 0.0, ALU.mult, ALU.add, [Bpos], [Bpos])
                    cp("dve", posi[0:rows, :], kf, [Bpos], [Bpos])
                    cp("dve", kf, posi[0:rows, :], [Bpos], [Bpos])
                    stt("dve", a, kf, -2 * PI, a, ALU.mult, ALU.add, [Bpos], [Bpos])
                    ts("dve", kf, a, PI, -2 * PI, ALU.is_gt, ALU.mult, [Bpos], [Bpos])
                    tt("dve", a, a, kf, ALU.add, [Bpos], [Bpos])
                    ts("dve", kf, a, -PI, 2 * PI, ALU.is_lt, ALU.mult, [Bpos], [Bpos])
                    tt("dve", a, a, kf, ALU.add, [Bpos], [Bpos])
                    ts("dve", a, a, 3.141592, -3.141592, ALU.min, ALU.max, [Bpos], [Bpos])
                    act(a, a, AF.Sin, [Bpos], [Bpos])
                    dst = tab[0:rows, :]
                    if signed:
                        ts("dve", dst, a, cf[0:rows, scol:scol + 1], 0.0, ALU.mult, ALU.add, [Bpos, Bc], [Brope])
                    else:
                        cp("dve", dst, a, [Bpos], [Brope])
            P.barrier()
            for ti in range(16):
                i2 = ti % 4
                sidx = tsi * 16 + ti
                P.dma(xt[i2], xsrc[ti * 128:(ti + 1) * 128, :], writes=[Bxt[i2]])
                act(junk, xt[i2], AF.Square, [Bxt[i2]], [Bjunk, Bstat[sidx]], accum_out=stat[:, sidx, 0:1])
                rsqrt_chain(stat[:, sidx, 1:2], stat[:, sidx, 0:1], 1.0 / D, [Bstat[sidx]], [Bstat[sidx]], stat[:, sidx, 2:3])
                act(xn[i2], xt[i2], AF.Copy, [Bxt[i2], Bstat[sidx]], [Bxn[i2]], scale=stat[:, sidx, 1:2])
                for half in range(2):
                    bank = 6 + half
                    for kk in range(8):
                        k = half * 8 + kk
                        P.add("pe", lambda e, bank=bank, kk=kk, k=k, i2=i2: e.transpose(
                            out=PSbf[bank][:, kk * 128:(kk + 1) * 128], in_=xn[i2][:, k * 128:(k + 1) * 128], identity=ident),
                            [Bxn[i2], Bc], [PSB[bank]])
                    src = PSbf[bank][:, 0:1024].rearrange("p (k n) -> p k n", k=8)
                    a1 = AB[:, 0, half * 8:(half + 1) * 8].unsqueeze(2).to_broadcast([128, 8, 128])
                    b1 = AB[:, 1, half * 8:(half + 1) * 8].unsqueeze(2).to_broadcast([128, 8, 128])
                    tt("dve", evt[half], src, a1, ALU.mult, [PSB[bank], Bab], [Bevt[half]])
                    tt("dve", hT[:, half * 8:(half + 1) * 8, ti * 128:(ti + 1) * 128], evt[half], b1, ALU.add,
                       [Bevt[half], Bab], [BhT[ti]])

            P.barrier()
            if debug and own:
                dH = nc.dram_tensor("dbgH", [128, 16, NT], BF16, kind="ExternalOutput").ap()
                dR = nc.dram_tensor("dbgR", [64, 4 * NT], BF16, kind="ExternalOutput").ap()
                P.dma(dH, hT, reads=BhT)
                P.dma(dR[:, 0:NT], cosM, reads=[Brope])
                P.dma(dR[:, NT:2 * NT], sinM, reads=[Brope])
                P.dma(dR[0:32, 2 * NT:3 * NT], cosD, reads=[Brope])
                P.dma(dR[0:32, 3 * NT:4 * NT], sinD, reads=[Brope])
            if stop_after == "B1":
                return nc, P.emit()
            def latent_segment(c0, gcol, is_q):
                wt, wb = load_w(c0, 512)
                for tg in range(4):
                    tsl = slice(tg * 512, (tg + 1) * 512)
                    hreads = [BhT[tg * 4 + j] for j in range(4)]
                    for m in range(4):
                        ps, pb, _ = next_bank()
                        for k in range(16):
                            mm(ps, wt[:, k, m * 128:(m + 1) * 128], hT[:, k, tsl], k == 0, k == 15, hreads + [wb], [pb])
                        i = state["sq"]
                        state["sq"] = i + 1
                        s_, bs_ = sq[i % 2], Bsq[i % 2]
                        act(s_, ps, AF.Square, [pb], [bs_])
                        ts("dve", lat[:, m, tsl], ps, gcol[:, m:m + 1], 0.0, ALU.mult, ALU.add, [pb, Bc], [Blat[tg]])
                        mm(PS[6], ones, s_, m == 0, m == 3, [bs_, Bc], [PSB[6]])
                    rsqrt_chain(rbc, PS[6], 1.0 / 512, [PSB[6]], [Brbc], rtmp)
                    for m in range(4):
                        tt("dve", lat[:, m, tsl], lat[:, m, tsl], rbc, ALU.mult, [Blat[tg], Brbc], [Blat[tg]])

            def up_fm(wsb, Bw, col0, M, tg, dst_ap, rope=None):
                tsl = slice(tg * 512, (tg + 1) * 512)
                ps, pb, _ = next_bank()
                for k in range(4):
                    mm(ps[0:M, :], wsb[:, k, col0:col0 + M], lat[:, k, tsl], k == 0, k == 3, [Bw, Blat[tg]], [pb])
                sg_, bsg = next_stg()
                act(sg_[0:M, :], ps[0:M, :], AF.Copy, [pb], [bsg])
                if rope is not None:
                    rope_rows(sg_, bsg, *rope, tg * 512)
                P.dma(dst_ap, sg_[0:M, :], reads=[bsg])

            tcols = slice(tokbase, tokbase + NT)
            if own:
                latent_segment(C_QA, qg, True)
                checkpoint()
                for tg in range(4):
                    c512 = slice(tg * 512, (tg + 1) * 512)
                    for h in range(8):
                        up_fm(wq_sb, Bwq, h * 192, 128, tg, QT_m[h, 0:128, c512])
                        up_fm(wq_sb, Bwq, h * 192 + 128, 64, tg, QT_m[h, 128:192, c512], rope=(64, perm64, cosM, sinM))
            checkpoint()
            latent_segment(C_CKV, kvg, False)
            for tg in range(4):
                c512 = slice(tokbase + tg * 512, tokbase + (tg + 1) * 512)
                for h in range(8):
                    up_fm(wkv_sb, Bwkv, h * 256, 128, tg, KT_m[h, :, c512])
                if tg == 0:
                    checkpoint()
                wkv_h = wkv_sb.rearrange("p k (h c) -> p k h c", c=256)
                for tt_ in range(4):
                    tile = tsi * 16 + tg * 4 + tt_
                    tok = slice(tg * 512 + tt_ * 128, tg * 512 + (tt_ + 1) * 128)
                    for hh in range(2):
                        ps, pb, _ = next_bank()
                        for k in range(4):
                            mm(ps.rearrange("p (h c) -> p h c", h=4), lat[:, k, tok], wkv_h[:, k, hh * 4:(hh + 1) * 4, 128:256],
                               k == 0, k == 3, [Bwkv, Blat[tg]], [pb])
                        sg_, bsg = next_stg()
                        act(sg_, ps, AF.Copy, [pb], [bsg])
                        P.dma(V_m[hh * 4:(hh + 1) * 4, :, tile, :].rearrange("h p d -> p h d"),
                              sg_.rearrange("p (h d) -> p h d", h=4), reads=[bsg])
            checkpoint()
            wt, wb = load_w(C_KR, 64)
            for tg in range(4):
                tsl = slice(tg * 512, (tg + 1) * 512)
                hreads = [BhT[tg * 4 + j] for j in range(4)]
                ps, pb, _ = next_bank()
                for k in range(16):
                    mm(ps[0:64, :], wt[:, k, 0:64], hT[:, k, tsl], k == 0, k == 15, hreads + [wb], [pb])
                sg_, bsg = next_stg()
                act(sg_[0:64, :], ps[0:64, :], AF.Copy, [pb], [bsg])
                rope_rows(sg_, bsg, 64, perm64, cosM, sinM, tg * 512)
                P.dma(KRT[:, tokbase + tg * 512: tokbase + (tg + 1) * 512], sg_[0:64, :], reads=[bsg])

            def dil_fm(cbase, dstT, ncol_tok_base):
                for cg in range(3):
                    wt, wb = load_w(cbase + cg * 512, 512)
                    for tg in range(4):
                        tsl = slice(tg * 512, (tg + 1) * 512)
                        hreads = [BhT[tg * 4 + j] for j in range(4)]
                        for m in range(4):
                            h = cg * 4 + m
                            ps, pb, _ = next_bank()
                            for k in range(16):
                                mm(ps, wt[:, k, m * 128:(m + 1) * 128], hT[:, k, tsl], k == 0, k == 15, hreads + [wb], [pb])
                            sg_, bsg = next_stg()
                            act(sg_, ps, AF.Copy, [pb], [bsg])
                            rope_rows(sg_, bsg, 32, perm32, cosD, sinD, tg * 512)
                            P.dma(dstT[h, :, ncol_tok_base + tg * 512: ncol_tok_base + (tg + 1) * 512], sg_, reads=[bsg])

            checkpoint()
            if own:
                dil_fm(C_DQ, QT_d, 0)
            checkpoint()
            dil_fm(C_DK, KT_d, tokbase)
            checkpoint()
            for cg in range(3):
                wt, wb = load_w(C_DV + cg * 512, 512)
                for ti in range(16):
                    tile = tsi * 16 + ti
                    ps, pb, _ = next_bank()
                    for k in range(16):
                        mm(ps, hT[:, k, ti * 128:(ti + 1) * 128], wt[:, k, :], k == 0, k == 15, [BhT[ti], wb], [pb])
                    sg_, bsg = next_stg()
                    act(sg_, ps, AF.Copy, [pb], [bsg])
                    P.dma(V_d[cg * 4:(cg + 1) * 4, :, tile, :].rearrange("h p d -> p h d"),
                          sg_.rearrange("p (h d) -> p h d", h=4), reads=[bsg])
            checkpoint()
            if own:
                for cg in range(8):
                    wt, wb = load_w(C_GA + cg * 512, 512)
                    for tg in range(4):
                        tsl = slice(tg * 512, (tg + 1) * 512)
                        hreads = [BhT[tg * 4 + j] for j in range(4)]
                        for m in range(4):
                            ps, pb, _ = next_bank()
                            for k in range(16):
                                mm(ps, wt[:, k, m * 128:(m + 1) * 128], hT[:, k, tsl], k == 0, k == 15, hreads + [wb], [pb])
                            sg_, bsg = next_stg()
                            act(sg_, ps, AF.Sigmoid, [pb], [bsg])
                            P.dma(SG[cg * 4 + m, :, tsl], sg_, reads=[bsg])
        except StopBuild:
            return nc, P.emit()
        P.barrier()
        A.release(mA)
        if stop_after == "B":
            return nc, P.emit()

        mC = A.mark()
        masks_sb = A.alloc([NM, 512], BF16)
        Bmask = Buf("masks")
        for i0 in range(0, NM, 16):
            i1 = min(NM, i0 + 16)
            P.dma(masks_sb[:, i0:i1, :], masks_in[:, i0:i1, :], writes=[Bmask])
        krt_sb = A.alloc([S], BF16, parts=64)
        Bkrt = Buf("krt")
        P.dma(krt_sb, KRT, writes=[Bkrt])
        NHB = 4
        hb_k = [A.alloc([S], BF16) for _ in range(NHB)]
        hb_v = [A.alloc([32, 128], BF16) for _ in range(NHB)]
        hb_q = [A.alloc([NT], BF16) for _ in range(NHB)]
        hb_qr = [A.alloc([NT], BF16, parts=64) for _ in range(2)]
        Bhk = [Buf("hk%d" % i) for i in range(NHB)]
        Bhv = [Buf("hv%d" % i) for i in range(NHB)]
        Bhq = [Buf("hq%d" % i) for i in range(NHB)]
        Bqr = [Buf("qr0"), Buf("qr1")]
        NPT = 4
        pT = [A.alloc([512], BF16) for _ in range(NPT)]
        BpT = [Buf("pT%d" % i) for i in range(NPT)]
        rden = A.alloc([512], F32)
        Brden = Buf("rden")
        ost = [A.alloc([512], BF16) for _ in range(2)]
        Bost = [Buf("ost0"), Buf("ost1")]
        cnt = {"pt": 0, "s": 0, "o": 0, "hb": 0}

        tiles = []

        def emit_attention():
            LA = 2
            flat = []
            for ti_, (pre, blocks, scale, dst) in enumerate(tiles):
                for bi in range(len(blocks)):
                    flat.append((ti_, bi))
            ptinfo = {}

            def s_stage(f):
                ti_, bi = flat[f]
                pre, blocks, scale, dst = tiles[ti_]
                if bi == 0:
                    for fn in pre:
                        fn()
                qparts, kparts, v_ap, vbufs, mi = blocks[bi]
                sb = f % 4
                npart = len(qparts)
                for pi in range(npart):
                    qa, qrows, qb = qparts[pi]
                    ka, krows, kb = kparts[pi]
                    mm(PS[sb], ka, qa, pi == 0, pi == npart - 1, qb + kb, [PSB[sb]])
                pt, bpt = pT[f % NPT], BpT[f % NPT]
                act(pt, PS[sb], AF.Exp, [PSB[sb]], [bpt], scale=scale)
                if mi is not None:
                    tt("pool" if f % 3 != 2 else "dve", pt, pt, masks_sb[:, mi, :], ALU.mult, [bpt, Bmask], [bpt])

            def pv_stage(f):
                ti_, bi = flat[f]
                pre, blocks, scale, dst = tiles[ti_]
                nb = len(blocks)
                qparts, kparts, v_ap, vbufs, mi = blocks[bi]
                ob, db = 4 + 2 * (ti_ % 2), 5 + 2 * (ti_ % 2)
                pt, bpt = pT[f % NPT], BpT[f % NPT]
                mm(PS[ob], v_ap, pt, bi == 0, bi == nb - 1, vbufs + [bpt], [PSB[ob]])
                mm(PS[db], ones, pt, bi == 0, bi == nb - 1, [bpt, Bc], [PSB[db]])
                if bi == nb - 1:
                    P.add("dve", lambda e: e.reciprocal(out=rden, in_=PS[db]), [PSB[db]], [Brden])
                    o_, bo_ = ost[ti_ % 2], Bost[ti_ % 2]
                    tt("dve", o_, PS[ob], rden, ALU.mult, [PSB[ob], Brden], [bo_])
                    P.dma(dst, o_, reads=[bo_], eng="pool")

            n = len(flat)
            for f in range(min(LA, n)):
                s_stage(f)
            for f in range(n):
                if f + LA < n:
                    s_stage(f + LA)
                pv_stage(f)

        sc_m = 192 ** -0.5

        def mla_loads(h):
            hi = h % NHB
            qi_ = h % 2

            def fn():
                P.dma(hb_k[hi], KT_m[h], writes=[Bhk[hi]])
                P.dma(hb_v[hi], V_m[h], writes=[Bhv[hi]])
                P.dma(hb_q[hi], QT_m[h, 0:128, :], writes=[Bhq[hi]])
                P.dma(hb_qr[qi_], QT_m[h, 128:192, :], writes=[Bqr[qi_]])
            return fn

        def dil_loads(slot_idx, g):
            hi = (8 + slot_idx * 3 + g) % NHB
            h = 4 * g + slot_idx

            def fn():
                P.dma(hb_k[hi], KT_d[h], writes=[Bhk[hi]])
                P.dma(hb_v[hi], V_d[h], writes=[Bhv[hi]])
                P.dma(hb_q[hi], QT_d[h], writes=[Bhq[hi]])
            return fn

        for h in range(8):
            hi = h % NHB
            qi_ = h % 2
            for t in range(4):
                pre = []
                if h == 0 and t == 0:
                    pre = [mla_loads(0), mla_loads(1)]
                elif t == 0:
                    pre = [mla_loads(h + 1)] if h + 1 < 8 else [dil_loads(0, 0)]
                qsl = slice(t * 512, (t + 1) * 512)
                blocks = []
                for kind, base in (("own", 0), ("oth", 1)):
                    for jk in range(4 * t + 4):
                        ksl = slice(base * NT + jk * 128, base * NT + (jk + 1) * 128)
                        mi = midx[("m", kind, jk - 4 * t)] if jk >= 4 * t else None
                        blocks.append((
                            [(hb_q[hi][:, qsl], 128, [Bhq[hi]]), (hb_qr[qi_][:, qsl], 64, [Bqr[qi_]])],
                            [(hb_k[hi][:, ksl], 128, [Bhk[hi]]), (krt_sb[:, ksl], 64, [Bkrt])],
                            hb_v[hi][:, base * 16 + jk, :], [Bhv[hi]], mi))
                tiles.append((pre, blocks, sc_m, OT_m[h, :, qsl]))
        sc_d = 128 ** -0.5
        for s_ in range(4):
            for t in range(4):
                pre = []
                if t == 0:
                    pre = [dil_loads(s_, 1), dil_loads(s_, 2)]
                    if s_ > 0:
                        pre = [dil_loads(s_, 0)] + pre
                qsl = slice(t * 512, (t + 1) * 512)
                blocks = []
                for g in range(3):
                    hi = (8 + s_ * 3 + g) % NHB
                    for kind, base in (("own", 0), ("oth", 1)):
                        for rho in range(-3, 10):
                            jk = 4 * t - rho
                            key = ("d", g, kind, rho)
                            if jk < 0 or jk > 15 or key not in midx:
                                continue
                            ksl = slice(base * NT + jk * 128, base * NT + (jk + 1) * 128)
                            blocks.append((
                                [(hb_q[hi][:, qsl], 128, [Bhq[hi]])],
                                [(hb_k[hi][:, ksl], 128, [Bhk[hi]])],
                                hb_v[hi][:, base * 16 + jk, :], [Bhv[hi]], midx[key]))
                tiles.append((pre, blocks, sc_d, OT_d[s_, :, qsl]))
        emit_attention()
        P.barrier()
        A.release(mC)
        if stop_after == "C":
            return nc, P.emit()

        for c_ in range(NSLOT // 1024):
            P.dma(XGz[c_], zt.unsqueeze(1).to_broadcast([128, 8, D]), reads=[Bz], writes=[Bxgz[c_]], ring="spz")
        mD = A.mark()
        merged = A.alloc([16, NT], BF16)
        Bmg = [Buf("mg%d" % i) for i in range(4)]
        mD1 = A.mark()
        wmo = A.alloc([8, D], BF16)
        wdo = A.alloc([4, D], BF16)
        Bwmo, Bwdo = Buf("wmo"), Buf("wdo")
        P.dma(wmo, w_mla_o.rearrange("(k p) n -> p k n", p=128), writes=[Bwmo], eng="pool")
        P.dma(wdo, w_dil_o.rearrange("(k p) n -> p k n", p=128), writes=[Bwdo], eng="pool")
        otm = [A.alloc([8, 512], BF16) for _ in range(2)]
        otd = [A.alloc([4, 512], BF16) for _ in range(2)]
        Bot = [Buf("ot0"), Buf("ot1")]
        Botd = [Buf("otd0"), Buf("otd1")]
        sga = [A.alloc([512], BF16) for _ in range(3)]
        sgb = [A.alloc([512], BF16) for _ in range(3)]
        Bsgt = [Buf("sg%d" % i) for i in range(3)]
        Bsgtb = [Buf("sgb%d" % i) for i in range(3)]
        t1 = [A.alloc([512], F32) for _ in range(2)]
        t2 = [A.alloc([512], F32) for _ in range(2)]
        Bt12 = [Buf("t12_0"), Buf("t12_1")]
        ci = 0
        for tg in range(4):
            tsl = slice(tg * 512, (tg + 1) * 512)
            o2 = tg % 2
            P.dma(otm[o2], OT_m[:, :, tsl].rearrange("h p t -> p h t"), writes=[Bot[o2]])
            P.dma(otd[o2], OT_d[:, :, tsl].rearrange("h p t -> p h t"), writes=[Botd[o2]])
            for m in range(16):
                s3 = ci % 3
                c2 = ci % 2
                ci += 1
                P.dma(sga[s3], SG[m, :, tsl], writes=[Bsgt[s3]])
                P.dma(sgb[s3], SG[16 + m, :, tsl], writes=[Bsgtb[s3]])
                ba, bb = (0, 1) if c2 == 0 else (2, 3)
                for k in range(8):
                    mm(PS[ba], wmo[:, k, m * 128:(m + 1) * 128], otm[o2][:, k, :], k == 0, k == 7, [Bwmo, Bot[o2]], [PSB[ba]])
                for k in range(4):
                    mm(PS[bb], wdo[:, k, m * 128:(m + 1) * 128], otd[o2][:, k, :], k == 0, k == 3, [Bwdo, Botd[o2]], [PSB[bb]])
                tt("dve", t1[c2], PS[ba], sga[s3], ALU.mult, [PSB[ba], Bsgt[s3]], [Bt12[c2]])
                tt("dve", t2[c2], PS[bb], sgb[s3], ALU.mult, [PSB[bb], Bsgtb[s3]], [Bt12[c2]])
                tt("pool", merged[:, m, tsl], t1[c2], t2[c2], ALU.add, [Bt12[c2]], [Bmg[tg]])
        A.release(mD1)

        P.barrier()
        AB2r = A.alloc([2, D], BF16)
        BAB2 = Buf("AB2r")
        mD2a = A.mark()
        rows2 = A.alloc([3 * D], F32)
        Brows2 = Buf("rows2")
        P.dma(rows2, rowv[:, 4 * D + 64:7 * D + 64].partition_broadcast(128), writes=[Brows2])
        sc2 = A.alloc([16], BF16)
        scB2 = A.alloc([16, 128], BF16)
        Bsc2 = Buf("sc2")
        act(sc2, colv_sb[:, 0:16], AF.Silu, [Bc], [Bsc2])
        for k in range(16):
            cp("dve", scB2[:, k, :], sc2[:, k:k + 1].to_broadcast([128, 128]), [Bsc2], [Bsc2])
        wa2 = [A.alloc([16, 512], BF16) for _ in range(2)]
        Bwa2 = [Buf("wa2_0"), Buf("wa2_1")]
        tmpr = A.alloc([512], F32)
        Btmpr = Buf("tmpr")
        wada_v2 = w_ada.rearrange("(k p) n -> p k n", p=128)
        gi2 = 0
        for which, seg in ((1, 3), (0, 4)):
            for n in range(4):
                wt2, wb2 = wa2[gi2 % 2], Bwa2[gi2 % 2]
                gi2 += 1
                c0 = seg * D + n * 512
                P.dma(wt2, wada_v2[:, :, c0:c0 + 512], writes=[wb2], eng="pool")
                bank = 4 + (n % 2)
                for k in range(16):
                    mm(PS[bank], scB2[:, k, :], wt2[:, k, :], k == 0, k == 15, [wb2, Bsc2], [PSB[bank]])
                nsl = slice(n * 512, (n + 1) * 512)
                if which == 1:
                    tt("dve", AB2r[:, 1, nsl], PS[bank], rows2[:, n * 512:(n + 1) * 512], ALU.add, [PSB[bank], Brows2], [BAB2])
                else:
                    tt("dve", tmpr, PS[bank], rows2[:, D + n * 512:D + (n + 1) * 512], ALU.add, [PSB[bank], Brows2], [Btmpr])
                    stt("dve", AB2r[:, 0, nsl], tmpr, 1.0, rows2[:, 2 * D + n * 512:2 * D + (n + 1) * 512], ALU.add, ALU.mult,
                        [Btmpr, Brows2], [BAB2])
        P.barrier()
        A.release(mD2a)
        wo = A.alloc([16, D], BF16)
        Bwo = Buf("wo")
        P.dma(wo, w_out.rearrange("(k p) n -> p k n", p=128), writes=[Bwo], eng="pool")
        xt2 = [A.alloc([D], F32)] * 2
        Bxt2 = [Buf("xt2_0")] * 2
        yt = [A.alloc([D], F32) for _ in range(2)]
        Byt = [Buf("yt0"), Buf("yt1")]
        h2k = [A.alloc([D], BF16) for _ in range(2)]
        Bh2k = [Buf("h2k0"), Buf("h2k1")]
        junk2 = A.alloc([D], BF16)
        Bjunk2 = Buf("junk2")
        stat2 = A.alloc([16, 12], F32)
        Bst2 = [Buf("st2_%d" % i) for i in range(16)]
        h2t = [A.alloc([16, 128], BF16) for _ in range(2)]
        Bh2t = [Buf("h2t0"), Buf("h2t1")]
        def d2_part1(ti):
            i2 = ti % 2
            tg = ti // 4
            tok = slice(ti * 128, (ti + 1) * 128)
            P.dma(xt2[i2], x_own[tok, :], writes=[Bxt2[i2]])
            for n in range(4):
                for k in range(16):
                    mm(PS[n], merged[:, k, tok], wo[:, k, n * 512:(n + 1) * 512], k == 0, k == 15, [Bmg[tg], Bwo], [PSB[n]])
            for n in range(4):
                act(junk2[:, 0:512], PS[n], AF.Square, [PSB[n]], [Bjunk2, Bst2[ti]], accum_out=stat2[:, ti, n:n + 1])
            P.add("dve", lambda e, ti=ti: e.tensor_reduce(out=stat2[:, ti, 4:5], in_=stat2[:, ti, 0:4], axis=mybir.AxisListType.X, op=ALU.add),
                  [Bst2[ti]], [Bst2[ti]])
            rsqrt_chain(stat2[:, ti, 5:6], stat2[:, ti, 4:5], 1.0 / D, [Bst2[ti]], [Bst2[ti]], stat2[:, ti, 6:7])
            for n in range(4):
                nsl = slice(n * 512, (n + 1) * 512)
                stt("dve", yt[i2][:, nsl], PS[n], stat2[:, ti, 5:6], Gab[:, 0, nsl], ALU.mult, ALU.mult,
                    [PSB[n], Bst2[ti], Bg], [Byt[i2]])
            tt("pool", yt[i2], yt[i2], xt2[i2], ALU.add, [Byt[i2], Bxt2[i2]], [Byt[i2]])
            P.dma(X1[tok, :], yt[i2], reads=[Byt[i2]])
            act(junk2, yt[i2], AF.Square, [Byt[i2]], [Bjunk2, Bst2[ti]], accum_out=stat2[:, ti, 7:8])
            rsqrt_chain(stat2[:, ti, 8:9], stat2[:, ti, 7:8], 1.0 / D, [Bst2[ti]], [Bst2[ti]], stat2[:, ti, 9:10])
            stt("dve", yt[i2], yt[i2], stat2[:, ti, 8:9], AB2r[:, 0, :], ALU.mult, ALU.mult, [Byt[i2], Bst2[ti], BAB2], [Byt[i2]])
            tt("pool", h2k[i2], yt[i2], AB2r[:, 1, :], ALU.add, [Byt[i2], BAB2], [Bh2k[i2]])
            P.dma(H2K[tok, :], h2k[i2], reads=[Bh2k[i2]])

        def d2_part2(ti):
            i2 = ti % 2
            tok = slice(ti * 128, (ti + 1) * 128)
            for half in range(2):
                bank = 6 + half
                for kk in range(8):
                    k = half * 8 + kk
                    P.add("pe", lambda e, bank=bank, kk=kk, k=k, i2=i2: e.transpose(
                        out=PSbf[bank][:, kk * 128:(kk + 1) * 128], in_=h2k[i2][:, k * 128:(k + 1) * 128], identity=ident),
                        [Bh2k[i2], Bc], [PSB[bank]])
                src = PSbf[bank][:, 0:1024].rearrange("p (k n) -> p k n", k=8)
                if half == 0:
                    cp("dve", h2t[i2][:, 0:8, :], src, [PSB[bank]], [Bh2t[i2]])
                else:
                    act(h2t[i2][:, 8:16, :], src, AF.Copy, [PSB[bank]], [Bh2t[i2]])
            P.dma(H2T[:, :, tok], h2t[i2], reads=[Bh2t[i2]])

        d2_part1(0)
        for ti in range(16):
            if ti + 1 < 16:
                d2_part1(ti + 1)
            d2_part2(ti)
        P.barrier()
        A.release(mD)
        if stop_after == "D":
            return nc, P.emit()

        mE = A.mark()
        Wt = A.alloc([16, NE], F32)
        sel_all = A.alloc([16, NE], F32)
        s8_all = A.alloc([16, 8], F32)
        smk = A.alloc([16, NE], BF16)
        destf = A.alloc([16, 8], F32)
        wk = A.alloc([16, 8], F32)
        dest_i = A.alloc([16, 8], I32)
        idxw = A.alloc([NBLK], I32)
        BWt = Buf("Wt")
        Bsel = [Buf("sel%d" % i) for i in range(16)]
        Bsmk = Buf("smk")
        Bdest = [Buf("dest%d" % i) for i in range(16)]
        Bidxw = Buf("idxw")
        mE0 = A.mark()
        wr_sb = A.alloc([16, NE], BF16)
        Bwr = Buf("wr")
        P.dma(wr_sb, w_router.rearrange("(k p) n -> p k n", p=128), writes=[Bwr], eng="pool")
        h2 = [A.alloc([16, 512], BF16) for _ in range(2)]
        Bh2 = [Buf("h2_0"), Buf("h2_1")]
        scs = A.alloc([NE], F32)
        bia = A.alloc([NE], F32)
        top8 = A.alloc([8, 8], F32)
        gsc = A.alloc([8], F32)
        g8 = A.alloc([8], F32)
        gm = A.alloc([8], F32)
        smask = A.alloc([NE], F32)
        wsum = A.alloc([4], F32)
        Brt_ = Buf("route")
        P.add("dve", lambda e: e.memset(destf, 0.0), [], Bdest)
        P.add("dve", lambda e: e.memset(wk, 0.0), [], Bdest)

        def route(ti, h2tile, bh2, col0):
            R = [Brt_]
            sel = sel_all[:, ti, :]
            s8 = s8_all[:, ti, :]
            for k in range(16):
                mm(PS[7][:, 0:NE], h2tile[:, k, col0:col0 + 128], wr_sb[:, k, :], k == 0, k == 15, [bh2, Bwr], [PSB[7]])
            act(scs, PS[7][:, 0:NE], AF.Sigmoid, [PSB[7]], R)
            tt("dve", bia, scs, rbias, ALU.add, R + [Bc], R)
            for g in range(8):
                P.add("dve", lambda e, g=g: e.max(out=top8[:, g, :], in_=bia[:, g * 8:(g + 1) * 8]), R, R)
            tt("dve", gsc, top8[:, :, 0], top8[:, :, 1], ALU.add, R, R)
            P.add("dve", lambda e: e.max(out=g8, in_=gsc), R, R)
            ts("dve", gm, gsc, g8[:, 3:4], 0.0, ALU.is_ge, ALU.add, R, R)
            gmb = gm.unsqueeze(2).to_broadcast([128, 8, 8])
            tt("dve", sel.rearrange("p (g c) -> p g c", g=8), bia.rearrange("p (g c) -> p g c", g=8), gmb, ALU.mult, R, R + [Bsel[ti]])
            ts("dve", gm, gm, -1.0, 4.0, ALU.add, ALU.mult, R, R)
            tt("dve", sel.rearrange("p (g c) -> p g c", g=8), sel.rearrange("p (g c) -> p g c", g=8),
               gm.unsqueeze(2).to_broadcast([128, 8, 8]), ALU.add, R + [Bsel[ti]], R + [Bsel[ti]])
            P.add("dve", lambda e: e.max(out=s8, in_=sel), R + [Bsel[ti]], R + [Bsel[ti]])
            ts("dve", smask, sel, s8[:, 5:6], 0.0, ALU.is_ge, ALU.add, R + [Bsel[ti]], R)
            cp("dve", smk[:, ti, :], smask, R, R + [Bsmk])
            tt("dve", smask, smask, scs, ALU.mult, R, R)
            P.add("dve", lambda e: e.tensor_reduce(out=wsum[:, 0:1], in_=smask, axis=mybir.AxisListType.X, op=ALU.add), R, R)
            ts("dve", wsum[:, 1:2], wsum[:, 0:1], 1e-20, 0.4, ALU.add, ALU.mult, R, R)
            P.add("dve", lambda e: e.reciprocal(out=wsum[:, 2:3], in_=wsum[:, 1:2]), R, R)
            ts("dve", Wt[:, ti, :], smask, wsum[:, 2:3], 0.0, ALU.mult, ALU.add, R, R + [BWt])

        wsg = A.alloc([16, 512], BF16)
        wsu = A.alloc([16, 512], BF16)
        wsd = A.alloc([4, D], BF16)
        Bws = Buf("ws")
        P.dma(wsg, w_sg.rearrange("(k p) n -> p k n", p=128), writes=[Bws], eng="pool")
        P.dma(wsu, w_su.rearrange("(k p) n -> p k n", p=128), writes=[Bws], eng="pool")
        P.dma(wsd, w_sd.rearrange("(k p) n -> p k n", p=128), writes=[Bws], eng="pool")
        sil3 = [A.alloc([512], F32) for _ in range(2)]
        Bsil3 = [Buf("sil3_0"), Buf("sil3_1")]
        aT3 = A.alloc([4, 512], BF16)
        BaT3 = Buf("aT3")
        sho_sb = [A.alloc([D], BF16) for _ in range(2)]
        Bsho = [Buf("sho0"), Buf("sho1")]
        for tg in range(4):
            hsel = tg % 2
            P.dma(h2[hsel], H2T[:, :, tg * 512:(tg + 1) * 512], writes=[Bh2[hsel]])
            for m in range(4):
                bg, bu = (0, 1) if m % 2 == 0 else (2, 3)
                for k in range(16):
                    mm(PS[bg], wsg[:, k, m * 128:(m + 1) * 128], h2[hsel][:, k, :], k == 0, k == 15, [Bws, Bh2[hsel]], [PSB[bg]])
                for k in range(16):
                    mm(PS[bu], wsu[:, k, m * 128:(m + 1) * 128], h2[hsel][:, k, :], k == 0, k == 15, [Bws, Bh2[hsel]], [PSB[bu]])
                s2 = m % 2
                act(sil3[s2], PS[bg], AF.Silu, [PSB[bg]], [Bsil3[s2]])
                tt("pool" if False else "dve", aT3[:, m, :], PS[bu], sil3[s2], ALU.mult, [PSB[bu], Bsil3[s2]], [BaT3])
            for tt_ in range(4):
                tile = tg * 4 + tt_
                i2 = tile % 2
                for n in range(4):
                    bk = 4 + (tt_ * 4 + n) % 3
                    for k in range(4):
                        mm(PS[bk], aT3[:, k, tt_ * 128:(tt_ + 1) * 128], wsd[:, k, n * 512:(n + 1) * 512], k == 0, k == 3,
                           [BaT3, Bws], [PSB[bk]])
                    act(sho_sb[i2][:, n * 512:(n + 1) * 512], PS[bk], AF.Copy, [PSB[bk]], [Bsho[i2]])
                P.dma(SHO[tile * 128:(tile + 1) * 128, :], sho_sb[i2], reads=[Bsho[i2]])
            for tt_ in range(4):
                route(tg * 4 + tt_, h2[hsel], Bh2[hsel], tt_ * 128)
        cnt = A.alloc([NE], F32)
        pcnt = A.alloc([NE], F32)
        ca = A.alloc([NE], F32)
        cb = A.alloc([NE], F32)
        base = A.alloc([NE], F32)
        thr = A.alloc([8], F32)
        cmp1 = A.alloc([NE, 8], F32)
        cmp2 = A.alloc([NBLK, NE], F32)
        blke = A.alloc([NBLK], F32)
        usedm = A.alloc([NBLK], F32)
        Be1 = Buf("e1")
        Bbase = Buf("base")
        E1 = [Be1]
        for ti in range(16):
            mm(PS[6][:, 0:NE], ones, smk[:, ti, :], ti == 0, ti == 15, [Bc, Bsmk], [PSB[6]])
        cp("dve", cnt, PS[6][:, 0:NE], [PSB[6]], E1)
        for j in range(8):
            P.add("dve", lambda e, j=j: e.memset(thr[:, j:j + 1], 256.0 * j), E1, E1)
        tt("dve", cmp1, cnt.unsqueeze(2).to_broadcast([128, NE, 8]), thr.unsqueeze(1).to_broadcast([128, NE, 8]), ALU.is_gt, E1, E1)
        P.add("dve", lambda e: e.tensor_reduce(out=pcnt, in_=cmp1, axis=mybir.AxisListType.X, op=ALU.add), E1, E1)
        cp("dve", ca, pcnt, E1, E1)
        src_, dst_ = ca, cb
        for sft in (1, 2, 4, 8, 16, 32):
            cp("dve", dst_[:, 0:sft], src_[:, 0:sft], E1, E1)
            tt("dve", dst_[:, sft:NE], src_[:, sft:NE], src_[:, 0:NE - sft], ALU.add, E1, E1)
            src_, dst_ = dst_, src_
        pends = src_
        tt("dve", base, pends, pcnt, ALU.subtract, E1, E1 + [Bbase])
        ts("dve", base, base, 256.0, 0.0, ALU.mult, ALU.add, E1 + [Bbase], E1 + [Bbase])
        jgrid = cf[:, 8:8 + NBLK]
        tt("dve", cmp2, pends.unsqueeze(1).to_broadcast([128, NBLK, NE]), jgrid.unsqueeze(2).to_broadcast([128, NBLK, NE]), ALU.is_le, E1 + [Bc], E1)
        P.add("dve", lambda e: e.tensor_reduce(out=blke, in_=cmp2, axis=mybir.AxisListType.X, op=ALU.add), E1, E1)
        ts("dve", blke, blke, 63.0, 128.0, ALU.min, ALU.mult, E1, E1)
        tt("dve", blke, blke, cf[:, 120:121].to_broadcast([128, NBLK]), ALU.add, E1 + [Bc], E1)
        ts("dve", usedm, jgrid, pends[:, NE - 1:NE], 1.0e6, ALU.is_ge, ALU.mult, E1 + [Bc], E1)
        tt("dve", blke, blke, usedm, ALU.add, E1, E1)
        cp("dve", idxw, blke, E1, [Bidxw])
        dfull = A.alloc([NE], F32)
        junk64 = A.alloc([NE], F32)
        h2kt = [A.alloc([D], BF16) for _ in range(2)]
        Bh2kt = [Buf("h2kt0"), Buf("h2kt1")]
        Bdf = Buf("dfull")
        for ti in range(16):
            i2 = ti % 2
            tok = slice(ti * 128, (ti + 1) * 128)
            P.dma(h2kt[i2], H2K[tok, :], writes=[Bh2kt[i2]])
            bank = 4 + ti % 2
            for j in range(ti):
                mm(PS[bank][:, 0:NE], ones, smk[:, j, :], j == 0, False, [Bc, Bsmk], [PSB[bank]])
            mm(PS[bank][:, 0:NE], Utri, smk[:, ti, :], ti == 0, True, [Bc, Bsmk], [PSB[bank]])
            tt("dve", dfull, PS[bank][:, 0:NE], base, ALU.add, [PSB[bank], Bbase], [Bdf])
            for k in range(6):
                P.add("dve", lambda e, ti=ti, k=k: e.scalar_tensor_tensor(
                    out=junk64, in0=sel_all[:, ti, :], scalar=s8_all[:, ti, k:k + 1], in1=dfull, op0=ALU.is_equal, op1=ALU.mult,
                    accum_out=destf[:, ti, k:k + 1]), [Bsel[ti], Bdf], [Bdest[ti], Bdf])
                P.add("dve", lambda e, ti=ti, k=k: e.scalar_tensor_tensor(
                    out=junk64, in0=sel_all[:, ti, :], scalar=s8_all[:, ti, k:k + 1], in1=Wt[:, ti, :], op0=ALU.is_equal, op1=ALU.mult,
                    accum_out=wk[:, ti, k:k + 1]), [Bsel[ti], BWt], [Bdest[ti], Bdf])
            ts("dve", junk64[:, 0:8], destf[:, ti, :], float(NSLOT) - 0.5, 0.0, ALU.is_lt, ALU.add, [Bdest[ti], Bdf], [Bdf])
            tt("dve", wk[:, ti, :], wk[:, ti, :], junk64[:, 0:8], ALU.mult, [Bdest[ti], Bdf], [Bdest[ti]])
            cp("dve", dest_i[:, ti, :], destf[:, ti, :], [Bdest[ti]], [Bdest[ti]])
            for k in range(6):
                P.add("pool", lambda e, ti=ti, k=k, i2=i2: e.indirect_dma_start(
                    out=XG[:, :], out_offset=bass.IndirectOffsetOnAxis(ap=dest_i[:, ti, k:k + 1], axis=0),
                    in_=h2kt[i2], in_offset=None, bounds_check=P.regs[NSLOT - 1], oob_is_err=False),
                    [Bdest[ti], Bh2kt[i2]] + (Bxgz if (ti == 0 and k == 0) else []), [], dma=True)
        P.barrier()
        A.release(mE0)
        if stop_after == "E2":
            return nc, P.emit()

        mE4 = A.mark()
        wg_t = [A.alloc([16, 512], BF16) for _ in range(2)]
        wu_t = [A.alloc([16, 512], BF16) for _ in range(2)]
        wd_t = [A.alloc([4, D], BF16) for _ in range(2)]
        Bweg = [Buf("weg0"), Buf("weg1")]
        Bweu = [Buf("weu0"), Buf("weu1")]
        Bwed = [Buf("wed0"), Buf("wed1")]
        xg_sb = [A.alloc([2, D], BF16) for _ in range(2)]
        Bxg = [Buf("xg0"), Buf("xg1")]
        xgT = [A.alloc([16, 256], BF16) for _ in range(2)]
        BxgT = [Buf("xgT0"), Buf("xgT1")]
        sil = [A.alloc([256], F32) for _ in range(2)]
        Bsil = [Buf("sil0"), Buf("sil1")]
        aT = [A.alloc([4, 256], BF16) for _ in range(2)]
        BaT = [Buf("aT0"), Buf("aT1")]
        yg_sb = [A.alloc([2, D], BF16) for _ in range(2)]
        Byg = [Buf("yg0"), Buf("yg1")]
        XGv = XG.rearrange("(j s p) d -> j p s d", s=2, p=128)
        YGv = YG.rearrange("(j s p) d -> j p s d", s=2, p=128)
        evs = {"c": 0}

        def e4_load(j):
            w2 = j % 2
            for (dst, src, bw) in ((wg_t[w2], w_eg, Bweg[w2]), (wu_t[w2], w_eu, Bweu[w2]), (wd_t[w2], w_ed, Bwed[w2])):
                P.add("pool", lambda e, dst=dst, src=src, j=j: e.indirect_dma_start(
                    out=dst.rearrange("p k n -> p (k n)"), out_offset=None, in_=src[:, :], in_offset=bass.IndirectOffsetOnAxis(ap=idxw[:, j:j + 1], axis=0),
                    bounds_check=P.regs[NE * 128 - 1], oob_is_err=False), [Bidxw], [bw], dma=True)
            P.dma(xg_sb[w2], XGv[j], writes=[Bxg[w2]])

        def e4_T(j):
            w2 = j % 2
            for s_ in range(2):
                for half in range(2):
                    bank = 6 + (s_ * 2 + half) % 2
                    for kk in range(8):
                        k = half * 8 + kk
                        P.add("pe", lambda e, bank=bank, kk=kk, k=k, w2=w2, s_=s_: e.transpose(
                            out=PSbf[bank][:, kk * 128:(kk + 1) * 128], in_=xg_sb[w2][:, s_, k * 128:(k + 1) * 128], identity=ident),
                            [Bxg[w2], Bc], [PSB[bank]])
                    src_ap = PSbf[bank][:, 0:1024].rearrange("p (k n) -> p k n", k=8)
                    dst_ap = xgT[w2][:, half * 8:(half + 1) * 8, s_ * 128:(s_ + 1) * 128]
                    if evs["c"] % 2 == 0:
                        cp("dve", dst_ap, src_ap, [PSB[bank]], [BxgT[w2]])
                    else:
                        act(dst_ap, src_ap, AF.Copy, [PSB[bank]], [BxgT[w2]])
                    evs["c"] += 1

        def e4_GU(j):
            w2 = j % 2
            for m in range(4):
                bg, bu = (0, 1) if m % 2 == 0 else (2, 3)
                for k in range(16):
                    mm(PS[bg][:, 0:256], wg_t[w2][:, k, m * 128:(m + 1) * 128], xgT[w2][:, k, :], k == 0, k == 15, [Bweg[w2], BxgT[w2]], [PSB[bg]])
                for k in range(16):
                    mm(PS[bu][:, 0:256], wu_t[w2][:, k, m * 128:(m + 1) * 128], xgT[w2][:, k, :], k == 0, k == 15, [Bweu[w2], BxgT[w2]], [PSB[bu]])
                s2 = m % 2
                act(sil[s2], PS[bg][:, 0:256], AF.Silu, [PSB[bg]], [Bsil[s2]])
                tt("dve", aT[w2][:, m, :], PS[bu][:, 0:256], sil[s2], ALU.mult, [PSB[bu], Bsil[s2]], [BaT[w2]])

        def e4_D(j):
            w2 = j % 2
            for s_ in range(2):
                for n in range(4):
                    bk = 4 + (s_ * 4 + n) % 2
                    for k in range(4):
                        mm(PS[bk], aT[w2][:, k, s_ * 128:(s_ + 1) * 128], wd_t[w2][:, k, n * 512:(n + 1) * 512], k == 0, k == 3,
                           [BaT[w2], Bwed[w2]], [PSB[bk]])
                    dst_ap = yg_sb[w2][:, s_, n * 512:(n + 1) * 512]
                    if n % 2 == 0:
                        cp("dve", dst_ap, PS[bk], [PSB[bk]], [Byg[w2]])
                    else:
                        act(dst_ap, PS[bk], AF.Copy, [PSB[bk]], [Byg[w2]])
            P.dma(YGv[j], yg_sb[w2], reads=[Byg[w2]])

        e4_load(0)
        e4_load(1)
        e4_T(0)
        e4_GU(0)
        for j in range(NBLK):
            if j + 1 < NBLK:
                e4_T(j + 1)
            e4_D(j)
            if j + 2 < NBLK:
                e4_load(j + 2)
            if j + 1 < NBLK:
                e4_GU(j + 1)
        P.barrier()
        A.release(mE4)

        acc = A.alloc([4, D], F32)
        Bacc = [Buf("acc%d" % i) for i in range(4)]
        sho_t = [A.alloc([D], BF16) for _ in range(2)]
        Bsho_t = [Buf("shot0"), Buf("shot1")]
        NYK = 12
        yk = [A.alloc([D], BF16) for _ in range(NYK)]
        Byk = [Buf("yk%d" % i) for i in range(NYK)]
        x1t = [A.alloc([D], F32) for _ in range(2)]
        Bx1 = [Buf("x1_0"), Buf("x1_1")]
        fo = [A.alloc([D], F32) for _ in range(2)]
        Bfo = [Buf("fo0"), Buf("fo1")]
        junk3 = A.alloc([D], BF16)
        Bjunk3 = Buf("junk3")
        stat3 = A.alloc([16, 4], F32)
        Bst3 = [Buf("st3_%d" % i) for i in range(16)]
        def e3_gather(tile):
            i2 = tile % 2
            tok = slice(tile * 128, (tile + 1) * 128)
            P.dma(x1t[i2], X1[tok, :], writes=[Bx1[i2]])
            P.dma(sho_t[i2], SHO[tok, :], writes=[Bsho_t[i2]])
            for k in range(6):
                y3 = (tile * 6 + k) % NYK
                P.add("pool", lambda e, tile=tile, k=k, y3=y3: e.indirect_dma_start(
                    out=yk[y3], out_offset=None, in_=YG[:, :], in_offset=bass.IndirectOffsetOnAxis(ap=dest_i[:, tile, k:k + 1], axis=0),
                    bounds_check=P.regs[NSLOT - 1], oob_is_err=False), [Bdest[tile]], [Byk[y3]], dma=True)

        def e3_combine(tile):
            tt_ = tile % 4
            i2 = tile % 2
            tok = slice(tile * 128, (tile + 1) * 128)
            for k in range(6):
                y3 = (tile * 6 + k) % NYK
                if k == 0:
                    stt("dve", acc[:, tt_, :], yk[y3], wk[:, tile, k:k + 1], sho_t[i2], ALU.mult, ALU.add,
                        [Byk[y3], Bdest[tile], Bsho_t[i2]], [Bacc[tt_]])
                else:
                    stt("dve", acc[:, tt_, :], yk[y3], wk[:, tile, k:k + 1], acc[:, tt_, :], ALU.mult, ALU.add,
                        [Byk[y3], Bdest[tile], Bacc[tt_]], [Bacc[tt_]])
            act(junk3, acc[:, tt_, :], AF.Square, [Bacc[tt_]], [Bjunk3, Bst3[tile]], accum_out=stat3[:, tile, 0:1])
            rsqrt_chain(stat3[:, tile, 1:2], stat3[:, tile, 0:1], 1.0 / D, [Bst3[tile]], [Bst3[tile]], stat3[:, tile, 2:3])
            stt("dve", fo[i2], acc[:, tt_, :], stat3[:, tile, 1:2], Gab[:, 1, :], ALU.mult, ALU.mult,
                [Bacc[tt_], Bst3[tile], Bg], [Bfo[i2]])
            tt("pool", fo[i2], fo[i2], x1t[i2], ALU.add, [Bfo[i2], Bx1[i2]], [Bfo[i2]])
            P.dma(out_d[tok, :], fo[i2], reads=[Bfo[i2]])

        e3_gather(0)
        for tile in range(16):
            if tile + 1 < 16:
                e3_gather(tile + 1)
            e3_combine(tile)
        A.release(mE)
        stats = P.emit()
    return nc, stats


def _consts():
    cb = np.zeros((128, 640), np.float32)
    cb[:, 512:640] = np.triu(np.ones((128, 128), np.float32), 1)
    cb[:, 0:128] = np.eye(128)
    cb[:, 128:256] = 1.0
    for m in range(64):
        cb[(m + 32) % 64, 256 + m] = 1.0
    for m in range(32):
        cb[(m + 16) % 32, 320 + m] = 1.0
    cf = np.zeros((128, 128), np.float32)
    cf[:, 8:120] = np.arange(112, dtype=np.float32)[None, :]
    cf[:, 120] = np.arange(128, dtype=np.float32)
    fm = (1.0 / (500000.0 ** (np.arange(0, 64, 2, dtype=np.float32) / 64))).astype(np.float32)
    fd = (1.0 / (500000.0 ** (np.arange(0, 32, 2, dtype=np.float32) / 32))).astype(np.float32)
    cf[0:32, 0] = fm
    cf[32:64, 0] = fm
    cf[0:32, 1] = -1.0
    cf[32:64, 1] = 1.0
    cf[0:16, 2] = fd
    cf[16:32, 2] = fd
    cf[0:16, 3] = -1.0
    cf[16:32, 3] = 1.0
    return cb.astype(ml_dtypes.bfloat16), cf


def _elayout(w):
    e, r, n = w.shape
    return np.ascontiguousarray(w.reshape(e, r // 128, 128, n).transpose(0, 2, 1, 3)).reshape(e * 128, (r // 128) * n)


def make_in_maps(x, c, positions, w_ada, b_ada, attn_pre_g, w_in, q_a_norm_g, w_q_up, kv_a_norm_g,
                 w_kv_up, w_mla_o, w_dil_o, w_out, attn_post_g, ffn_pre_g, w_router, router_bias,
                 w_exp_gate, w_exp_up, w_exp_down, w_sh_gate, w_sh_up, w_sh_down, ffn_post_g):
    f = lambda a: np.ascontiguousarray(np.asarray(a, dtype=np.float32))
    x = f(x)
    c = f(c)
    positions = np.asarray(positions).astype(np.int32)
    cb, cf = _consts()
    shared = {
        "cst_bf": cb, "cst_f": cf,
        "w_ada": f(w_ada)[0], "w_in": f(w_in)[0], "w_q_up": f(w_q_up)[0], "w_kv_up": f(w_kv_up)[0],
        "w_mla_o": f(w_mla_o)[0], "w_dil_o": f(w_dil_o)[0], "w_out": f(w_out)[0], "w_router": f(w_router)[0],
        "w_exp_gate": _elayout(f(w_exp_gate)[0]), "w_exp_up": _elayout(f(w_exp_up)[0]), "w_exp_down": _elayout(f(w_exp_down)[0]),
        "w_sh_gate": f(w_sh_gate)[0], "w_sh_up": f(w_sh_up)[0], "w_sh_down": f(w_sh_down)[0],
    }
    b_ada = f(b_ada)[0]

    def colform(v):
        return np.ascontiguousarray(v.reshape(-1, 128).T)

    rowv = np.concatenate([b_ada[2 * D:3 * D], b_ada[5 * D:6 * D], f(attn_post_g)[0], f(ffn_post_g)[0], f(router_bias)[0],
                           b_ada[3 * D:4 * D], b_ada[4 * D:5 * D], f(ffn_pre_g)[0]])[None, :]
    mask_cache = {}
    in_maps = []
    for core in range(8):
        b, p = core // 2, core % 2
        xb = x[b].reshape(32, 128, D)
        pb = positions[b].reshape(32, 128)
        colv = np.zeros((128, 128), np.float32)
        colv[:, 0:16] = colform(c[b])
        colv[:, 16:32] = colform(b_ada[0:D])
        colv[:, 32:48] = colform(b_ada[D:2 * D])
        colv[:, 48:64] = colform(b_ada[3 * D:4 * D])
        colv[:, 64:80] = colform(b_ada[4 * D:5 * D])
        colv[:, 80:96] = colform(f(attn_pre_g)[0])
        colv[:, 96:112] = colform(f(ffn_pre_g)[0])
        colv[:, 112:116] = colform(f(q_a_norm_g)[0])
        colv[:, 116:120] = colform(f(kv_a_norm_g)[0])
        if p not in mask_cache:
            m, _ = mask_table(p)
            mask_cache[p] = np.ascontiguousarray(m.transpose(1, 0, 2)).astype(ml_dtypes.bfloat16)
        d = dict(shared)
        d.update({
            "x_own": np.ascontiguousarray(xb[p::2].reshape(NT, D)),
            "x_oth": np.ascontiguousarray(xb[1 - p::2].reshape(NT, D)),
            "pos": np.ascontiguousarray(np.stack([pb[p::2].reshape(NT), pb[1 - p::2].reshape(NT)])),
            "colv": colv, "rowv": np.ascontiguousarray(rowv), "masks": mask_cache[p],
        })
        in_maps.append(d)
    return in_maps


_NC = None


def kernel(**inputs):
    global _NC
    if _NC is None:
        _NC = build()[0]
    in_maps = make_in_maps(**inputs)
    res = run_bass_kernel_spmd(_NC, in_maps, core_ids=list(range(8)))
    out = np.zeros((4, 32, 128, D), np.float32)
    for core in range(8):
        b, p = core // 2, core % 2
        out[b, p::2] = np.asarray(res.results[core]["out"], dtype=np.float32).reshape(16, 128, D)
    return out.reshape(4, S, D)
```

```python
import math
import numpy as np
import ml_dtypes
from contextlib import ExitStack
import concourse.bass as bass
import concourse.mybir as mybir
from concourse.bass_utils import run_bass_kernel_spmd

F32 = mybir.dt.float32
BF16 = mybir.dt.bfloat16
I32 = mybir.dt.int32
U8 = mybir.dt.uint8
ALU = mybir.AluOpType
AF = mybir.ActivationFunctionType

SAME_ENGINE_SYNC = True

D = 2048
S = 4096
NT = 2048
NE = 64
IN_COLS = 9792
C_QA, C_CKV, C_KR, C_DQ, C_DK, C_DV, C_GA, C_GB = 0, 512, 1024, 1088, 2624, 4160, 5696, 7744
EPS = 1e-6
PI = math.pi
DIL = ((128, 1), (512, 4), (2048, 16))


class Buf:
    __slots__ = ("name", "w", "r", "excl")

    def __init__(self, name="", excl=False):
        self.name = name
        self.w = None
        self.r = {}
        self.excl = excl


class Op:
    __slots__ = ("eng", "fn", "deps", "signal", "semval", "sem", "is_dma", "guard", "idx")

    def __init__(self, eng, fn, is_dma):
        self.eng = eng
        self.fn = fn
        self.deps = []
        self.signal = False
        self.semval = None
        self.sem = None
        self.is_dma = is_dma
        self.guard = None


class Prog:
    ENGS = ("pe", "act", "dve", "pool", "sp")

    def __init__(self, nc, stack):
        self.nc = nc
        self.ops = {e: [] for e in self.ENGS}
        self.last = {e: None for e in self.ENGS}
        self.barrier_deps = []
        self.dma_since_barrier = []
        self.reg_requests = []
        self.regs = {}
        n_dma_sems = {"sp": 24, "pool": 12, "act": 4}
        self.csem = {}
        for e in ("pe", "act", "dve", "pool"):
            self.csem[e] = stack.enter_context(nc.semaphore("c_" + e))
        self.dsem, self.dsem_last, self.dsem_cnt, self.dsem_rr = {}, {}, {}, {}
        for e, n in n_dma_sems.items():
            self.dsem[e] = [stack.enter_context(nc.semaphore("d_%s%d" % (e, i))) for i in range(n)]
            self.dsem_last[e] = [None] * n
            self.dsem_cnt[e] = [0] * n
            self.dsem_rr[e] = 0

    def add(self, eng, fn, reads=(), writes=(), dma=False):
        op = Op(eng, fn, dma)
        op.idx = len(self.ops[eng])
        best = {}
        dmas = {}
        ex = [b for b in reads if b.excl]
        if ex:
            reads = [b for b in reads if not b.excl]
            writes = list(writes) + [b for b in ex if b not in writes]

        def adddep(d):
            if d is None:
                return
            if d.is_dma:
                dmas[id(d)] = d
            else:
                b = best.get(d.eng)
                if b is None or d.idx > b.idx:
                    best[d.eng] = d

        for b in reads:
            adddep(b.w)
        for b in writes:
            adddep(b.w)
            for r in b.r.values():
                adddep(r)
        for d in self.barrier_deps:
            adddep(d)
        fdeps = []
        for d in list(best.values()) + list(dmas.values()):
            if not d.is_dma and d.eng == eng and not dma:
                if eng == "pe" or not SAME_ENGINE_SYNC:
                    continue
            fdeps.append(d)
            d.signal = True
        op.deps = fdeps
        for b in reads:
            b.r[("d", id(op)) if dma else eng] = op
        for b in writes:
            b.w = op
            b.r = {}
        if dma:
            i = self.dsem_rr[eng]
            n = len(self.dsem[eng])
            self.dsem_rr[eng] = (i + 1) % n
            op.sem = self.dsem[eng][i]
            op.guard = self.dsem_last[eng][i]
            self.dsem_cnt[eng][i] += 1
            op.semval = 16 * self.dsem_cnt[eng][i]
            self.dsem_last[eng][i] = op
            op.signal = True
            self.dma_since_barrier.append(op)
        else:
            self.last[eng] = op
        self.ops[eng].append(op)
        return op

    def barrier(self):
        deps = [o for o in self.last.values() if o is not None] + list(self.dma_since_barrier)
        for e in self.dsem:
            for o in self.dsem_last[e]:
                if o is not None and o not in deps:
                    deps.append(o)
        self.barrier_deps = deps
        self.dma_since_barrier = []

    def dma(self, out, in_, reads=(), writes=(), eng="sp"):
        return self.add(eng, lambda e: e.dma_start(out=out, in_=in_), reads, writes, dma=True)

    def emit(self):
        nc = self.nc
        final_deps = [o for o in self.last.values() if o is not None]
        for e in self.dsem:
            for o in self.dsem_last[e]:
                if o is not None:
                    final_deps.append(o)
        for d in final_deps:
            d.signal = True
        for e in ("pe", "act", "dve", "pool"):
            cnt = 0
            for op in self.ops[e]:
                if op.is_dma:
                    continue
                if op.signal:
                    cnt += 1
                    op.semval = cnt
                    op.sem = self.csem[e]
        stats = {}

        def run_engine(ename, eh, final=False):
            waited = {}
            nw = 0

            def wait_for(d):
                nonlocal nw
                key = id(d.sem)
                if waited.get(key, 0) >= d.semval:
                    return
                waited[key] = d.semval
                eh.wait_ge(d.sem, d.semval)
                nw += 1

            if ename == "pool":
                for v in self.reg_requests:
                    self.regs[v] = eh.to_reg(v)
            for op in self.ops[ename]:
                for d in op.deps:
                    wait_for(d)
                if op.guard is not None:
                    wait_for(op.guard)
                ins = op.fn(eh)
                if op.signal:
                    ins.then_inc(op.sem, 16 if op.is_dma else 1)
            if final:
                for d in final_deps:
                    wait_for(d)
            stats[ename] = (len(self.ops[ename]), nw)

        with nc.Block() as block:
            @block.tensor
            def _(t):
                run_engine("pe", t)

            @block.scalar
            def _(s):
                run_engine("act", s)

            @block.vector
            def _(v):
                run_engine("dve", v)

            @block.gpsimd
            def _(g):
                run_engine("pool", g)

            @block.sync
            def _(s):
                run_engine("sp", s, final=True)
        return stats


class StopBuild(Exception):
    pass


class Arena:
    def __init__(self, ap, size):
        self.ap = ap
        self.size = size
        self.off = 0

    def alloc(self, free_shape, dtype, parts=128):
        esz = {F32: 4, BF16: 2, I32: 4, U8: 1}[dtype]
        n = esz
        for s in free_shape:
            n *= s
        off = (self.off + 63) // 64 * 64
        assert off + n <= self.size, ("arena overflow", off, n, self.size)
        self.off = off + n
        a = self.ap[0:parts, off:off + n].bitcast(dtype)
        if len(free_shape) == 2:
            a = a.rearrange("p (a b) -> p a b", a=free_shape[0])
        elif len(free_shape) == 3:
            a = a.rearrange("p (a b c) -> p a b c", a=free_shape[0], b=free_shape[1])
        return a

    def mark(self):
        return self.off

    def release(self, m):
        self.off = m


def mask_table(p):
    masks = []
    idx = {}
    ki = np.arange(128)[:, None]
    qi = np.arange(128)[None, :]

    def add(key, fn):
        m = np.zeros((128, 512), np.float32)
        for a in range(4):
            m[:, a * 128:(a + 1) * 128] = fn(a)
        idx[key] = len(masks)
        masks.append(m)

    for c in range(4):
        add(("m", "own", c), lambda a, c=c: (np.ones((128, 128)) if a > c else ((ki <= qi) if a == c else np.zeros((128, 128)))))
        add(("m", "oth", c), lambda a, c=c: (np.ones((128, 128)) if a > c else (np.full((128, 128), float(p)) if a == c else np.zeros((128, 128)))))
    def dmask(pp, kind, rho, w, d):
        off = 0 if kind == "own" else 128 * (2 * pp - 1)
        ms = []
        for a in range(4):
            delta = 256 * (a + rho) + off + qi - ki
            ms.append(((delta >= 0) & (delta <= w) & (delta % d == 0)).astype(np.float32))
        return np.concatenate(ms, axis=1)

    for g, (w, d) in enumerate(DIL):
        for kind in ("own", "oth"):
            for rho in range(-3, 10):
                m0, m1 = dmask(0, kind, rho, w, d), dmask(1, kind, rho, w, d)
                if m0.any() or m1.any():
                    idx[("d", g, kind, rho)] = len(masks)
                    masks.append(m1 if p == 1 else m0)
    return np.stack(masks), idx


def build(debug=False, stop_after=None):
    nc = bass.Bass("TRN2", target_bir_lowering=False)
    _, midx = mask_table(0)
    _, midx1 = mask_table(1)
    assert midx == midx1
    NM = len(midx)

    def din(name, shape, dt=F32):
        return nc.dram_tensor(name, list(shape), dt, kind="ExternalInput").ap()

    def dscr(name, shape, dt):
        return nc.dram_tensor(name, list(shape), dt, kind=("ExternalOutput" if debug else "Internal")).ap()

    x_own = din("x_own", [NT, D])
    x_oth = din("x_oth", [NT, D])
    pos_in = din("pos", [2, NT], I32)
    colv = din("colv", [128, 128])
    rowv = din("rowv", [1, 7 * D + 64])
    cst_bf = din("cst_bf", [128, 640], BF16)
    cst_f = din("cst_f", [128, 128])
    masks_in = din("masks", [128, NM, 512], BF16)
    w_ada = din("w_ada", [D, 6 * D])
    w_in = din("w_in", [D, IN_COLS])
    w_q_up = din("w_q_up", [512, 1536])
    w_kv_up = din("w_kv_up", [512, 2048])
    w_mla_o = din("w_mla_o", [1024, D])
    w_dil_o = din("w_dil_o", [512, D])
    w_out = din("w_out", [D, D])
    w_router = din("w_router", [D, NE])
    w_eg = din("w_exp_gate", [NE * 128, 8192])
    w_eu = din("w_exp_up", [NE * 128, 8192])
    w_ed = din("w_exp_down", [NE * 128, 8192])
    w_sg = din("w_sh_gate", [D, 512])
    w_su = din("w_sh_up", [D, 512])
    w_sd = din("w_sh_down", [512, D])
    out_d = nc.dram_tensor("out", [NT, D], F32, kind="ExternalOutput").ap()

    QT_m = dscr("QT_m", [8, 192, NT], BF16)
    KT_m = dscr("KT_m", [8, 128, S], BF16)
    KRT = dscr("KRT", [64, S], BF16)
    V_m = dscr("V_m", [8, 128, 32, 128], BF16)
    QT_d = dscr("QT_d", [12, 128, NT], BF16)
    KT_d = dscr("KT_d", [12, 128, S], BF16)
    V_d = dscr("V_d", [12, 128, 32, 128], BF16)
    SG = dscr("SG", [32, 128, NT], BF16)
    OT_m = dscr("OT_m", [8, 128, NT], BF16)
    OT_d = dscr("OT_d", [4, 128, NT], BF16)
    X1 = dscr("X1", [NT, D], F32)
    H2T = dscr("H2T", [128, 16, NT], BF16)
    H2K = dscr("H2K", [NT, D], BF16)
    SHO = dscr("SHO", [NT, D], BF16)
    NBLK = 96
    NSLOT = NBLK * 256
    XG = dscr("XG", [NSLOT, D], BF16)
    YG = dscr("YG", [NSLOT, D], BF16)

    st = ExitStack()
    with st:
        P = Prog(nc, st)
        P.reg_requests = [96 * 256 - 1, NE * 128 - 1]
        ASZ = 206 * 1024
        arena_t = st.enter_context(nc.sbuf_tensor("arena", [128, ASZ], U8))
        A = Arena(arena_t[:, :], ASZ)
        PS = [st.enter_context(nc.psum_tensor("ps%d" % i, [128, 512], F32))[:] for i in range(8)]
        PSB = [Buf("ps%d" % i, excl=True) for i in range(8)]
        PSbf = [p.bitcast(BF16) for p in PS]

        cbf = A.alloc([640], BF16)
        Utri = cbf[:, 512:640]
        ident = cbf[:, 0:128]
        ones = cbf[:, 128:256]
        perm64 = cbf[0:64, 256:320]
        perm32 = cbf[0:32, 320:352]
        cf = A.alloc([128], F32)
        colv_sb = A.alloc([128], F32)
        rbias = A.alloc([NE], F32)
        AB = A.alloc([4, 16], F32)
        Gab = A.alloc([2, D], F32)
        Bc = Buf("consts")
        Bab = Buf("AB")
        Bg = Buf("G")
        P.dma(cbf, cst_bf, writes=[Bc])
        P.dma(cf, cst_f, writes=[Bc])
        P.dma(colv_sb, colv, writes=[Bc])
        P.dma(rbias, rowv[:, 4 * D:4 * D + NE].partition_broadcast(128), writes=[Bc])
        qg = colv_sb[:, 112:116]
        kvg = colv_sb[:, 116:120]

        def act(out, in_, func, reads, writes, **kw):
            return P.add("act", lambda e: e.activation(out=out, in_=in_, func=func, **kw), reads, writes)

        def tt(eng, out, in0, in1, op, reads, writes):
            return P.add(eng, lambda e: e.tensor_tensor(out=out, in0=in0, in1=in1, op=op), reads, writes)

        def ts(eng, out, in0, s1, s2, op0, op1, reads, writes):
            return P.add(eng, lambda e: e.tensor_scalar(out=out, in0=in0, scalar1=s1, scalar2=s2, op0=op0, op1=op1), reads, writes)

        def stt(eng, out, in0, scalar, in1, op0, op1, reads, writes):
            return P.add(eng, lambda e: e.scalar_tensor_tensor(out=out, in0=in0, scalar=scalar, in1=in1, op0=op0, op1=op1), reads, writes)

        def cp(eng, out, in_, reads, writes):
            return P.add(eng, lambda e: e.tensor_copy(out=out, in_=in_), reads, writes)

        def mm(out, lhsT, rhs, start, stop, reads, writes):
            return P.add("pe", lambda e: e.matmul(out, lhsT=lhsT, rhs=rhs, start=start, stop=stop), reads, writes)

        def rsqrt_chain(dst, src, scale, reads, writes, tmp):
            ts("dve", tmp, src, scale, EPS, ALU.mult, ALU.add, reads, writes)
            act(tmp, tmp, AF.Sqrt, writes, writes)
            P.add("dve", lambda e: e.reciprocal(out=dst, in_=tmp), writes, writes)

        mA = A.mark()
        zt = A.alloc([8192], BF16)
        Bz = Buf("zero")
        P.add("pool", lambda e: e.memset(zt, 0.0), [], [Bz])
        XGz = XG.rearrange("(c p r) d -> c p (r d)", p=128, r=4)
        for c_ in range(NSLOT // 512):
            P.dma(XGz[c_], zt, reads=[Bz])
        rows_sb = A.alloc([4 * D], F32)
        Brows = Buf("rows")
        P.dma(rows_sb, rowv[:, 0:4 * D].partition_broadcast(128), writes=[Brows])
        sc = A.alloc([16], BF16)
        scB = A.alloc([16, 128], BF16)
        Bsc = Buf("sc")
        act(sc, colv_sb[:, 0:16], AF.Silu, [Bc], [Bsc])
        for k in range(16):
            cp("dve", scB[:, k, :], sc[:, k:k + 1].to_broadcast([128, 128]), [Bsc], [Bsc])
        wada_v = w_ada.rearrange("(k p) n -> p k n", p=128)
        wa_t = [A.alloc([16, 512], BF16) for _ in range(2)]
        wa_b = [Buf("wa0"), Buf("wa1")]
        modT = A.alloc([64], F32)
        col_segs = [0, 1]
        gi = 0
        for si, seg in enumerate(col_segs):
            for n in range(4):
                wt, wb = wa_t[gi % 2], wa_b[gi % 2]
                gi += 1
                c0 = seg * D + n * 512
                P.dma(wt, wada_v[:, :, c0:c0 + 512], writes=[wb], eng="pool")
                for m in range(4):
                    col = si * 16 + n * 4 + m
                    for k in range(16):
                        mm(PS[0][:, col:col + 1], wt[:, k, m * 128:(m + 1) * 128], sc[:, k:k + 1], k == 0, k == 15,
                           [wb, Bsc], [PSB[0]])
        cp("dve", modT[:, 0:32], PS[0][:, 0:32], [PSB[0]], [Bab])
        tt("dve", modT[:, 0:32], modT[:, 0:32], colv_sb[:, 16:48], ALU.add, [Bab, Bc], [Bab])
        stt("dve", AB[:, 0, :], modT[:, 16:32], 1.0, colv_sb[:, 80:96], ALU.add, ALU.mult, [Bab, Bc], [Bab])
        cp("dve", AB[:, 1, :], modT[:, 0:16], [Bab], [Bab])
        for gsel, seg in enumerate([2, 5]):
            for n in range(4):
                wt, wb = wa_t[gi % 2], wa_b[gi % 2]
                gi += 1
                c0 = seg * D + n * 512
                P.dma(wt, wada_v[:, :, c0:c0 + 512], writes=[wb], eng="pool")
                bank = 1 + (n % 2)
                for k in range(16):
                    mm(PS[bank], scB[:, k, :], wt[:, k, :], k == 0, k == 15, [wb, Bsc], [PSB[bank]])
                dst = Gab[:, gsel, n * 512:(n + 1) * 512]
                tt("dve", dst, PS[bank], rows_sb[:, gsel * D + n * 512: gsel * D + (n + 1) * 512], ALU.add, [PSB[bank], Brows], [Bg])
                tt("dve", dst, dst, rows_sb[:, (2 + gsel) * D + n * 512:(2 + gsel) * D + (n + 1) * 512], ALU.mult, [Bg, Brows], [Bg])
        P.barrier()
        A.release(mA)
        if debug:
            dAB = nc.dram_tensor("dbgAB", [128, 64], F32, kind="ExternalOutput").ap()
            dG = nc.dram_tensor("dbgG", [128, 2 * D], F32, kind="ExternalOutput").ap()
            P.dma(dAB, AB, reads=[Bab])
            P.dma(dG, Gab, reads=[Bg])
        if stop_after == "A":
            return nc, P.emit()

        cosM = A.alloc([NT], BF16, parts=64)
        sinM = A.alloc([NT], BF16, parts=64)
        cosD = A.alloc([NT], BF16, parts=32)
        sinD = A.alloc([NT], BF16, parts=32)
        Brope = Buf("rope")

        hT = A.alloc([16, NT], BF16)
        BhT = [Buf("hT%d" % i) for i in range(16)]
        wq_sb = A.alloc([4, 1536], BF16)
        wkv_sb = A.alloc([4, 2048], BF16)
        Bwq, Bwkv = Buf("wq"), Buf("wkv")
        P.dma(wq_sb, w_q_up.rearrange("(k p) n -> p k n", p=128), writes=[Bwq], eng="pool")
        P.dma(wkv_sb, w_kv_up.rearrange("(k p) n -> p k n", p=128), writes=[Bwkv], eng="pool")
        stat = A.alloc([32, 4], F32)
        Bstat = [Buf("stat%d" % i) for i in range(32)]
        mBov = A.mark()
        xt = [A.alloc([D], F32) for _ in range(2)]
        Bxt = [Buf("xt0"), Buf("xt1")]
        xn = [A.alloc([D], BF16) for _ in range(2)]
        Bxn = [Buf("xn0"), Buf("xn1")]
        junk = A.alloc([D], BF16)
        Bjunk = Buf("junk")
        evt = [A.alloc([8, 128], F32) for _ in range(2)]
        Bevt = [Buf("evt0"), Buf("evt1")]
        posi = A.alloc([NT], I32, parts=64)
        posf = A.alloc([NT], F32, parts=64)
        ang = A.alloc([NT], F32, parts=64)
        posf2 = A.alloc([NT], F32, parts=64)
        Bpos = Buf("pos")
        A.release(mBov)
        wt_t = [A.alloc([16, 512], BF16) for _ in range(2)]
        wt_b = [Buf("wt0"), Buf("wt1")]
        lat = A.alloc([4, NT], BF16)
        Blat = [Buf("lat%d" % i) for i in range(4)]
        sq = [A.alloc([512], BF16) for _ in range(2)]
        Bsq = [Buf("sq0"), Buf("sq1")]
        rbc = A.alloc([512], F32)
        rtmp = A.alloc([512], F32)
        Brbc = Buf("rbc")
        NSTG = 6
        stg = [A.alloc([512], BF16) for _ in range(NSTG)]
        Bstg = [Buf("stg%d" % i) for i in range(NSTG)]
        rt = [A.alloc([512], F32) for _ in range(2)]
        Brt = [Buf("rt0"), Buf("rt1")]
        w_in_v = w_in.rearrange("(k p) n -> p k n", p=128)
        state = {"stg": 0, "wt": 0, "bank": 0, "rt": 0, "sq": 0, "ck": 0}

        def checkpoint():
            state["ck"] += 1
            if stop_after == "B2:%d" % state["ck"]:
                raise StopBuild()

        def next_stg():
            i = state["stg"]
            state["stg"] = (i + 1) % NSTG
            return stg[i], Bstg[i]

        def next_bank(lo=0, hi=6):
            i = state["bank"]
            state["bank"] = i + 1
            b = lo + i % (hi - lo)
            return PS[b], PSB[b], b

        def load_w(c0, ncols):
            i = state["wt"]
            state["wt"] = i + 1
            wt, wb = wt_t[i % 2], wt_b[i % 2]
            P.dma(wt[:, :, 0:ncols], w_in_v[:, :, c0:c0 + ncols], writes=[wb], eng="pool")
            return wt, wb

        def rope_rows(tile, Btile, rows, perm, ctab, stab, tok0):
            ps, pb, _ = next_bank(6, 8)
            mm(ps[0:rows, :], perm, tile[0:rows, :], True, True, [Btile, Bc], [pb])
            i = state["rt"]
            state["rt"] = i + 1
            r1, B1_ = rt[i % 2], Brt[i % 2]
            tt("dve", r1[0:rows, :], ps[0:rows, :], stab[0:rows, tok0:tok0 + 512], ALU.mult, [pb, Brope], [B1_])
            tt("dve", tile[0:rows, :], tile[0:rows, :], ctab[0:rows, tok0:tok0 + 512], ALU.mult, [Btile, Brope], [Btile])
            tt("dve", tile[0:rows, :], tile[0:rows, :], r1[0:rows, :], ALU.add, [Btile, B1_], [Btile])

        try:
          for tsi, xsrc in enumerate((x_own, x_oth)):
            own = tsi == 0
            tokbase = tsi * NT
            P.barrier()
            P.dma(posi, pos_in[tsi:tsi + 1, :].partition_broadcast(64), writes=[Bpos])
            cp("dve", posf, posi, [Bpos], [Bpos])
            for (rows, fcol, scol, ctab, stab) in ((64, 0, 1, cosM, sinM), (32, 2, 3, cosD, sinD)):
                for (shift, tab, signed) in ((0.0, stab, True), (0.5 * PI, ctab, False)):
                    a = ang[0:rows, :]
                    kf = posf2[0:rows, :]
                    ts("dve", a, posf[0:rows, :], cf[0:rows, fcol:fcol + 1], shift, ALU.mult, ALU.add, [Bpos, Bc], [Bpos])
                    ts("dve", kf, a, 1.0 / (2 * PI), 0.0, ALU.mult, ALU.add, [Bpos], [Bpos])
                    cp("dve", posi[0:rows, :], kf, [Bpos], [Bpos])
                    cp("dve", kf, posi[0:rows, :], [Bpos], [Bpos])
                    stt("dve", a, kf, -2 * PI, a, ALU.mult, ALU.add, [Bpos], [Bpos])
                    ts("dve", kf, a, PI, -2 * PI, ALU.is_gt, ALU.mult, [Bpos], [Bpos])
                    tt("dve", a, a, kf, ALU.add, [Bpos], [Bpos])
                    ts("dve", kf, a, -PI, 2 * PI, ALU.is_lt, ALU.mult, [Bpos], [Bpos])
                    tt("dve", a, a, kf, ALU.add, [Bpos], [Bpos])
                    ts("dve", a, a, 3.141592, -3.141592, ALU.min, ALU.max, [Bpos], [Bpos])
                    act(a, a, AF.Sin, [Bpos], [Bpos])
                    dst = tab[0:rows, :]
                    if signed:
                        ts("dve", dst, a, cf[0:rows, scol:scol + 1], 0.0, ALU.mult, ALU.add, [Bpos, Bc], [Brope])
                    else:
                        cp("dve", dst, a, [Bpos], [Brope])
            for ti in range(16):
                i2 = ti % 2
                sidx = tsi * 16 + ti
                P.dma(xt[i2], xsrc[ti * 128:(ti + 1) * 128, :], writes=[Bxt[i2]])
                act(junk, xt[i2], AF.Square, [Bxt[i2]], [Bjunk, Bstat[sidx]], accum_out=stat[:, sidx, 0:1])
                rsqrt_chain(stat[:, sidx, 1:2], stat[:, sidx, 0:1], 1.0 / D, [Bstat[sidx]], [Bstat[sidx]], stat[:, sidx, 2:3])
                act(xn[i2], xt[i2], AF.Copy, [Bxt[i2], Bstat[sidx]], [Bxn[i2]], scale=stat[:, sidx, 1:2])
                for half in range(2):
                    bank = 6 + half
                    for kk in range(8):
                        k = half * 8 + kk
                        P.add("pe", lambda e, bank=bank, kk=kk, k=k, i2=i2: e.transpose(
                            out=PSbf[bank][:, kk * 128:(kk + 1) * 128], in_=xn[i2][:, k * 128:(k + 1) * 128], identity=ident),
                            [Bxn[i2], Bc], [PSB[bank]])
                    src = PSbf[bank][:, 0:1024].rearrange("p (k n) -> p k n", k=8)
                    a1 = AB[:, 0, half * 8:(half + 1) * 8].unsqueeze(2).to_broadcast([128, 8, 128])
                    b1 = AB[:, 1, half * 8:(half + 1) * 8].unsqueeze(2).to_broadcast([128, 8, 128])
                    tt("dve", evt[half], src, a1, ALU.mult, [PSB[bank], Bab], [Bevt[half]])
                    tt("dve", hT[:, half * 8:(half + 1) * 8, ti * 128:(ti + 1) * 128], evt[half], b1, ALU.add,
                       [Bevt[half], Bab], [BhT[ti]])

            P.barrier()
            if debug and own:
                dH = nc.dram_tensor("dbgH", [128, 16, NT], BF16, kind="ExternalOutput").ap()
                dR = nc.dram_tensor("dbgR", [64, 4 * NT], BF16, kind="ExternalOutput").ap()
                P.dma(dH, hT, reads=BhT)
                P.dma(dR[:, 0:NT], cosM, reads=[Brope])
                P.dma(dR[:, NT:2 * NT], sinM, reads=[Brope])
                P.dma(dR[0:32, 2 * NT:3 * NT], cosD, reads=[Brope])
                P.dma(dR[0:32, 3 * NT:4 * NT], sinD, reads=[Brope])
            if stop_after == "B1":
                return nc, P.emit()
            def latent_segment(c0, gcol, is_q):
                wt, wb = load_w(c0, 512)
                for tg in range(4):
                    tsl = slice(tg * 512, (tg + 1) * 512)
                    hreads = [BhT[tg * 4 + j] for j in range(4)]
                    for m in range(4):
                        ps, pb, _ = next_bank()
                        for k in range(16):
                            mm(ps, wt[:, k, m * 128:(m + 1) * 128], hT[:, k, tsl], k == 0, k == 15, hreads + [wb], [pb])
                        i = state["sq"]
                        state["sq"] = i + 1
                        s_, bs_ = sq[i % 2], Bsq[i % 2]
                        act(s_, ps, AF.Square, [pb], [bs_])
                        ts("dve", lat[:, m, tsl], ps, gcol[:, m:m + 1], 0.0, ALU.mult, ALU.add, [pb, Bc], [Blat[tg]])
                        mm(PS[6], ones, s_, m == 0, m == 3, [bs_, Bc], [PSB[6]])
                    rsqrt_chain(rbc, PS[6], 1.0 / 512, [PSB[6]], [Brbc], rtmp)
                    for m in range(4):
                        tt("dve", lat[:, m, tsl], lat[:, m, tsl], rbc, ALU.mult, [Blat[tg], Brbc], [Blat[tg]])

            def up_fm(wsb, Bw, col0, M, tg, dst_ap, rope=None):
                tsl = slice(tg * 512, (tg + 1) * 512)
                ps, pb, _ = next_bank()
                for k in range(4):
                    mm(ps[0:M, :], wsb[:, k, col0:col0 + M], lat[:, k, tsl], k == 0, k == 3, [Bw, Blat[tg]], [pb])
                sg_, bsg = next_stg()
                act(sg_[0:M, :], ps[0:M, :], AF.Copy, [pb], [bsg])
                if rope is not None:
                    rope_rows(sg_, bsg, *rope, tg * 512)
                P.dma(dst_ap, sg_[0:M, :], reads=[bsg])

            tcols = slice(tokbase, tokbase + NT)
            if own:
                latent_segment(C_QA, qg, True)
                checkpoint()
                for tg in range(4):
                    c512 = slice(tg * 512, (tg + 1) * 512)
                    for h in range(8):
                        up_fm(wq_sb, Bwq, h * 192, 128, tg, QT_m[h, 0:128, c512])
                        up_fm(wq_sb, Bwq, h * 192 + 128, 64, tg, QT_m[h, 128:192, c512], rope=(64, perm64, cosM, sinM))
            checkpoint()
            latent_segment(C_CKV, kvg, False)
            for tg in range(4):
                c512 = slice(tokbase + tg * 512, tokbase + (tg + 1) * 512)
                for h in range(8):
                    up_fm(wkv_sb, Bwkv, h * 256, 128, tg, KT_m[h, :, c512])
                if tg == 0:
                    checkpoint()
                wkv_h = wkv_sb.rearrange("p k (h c) -> p k h c", c=256)
                for tt_ in range(4):
                    tile = tsi * 16 + tg * 4 + tt_
                    tok = slice(tg * 512 + tt_ * 128, tg * 512 + (tt_ + 1) * 128)
                    for hh in range(2):
                        ps, pb, _ = next_bank()
                        for k in range(4):
                            mm(ps.rearrange("p (h c) -> p h c", h=4), lat[:, k, tok], wkv_h[:, k, hh * 4:(hh + 1) * 4, 128:256],
                               k == 0, k == 3, [Bwkv, Blat[tg]], [pb])
                        sg_, bsg = next_stg()
                        act(sg_, ps, AF.Copy, [pb], [bsg])
                        P.dma(V_m[hh * 4:(hh + 1) * 4, :, tile, :].rearrange("h p d -> p h d"),
                              sg_.rearrange("p (h d) -> p h d", h=4), reads=[bsg])
            checkpoint()
            wt, wb = load_w(C_KR, 64)
            for tg in range(4):
                tsl = slice(tg * 512, (tg + 1) * 512)
                hreads = [BhT[tg * 4 + j] for j in range(4)]
                ps, pb, _ = next_bank()
                for k in range(16):
                    mm(ps[0:64, :], wt[:, k, 0:64], hT[:, k, tsl], k == 0, k == 15, hreads + [wb], [pb])
                sg_, bsg = next_stg()
                act(sg_[0:64, :], ps[0:64, :], AF.Copy, [pb], [bsg])
                rope_rows(sg_, bsg, 64, perm64, cosM, sinM, tg * 512)
                P.dma(KRT[:, tokbase + tg * 512: tokbase + (tg + 1) * 512], sg_[0:64, :], reads=[bsg])

            def dil_fm(cbase, dstT, ncol_tok_base):
                for cg in range(3):
                    wt, wb = load_w(cbase + cg * 512, 512)
                    for tg in range(4):
                        tsl = slice(tg * 512, (tg + 1) * 512)
                        hreads = [BhT[tg * 4 + j] for j in range(4)]
                        for m in range(4):
                            h = cg * 4 + m
                            ps, pb, _ = next_bank()
                            for k in range(16):
                                mm(ps, wt[:, k, m * 128:(m + 1) * 128], hT[:, k, tsl], k == 0, k == 15, hreads + [wb], [pb])
                            sg_, bsg = next_stg()
                            act(sg_, ps, AF.Copy, [pb], [bsg])
                            rope_rows(sg_, bsg, 32, perm32, cosD, sinD, tg * 512)
                            P.dma(dstT[h, :, ncol_tok_base + tg * 512: ncol_tok_base + (tg + 1) * 512], sg_, reads=[bsg])

            checkpoint()
            if own:
                dil_fm(C_DQ, QT_d, 0)
            checkpoint()
            dil_fm(C_DK, KT_d, tokbase)
            checkpoint()
            for cg in range(3):
                wt, wb = load_w(C_DV + cg * 512, 512)
                for ti in range(16):
                    tile = tsi * 16 + ti
                    ps, pb, _ = next_bank()
                    for k in range(16):
                        mm(ps, hT[:, k, ti * 128:(ti + 1) * 128], wt[:, k, :], k == 0, k == 15, [BhT[ti], wb], [pb])
                    sg_, bsg = next_stg()
                    act(sg_, ps, AF.Copy, [pb], [bsg])
                    P.dma(V_d[cg * 4:(cg + 1) * 4, :, tile, :].rearrange("h p d -> p h d"),
                          sg_.rearrange("p (h d) -> p h d", h=4), reads=[bsg])
            checkpoint()
            if own:
                for cg in range(8):
                    wt, wb = load_w(C_GA + cg * 512, 512)
                    for tg in range(4):
                        tsl = slice(tg * 512, (tg + 1) * 512)
                        hreads = [BhT[tg * 4 + j] for j in range(4)]
                        for m in range(4):
                            ps, pb, _ = next_bank()
                            for k in range(16):
                                mm(ps, wt[:, k, m * 128:(m + 1) * 128], hT[:, k, tsl], k == 0, k == 15, hreads + [wb], [pb])
                            sg_, bsg = next_stg()
                            act(sg_, ps, AF.Sigmoid, [pb], [bsg])
                            P.dma(SG[cg * 4 + m, :, tsl], sg_, reads=[bsg])
        except StopBuild:
            return nc, P.emit()
        P.barrier()
        A.release(mA)
        if stop_after == "B":
            return nc, P.emit()

        mC = A.mark()
        masks_sb = A.alloc([NM, 512], BF16)
        Bmask = Buf("masks")
        for i0 in range(0, NM, 16):
            i1 = min(NM, i0 + 16)
            P.dma(masks_sb[:, i0:i1, :], masks_in[:, i0:i1, :], writes=[Bmask])
        krt_sb = A.alloc([S], BF16, parts=64)
        Bkrt = Buf("krt")
        P.dma(krt_sb, KRT, writes=[Bkrt])
        NHB = 4
        hb_k = [A.alloc([S], BF16) for _ in range(NHB)]
        hb_v = [A.alloc([32, 128], BF16) for _ in range(NHB)]
        hb_q = [A.alloc([NT], BF16) for _ in range(NHB)]
        hb_qr = [A.alloc([NT], BF16, parts=64) for _ in range(2)]
        Bhk = [Buf("hk%d" % i) for i in range(NHB)]
        Bhv = [Buf("hv%d" % i) for i in range(NHB)]
        Bhq = [Buf("hq%d" % i) for i in range(NHB)]
        Bqr = [Buf("qr0"), Buf("qr1")]
        NPT = 4
        pT = [A.alloc([512], BF16) for _ in range(NPT)]
        BpT = [Buf("pT%d" % i) for i in range(NPT)]
        rden = A.alloc([512], F32)
        Brden = Buf("rden")
        ost = [A.alloc([512], BF16) for _ in range(2)]
        Bost = [Buf("ost0"), Buf("ost1")]
        cnt = {"pt": 0, "s": 0, "o": 0, "hb": 0}

        tiles = []

        def emit_attention():
            LA = 2
            flat = []
            for ti_, (pre, blocks, scale, dst) in enumerate(tiles):
                for bi in range(len(blocks)):
                    flat.append((ti_, bi))
            ptinfo = {}

            def s_stage(f):
                ti_, bi = flat[f]
                pre, blocks, scale, dst = tiles[ti_]
                if bi == 0:
                    for fn in pre:
                        fn()
                qparts, kparts, v_ap, vbufs, mi = blocks[bi]
                sb = f % 4
                npart = len(qparts)
                for pi in range(npart):
                    qa, qrows, qb = qparts[pi]
                    ka, krows, kb = kparts[pi]
                    mm(PS[sb], ka, qa, pi == 0, pi == npart - 1, qb + kb, [PSB[sb]])
                pt, bpt = pT[f % NPT], BpT[f % NPT]
                act(pt, PS[sb], AF.Exp, [PSB[sb]], [bpt], scale=scale)
                if mi is not None:
                    tt("pool" if f % 3 != 2 else "dve", pt, pt, masks_sb[:, mi, :], ALU.mult, [bpt, Bmask], [bpt])

            def pv_stage(f):
                ti_, bi = flat[f]
                pre, blocks, scale, dst = tiles[ti_]
                nb = len(blocks)
                qparts, kparts, v_ap, vbufs, mi = blocks[bi]
                ob, db = 4 + 2 * (ti_ % 2), 5 + 2 * (ti_ % 2)
                pt, bpt = pT[f % NPT], BpT[f % NPT]
                mm(PS[ob], v_ap, pt, bi == 0, bi == nb - 1, vbufs + [bpt], [PSB[ob]])
                mm(PS[db], ones, pt, bi == 0, bi == nb - 1, [bpt, Bc], [PSB[db]])
                if bi == nb - 1:
                    P.add("dve", lambda e: e.reciprocal(out=rden, in_=PS[db]), [PSB[db]], [Brden])
                    o_, bo_ = ost[ti_ % 2], Bost[ti_ % 2]
                    tt("dve", o_, PS[ob], rden, ALU.mult, [PSB[ob], Brden], [bo_])
                    P.dma(dst, o_, reads=[bo_], eng="pool")

            n = len(flat)
            for f in range(min(LA, n)):
                s_stage(f)
            for f in range(n):
                if f + LA < n:
                    s_stage(f + LA)
                pv_stage(f)

        sc_m = 192 ** -0.5

        def mla_loads(h):
            hi = h % NHB
            qi_ = h % 2

            def fn():
                P.dma(hb_k[hi], KT_m[h], writes=[Bhk[hi]])
                P.dma(hb_v[hi], V_m[h], writes=[Bhv[hi]])
                P.dma(hb_q[hi], QT_m[h, 0:128, :], writes=[Bhq[hi]])
                P.dma(hb_qr[qi_], QT_m[h, 128:192, :], writes=[Bqr[qi_]])
            return fn

        def dil_loads(slot_idx, g):
            hi = (8 + slot_idx * 3 + g) % NHB
            h = 4 * g + slot_idx

            def fn():
                P.dma(hb_k[hi], KT_d[h], writes=[Bhk[hi]])
                P.dma(hb_v[hi], V_d[h], writes=[Bhv[hi]])
                P.dma(hb_q[hi], QT_d[h], writes=[Bhq[hi]])
            return fn

        for h in range(8):
            hi = h % NHB
            qi_ = h % 2
            for t in range(4):
                pre = []
                if h == 0 and t == 0:
                    pre = [mla_loads(0), mla_loads(1)]
                elif t == 0:
                    pre = [mla_loads(h + 1)] if h + 1 < 8 else [dil_loads(0, 0)]
                qsl = slice(t * 512, (t + 1) * 512)
                blocks = []
                for kind, base in (("own", 0), ("oth", 1)):
                    for jk in range(4 * t + 4):
                        ksl = slice(base * NT + jk * 128, base * NT + (jk + 1) * 128)
                        mi = midx[("m", kind, jk - 4 * t)] if jk >= 4 * t else None
                        blocks.append((
                            [(hb_q[hi][:, qsl], 128, [Bhq[hi]]), (hb_qr[qi_][:, qsl], 64, [Bqr[qi_]])],
                            [(hb_k[hi][:, ksl], 128, [Bhk[hi]]), (krt_sb[:, ksl], 64, [Bkrt])],
                            hb_v[hi][:, base * 16 + jk, :], [Bhv[hi]], mi))
                tiles.append((pre, blocks, sc_m, OT_m[h, :, qsl]))
        sc_d = 128 ** -0.5
        for s_ in range(4):
            for t in range(4):
                pre = []
                if t == 0:
                    pre = [dil_loads(s_, 1), dil_loads(s_, 2)]
                    if s_ > 0:
                        pre = [dil_loads(s_, 0)] + pre
                qsl = slice(t * 512, (t + 1) * 512)
                blocks = []
                for g in range(3):
                    hi = (8 + s_ * 3 + g) % NHB
                    for kind, base in (("own", 0), ("oth", 1)):
                        for rho in range(-3, 10):
                            jk = 4 * t - rho
                            key = ("d", g, kind, rho)
                            if jk < 0 or jk > 15 or key not in midx:
                                continue
                            ksl = slice(base * NT + jk * 128, base * NT + (jk + 1) * 128)
                            blocks.append((
                                [(hb_q[hi][:, qsl], 128, [Bhq[hi]])],
                                [(hb_k[hi][:, ksl], 128, [Bhk[hi]])],
                                hb_v[hi][:, base * 16 + jk, :], [Bhv[hi]], midx[key]))
                tiles.append((pre, blocks, sc_d, OT_d[s_, :, qsl]))
        emit_attention()
        P.barrier()
        A.release(mC)
        if stop_after == "C":
            return nc, P.emit()

        mD = A.mark()
        merged = A.alloc([16, NT], BF16)
        Bmg = [Buf("mg%d" % i) for i in range(4)]
        mD1 = A.mark()
        wmo = A.alloc([8, D], BF16)
        wdo = A.alloc([4, D], BF16)
        Bwmo, Bwdo = Buf("wmo"), Buf("wdo")
        P.dma(wmo, w_mla_o.rearrange("(k p) n -> p k n", p=128), writes=[Bwmo], eng="pool")
        P.dma(wdo, w_dil_o.rearrange("(k p) n -> p k n", p=128), writes=[Bwdo], eng="pool")
        otm = [A.alloc([8, 512], BF16) for _ in range(2)]
        otd = [A.alloc([4, 512], BF16) for _ in range(2)]
        Bot = [Buf("ot0"), Buf("ot1")]
        Botd = [Buf("otd0"), Buf("otd1")]
        sga = [A.alloc([512], BF16) for _ in range(3)]
        sgb = [A.alloc([512], BF16) for _ in range(3)]
        Bsgt = [Buf("sg%d" % i) for i in range(3)]
        Bsgtb = [Buf("sgb%d" % i) for i in range(3)]
        t1 = [A.alloc([512], F32) for _ in range(2)]
        t2 = [A.alloc([512], F32) for _ in range(2)]
        Bt12 = [Buf("t12_0"), Buf("t12_1")]
        ci = 0
        for tg in range(4):
            tsl = slice(tg * 512, (tg + 1) * 512)
            o2 = tg % 2
            P.dma(otm[o2], OT_m[:, :, tsl].rearrange("h p t -> p h t"), writes=[Bot[o2]])
            P.dma(otd[o2], OT_d[:, :, tsl].rearrange("h p t -> p h t"), writes=[Botd[o2]])
            for m in range(16):
                s3 = ci % 3
                c2 = ci % 2
                ci += 1
                P.dma(sga[s3], SG[m, :, tsl], writes=[Bsgt[s3]])
                P.dma(sgb[s3], SG[16 + m, :, tsl], writes=[Bsgtb[s3]])
                ba, bb = (0, 1) if c2 == 0 else (2, 3)
                for k in range(8):
                    mm(PS[ba], wmo[:, k, m * 128:(m + 1) * 128], otm[o2][:, k, :], k == 0, k == 7, [Bwmo, Bot[o2]], [PSB[ba]])
                for k in range(4):
                    mm(PS[bb], wdo[:, k, m * 128:(m + 1) * 128], otd[o2][:, k, :], k == 0, k == 3, [Bwdo, Botd[o2]], [PSB[bb]])
                tt("dve", t1[c2], PS[ba], sga[s3], ALU.mult, [PSB[ba], Bsgt[s3]], [Bt12[c2]])
                tt("dve", t2[c2], PS[bb], sgb[s3], ALU.mult, [PSB[bb], Bsgtb[s3]], [Bt12[c2]])
                tt("pool", merged[:, m, tsl], t1[c2], t2[c2], ALU.add, [Bt12[c2]], [Bmg[tg]])
        A.release(mD1)

        P.barrier()
        AB2r = A.alloc([2, D], BF16)
        BAB2 = Buf("AB2r")
        mD2a = A.mark()
        rows2 = A.alloc([3 * D], F32)
        Brows2 = Buf("rows2")
        P.dma(rows2, rowv[:, 4 * D + 64:7 * D + 64].partition_broadcast(128), writes=[Brows2])
        sc2 = A.alloc([16], BF16)
        scB2 = A.alloc([16, 128], BF16)
        Bsc2 = Buf("sc2")
        act(sc2, colv_sb[:, 0:16], AF.Silu, [Bc], [Bsc2])
        for k in range(16):
            cp("dve", scB2[:, k, :], sc2[:, k:k + 1].to_broadcast([128, 128]), [Bsc2], [Bsc2])
        wa2 = [A.alloc([16, 512], BF16) for _ in range(2)]
        Bwa2 = [Buf("wa2_0"), Buf("wa2_1")]
        tmpr = A.alloc([512], F32)
        Btmpr = Buf("tmpr")
        wada_v2 = w_ada.rearrange("(k p) n -> p k n", p=128)
        gi2 = 0
        for which, seg in ((1, 3), (0, 4)):
            for n in range(4):
                wt2, wb2 = wa2[gi2 % 2], Bwa2[gi2 % 2]
                gi2 += 1
                c0 = seg * D + n * 512
                P.dma(wt2, wada_v2[:, :, c0:c0 + 512], writes=[wb2], eng="pool")
                bank = 4 + (n % 2)
                for k in range(16):
                    mm(PS[bank], scB2[:, k, :], wt2[:, k, :], k == 0, k == 15, [wb2, Bsc2], [PSB[bank]])
                nsl = slice(n * 512, (n + 1) * 512)
                if which == 1:
                    tt("dve", AB2r[:, 1, nsl], PS[bank], rows2[:, n * 512:(n + 1) * 512], ALU.add, [PSB[bank], Brows2], [BAB2])
                else:
                    tt("dve", tmpr, PS[bank], rows2[:, D + n * 512:D + (n + 1) * 512], ALU.add, [PSB[bank], Brows2], [Btmpr])
                    stt("dve", AB2r[:, 0, nsl], tmpr, 1.0, rows2[:, 2 * D + n * 512:2 * D + (n + 1) * 512], ALU.add, ALU.mult,
                        [Btmpr, Brows2], [BAB2])
        P.barrier()
        A.release(mD2a)
        wo = A.alloc([16, D], BF16)
        Bwo = Buf("wo")
        P.dma(wo, w_out.rearrange("(k p) n -> p k n", p=128), writes=[Bwo], eng="pool")
        xt2 = [A.alloc([D], F32)] * 2
        Bxt2 = [Buf("xt2_0")] * 2
        yt = [A.alloc([D], F32) for _ in range(2)]
        Byt = [Buf("yt0"), Buf("yt1")]
        h2k = [A.alloc([D], BF16) for _ in range(2)]
        Bh2k = [Buf("h2k0"), Buf("h2k1")]
        junk2 = A.alloc([D], BF16)
        Bjunk2 = Buf("junk2")
        stat2 = A.alloc([16, 12], F32)
        Bst2 = [Buf("st2_%d" % i) for i in range(16)]
        h2t = [A.alloc([16, 128], BF16) for _ in range(2)]
        Bh2t = [Buf("h2t0"), Buf("h2t1")]
        def d2_part1(ti):
            i2 = ti % 2
            tg = ti // 4
            tok = slice(ti * 128, (ti + 1) * 128)
            P.dma(xt2[i2], x_own[tok, :], writes=[Bxt2[i2]])
            for n in range(4):
                for k in range(16):
                    mm(PS[n], merged[:, k, tok], wo[:, k, n * 512:(n + 1) * 512], k == 0, k == 15, [Bmg[tg], Bwo], [PSB[n]])
            for n in range(4):
                act(junk2[:, 0:512], PS[n], AF.Square, [PSB[n]], [Bjunk2, Bst2[ti]], accum_out=stat2[:, ti, n:n + 1])
            P.add("dve", lambda e, ti=ti: e.tensor_reduce(out=stat2[:, ti, 4:5], in_=stat2[:, ti, 0:4], axis=mybir.AxisListType.X, op=ALU.add),
                  [Bst2[ti]], [Bst2[ti]])
            rsqrt_chain(stat2[:, ti, 5:6], stat2[:, ti, 4:5], 1.0 / D, [Bst2[ti]], [Bst2[ti]], stat2[:, ti, 6:7])
            for n in range(4):
                nsl = slice(n * 512, (n + 1) * 512)
                stt("dve", yt[i2][:, nsl], PS[n], stat2[:, ti, 5:6], Gab[:, 0, nsl], ALU.mult, ALU.mult,
                    [PSB[n], Bst2[ti], Bg], [Byt[i2]])
            tt("pool", yt[i2], yt[i2], xt2[i2], ALU.add, [Byt[i2], Bxt2[i2]], [Byt[i2]])
            P.dma(X1[tok, :], yt[i2], reads=[Byt[i2]])
            act(junk2, yt[i2], AF.Square, [Byt[i2]], [Bjunk2, Bst2[ti]], accum_out=stat2[:, ti, 7:8])
            rsqrt_chain(stat2[:, ti, 8:9], stat2[:, ti, 7:8], 1.0 / D, [Bst2[ti]], [Bst2[ti]], stat2[:, ti, 9:10])
            stt("dve", yt[i2], yt[i2], stat2[:, ti, 8:9], AB2r[:, 0, :], ALU.mult, ALU.mult, [Byt[i2], Bst2[ti], BAB2], [Byt[i2]])
            tt("pool", h2k[i2], yt[i2], AB2r[:, 1, :], ALU.add, [Byt[i2], BAB2], [Bh2k[i2]])
            P.dma(H2K[tok, :], h2k[i2], reads=[Bh2k[i2]])

        def d2_part2(ti):
            i2 = ti % 2
            tok = slice(ti * 128, (ti + 1) * 128)
            for half in range(2):
                bank = 6 + half
                for kk in range(8):
                    k = half * 8 + kk
                    P.add("pe", lambda e, bank=bank, kk=kk, k=k, i2=i2: e.transpose(
                        out=PSbf[bank][:, kk * 128:(kk + 1) * 128], in_=h2k[i2][:, k * 128:(k + 1) * 128], identity=ident),
                        [Bh2k[i2], Bc], [PSB[bank]])
                src = PSbf[bank][:, 0:1024].rearrange("p (k n) -> p k n", k=8)
                if half == 0:
                    cp("dve", h2t[i2][:, 0:8, :], src, [PSB[bank]], [Bh2t[i2]])
                else:
                    act(h2t[i2][:, 8:16, :], src, AF.Copy, [PSB[bank]], [Bh2t[i2]])
            P.dma(H2T[:, :, tok], h2t[i2], reads=[Bh2t[i2]])

        d2_part1(0)
        for ti in range(16):
            if ti + 1 < 16:
                d2_part1(ti + 1)
            d2_part2(ti)
        P.barrier()
        A.release(mD)
        if stop_after == "D":
            return nc, P.emit()

        mE = A.mark()
        Wt = A.alloc([16, NE], F32)
        sel_all = A.alloc([16, NE], F32)
        s8_all = A.alloc([16, 8], F32)
        smk = A.alloc([16, NE], BF16)
        destf = A.alloc([16, 8], F32)
        wk = A.alloc([16, 8], F32)
        dest_i = A.alloc([16, 8], I32)
        idxw = A.alloc([NBLK], I32)
        BWt = Buf("Wt")
        Bsel = [Buf("sel%d" % i) for i in range(16)]
        Bsmk = Buf("smk")
        Bdest = [Buf("dest%d" % i) for i in range(16)]
        Bidxw = Buf("idxw")
        mE0 = A.mark()
        wr_sb = A.alloc([16, NE], BF16)
        Bwr = Buf("wr")
        P.dma(wr_sb, w_router.rearrange("(k p) n -> p k n", p=128), writes=[Bwr], eng="pool")
        h2 = [A.alloc([16, 512], BF16) for _ in range(2)]
        Bh2 = [Buf("h2_0"), Buf("h2_1")]
        scs = A.alloc([NE], F32)
        bia = A.alloc([NE], F32)
        top8 = A.alloc([8, 8], F32)
        gsc = A.alloc([8], F32)
        g8 = A.alloc([8], F32)
        gm = A.alloc([8], F32)
        smask = A.alloc([NE], F32)
        wsum = A.alloc([4], F32)
        Brt_ = Buf("route")
        P.add("dve", lambda e: e.memset(destf, 0.0), [], Bdest)
        P.add("dve", lambda e: e.memset(wk, 0.0), [], Bdest)

        def route(ti, h2tile, bh2, col0):
            R = [Brt_]
            sel = sel_all[:, ti, :]
            s8 = s8_all[:, ti, :]
            for k in range(16):
                mm(PS[7][:, 0:NE], h2tile[:, k, col0:col0 + 128], wr_sb[:, k, :], k == 0, k == 15, [bh2, Bwr], [PSB[7]])
            act(scs, PS[7][:, 0:NE], AF.Sigmoid, [PSB[7]], R)
            tt("dve", bia, scs, rbias, ALU.add, R + [Bc], R)
            for g in range(8):
                P.add("dve", lambda e, g=g: e.max(out=top8[:, g, :], in_=bia[:, g * 8:(g + 1) * 8]), R, R)
            tt("dve", gsc, top8[:, :, 0], top8[:, :, 1], ALU.add, R, R)
            P.add("dve", lambda e: e.max(out=g8, in_=gsc), R, R)
            ts("dve", gm, gsc, g8[:, 3:4], 0.0, ALU.is_ge, ALU.add, R, R)
            gmb = gm.unsqueeze(2).to_broadcast([128, 8, 8])
            tt("dve", sel.rearrange("p (g c) -> p g c", g=8), bia.rearrange("p (g c) -> p g c", g=8), gmb, ALU.mult, R, R + [Bsel[ti]])
            ts("dve", gm, gm, -1.0, 4.0, ALU.add, ALU.mult, R, R)
            tt("dve", sel.rearrange("p (g c) -> p g c", g=8), sel.rearrange("p (g c) -> p g c", g=8),
               gm.unsqueeze(2).to_broadcast([128, 8, 8]), ALU.add, R + [Bsel[ti]], R + [Bsel[ti]])
            P.add("dve", lambda e: e.max(out=s8, in_=sel), R + [Bsel[ti]], R + [Bsel[ti]])
            ts("dve", smask, sel, s8[:, 5:6], 0.0, ALU.is_ge, ALU.add, R + [Bsel[ti]], R)
            cp("dve", smk[:, ti, :], smask, R, R + [Bsmk])
            tt("dve", smask, smask, scs, ALU.mult, R, R)
            P.add("dve", lambda e: e.tensor_reduce(out=wsum[:, 0:1], in_=smask, axis=mybir.AxisListType.X, op=ALU.add), R, R)
            ts("dve", wsum[:, 1:2], wsum[:, 0:1], 1e-20, 0.4, ALU.add, ALU.mult, R, R)
            P.add("dve", lambda e: e.reciprocal(out=wsum[:, 2:3], in_=wsum[:, 1:2]), R, R)
            ts("dve", Wt[:, ti, :], smask, wsum[:, 2:3], 0.0, ALU.mult, ALU.add, R, R + [BWt])

        wsg = A.alloc([16, 512], BF16)
        wsu = A.alloc([16, 512], BF16)
        wsd = A.alloc([4, D], BF16)
        Bws = Buf("ws")
        P.dma(wsg, w_sg.rearrange("(k p) n -> p k n", p=128), writes=[Bws], eng="pool")
        P.dma(wsu, w_su.rearrange("(k p) n -> p k n", p=128), writes=[Bws], eng="pool")
        P.dma(wsd, w_sd.rearrange("(k p) n -> p k n", p=128), writes=[Bws], eng="pool")
        sil3 = [A.alloc([512], F32) for _ in range(2)]
        Bsil3 = [Buf("sil3_0"), Buf("sil3_1")]
        aT3 = A.alloc([4, 512], BF16)
        BaT3 = Buf("aT3")
        sho_sb = [A.alloc([D], BF16) for _ in range(2)]
        Bsho = [Buf("sho0"), Buf("sho1")]
        for tg in range(4):
            hsel = tg % 2
            P.dma(h2[hsel], H2T[:, :, tg * 512:(tg + 1) * 512], writes=[Bh2[hsel]])
            for m in range(4):
                bg, bu = (0, 1) if m % 2 == 0 else (2, 3)
                for k in range(16):
                    mm(PS[bg], wsg[:, k, m * 128:(m + 1) * 128], h2[hsel][:, k, :], k == 0, k == 15, [Bws, Bh2[hsel]], [PSB[bg]])
                for k in range(16):
                    mm(PS[bu], wsu[:, k, m * 128:(m + 1) * 128], h2[hsel][:, k, :], k == 0, k == 15, [Bws, Bh2[hsel]], [PSB[bu]])
                s2 = m % 2
                act(sil3[s2], PS[bg], AF.Silu, [PSB[bg]], [Bsil3[s2]])
                tt("pool" if False else "dve", aT3[:, m, :], PS[bu], sil3[s2], ALU.mult, [PSB[bu], Bsil3[s2]], [BaT3])
            for tt_ in range(4):
                tile = tg * 4 + tt_
                i2 = tile % 2
                for n in range(4):
                    bk = 4 + (tt_ * 4 + n) % 3
                    for k in range(4):
                        mm(PS[bk], aT3[:, k, tt_ * 128:(tt_ + 1) * 128], wsd[:, k, n * 512:(n + 1) * 512], k == 0, k == 3,
                           [BaT3, Bws], [PSB[bk]])
                    act(sho_sb[i2][:, n * 512:(n + 1) * 512], PS[bk], AF.Copy, [PSB[bk]], [Bsho[i2]])
                P.dma(SHO[tile * 128:(tile + 1) * 128, :], sho_sb[i2], reads=[Bsho[i2]])
            for tt_ in range(4):
                route(tg * 4 + tt_, h2[hsel], Bh2[hsel], tt_ * 128)
        cnt = A.alloc([NE], F32)
        pcnt = A.alloc([NE], F32)
        ca = A.alloc([NE], F32)
        cb = A.alloc([NE], F32)
        base = A.alloc([NE], F32)
        thr = A.alloc([8], F32)
        cmp1 = A.alloc([NE, 8], F32)
        Be1 = Buf("e1")
        Bbase = Buf("base")
        E1 = [Be1]
        for ti in range(16):
            mm(PS[6][:, 0:NE], ones, smk[:, ti, :], ti == 0, ti == 15, [Bc, Bsmk], [PSB[6]])
        cp("dve", cnt, PS[6][:, 0:NE], [PSB[6]], E1)
        for j in range(8):
            P.add("dve", lambda e, j=j: e.memset(thr[:, j:j + 1], 256.0 * j), E1, E1)
        tt("dve", cmp1, cnt.unsqueeze(2).to_broadcast([128, NE, 8]), thr.unsqueeze(1).to_broadcast([128, NE, 8]), ALU.is_gt, E1, E1)
        P.add("dve", lambda e: e.tensor_reduce(out=pcnt, in_=cmp1, axis=mybir.AxisListType.X, op=ALU.add), E1, E1)
        cp("dve", ca, pcnt, E1, E1)
        src_, dst_ = ca, cb
        for sft in (2, 4, 8, 16, 32):
            cp("dve", dst_[:, 0:sft], src_[:, 0:sft], E1, E1)
            tt("dve", dst_[:, sft:NE], src_[:, sft:NE], src_[:, 0:NE - sft], ALU.add, E1, E1)
            src_, dst_ = dst_, src_
        pends = src_
        tt("dve", base, pends, pcnt, ALU.subtract, E1, E1 + [Bbase])
        ts("dve", base, base, 512.0, 0.0, ALU.mult, ALU.add, E1 + [Bbase], E1 + [Bbase])
        base2 = base.rearrange("p (i two) -> p i two", two=2)
        ts("dve", base2[:, :, 1], base2[:, :, 1], 256.0, 0.0, ALU.add, ALU.add, E1 + [Bbase], E1 + [Bbase])
        HB = NBLK // 2
        lpg = cf[:, 8:8 + HB]
        pends2 = pends.rearrange("p (i two) -> p i two", two=2)
        idxw2 = idxw.rearrange("p (l two) -> p l two", two=2)
        cmpL = A.alloc([HB, NE // 2], F32)
        cntL = A.alloc([HB], F32)
        usedL = A.alloc([HB], F32)
        eqL = A.alloc([HB], F32)
        for lane in range(2):
            tt("dve", cmpL, pends2[:, :, lane].unsqueeze(1).to_broadcast([128, HB, NE // 2]),
               lpg.unsqueeze(2).to_broadcast([128, HB, NE // 2]), ALU.is_le, E1 + [Bc], E1)
            P.add("dve", lambda e: e.tensor_reduce(out=cntL, in_=cmpL, axis=mybir.AxisListType.X, op=ALU.add), E1, E1)
            ts("dve", cntL, cntL, 31.0, 2.0, ALU.min, ALU.mult, E1, E1)
            ts("dve", cntL, cntL, float(lane), 128.0, ALU.add, ALU.mult, E1, E1)
            tt("dve", cntL, cntL, cf[:, 120:121].to_broadcast([128, HB]), ALU.add, E1 + [Bc], E1)
            ts("dve", usedL, lpg, pends[:, NE - 2 + lane:NE - 1 + lane], 1.0e6, ALU.is_ge, ALU.mult, E1 + [Bc], E1)
            P.add("dve", lambda e: e.memset(eqL[:, 0:1], 0.0), E1, E1)
            tt("dve", eqL[:, 1:HB], cntL[:, 1:HB], cntL[:, 0:HB - 1], ALU.is_equal, E1, E1)
            ts("dve", eqL, eqL, 1.0e6, 0.0, ALU.mult, ALU.add, E1, E1)
            tt("dve", cntL, cntL, usedL, ALU.add, E1, E1)
            tt("dve", cntL, cntL, eqL, ALU.add, E1, E1)
            cp("dve", idxw2[:, :, lane], cntL, E1, E1 + [Bidxw])
        dfull = A.alloc([NE], F32)
        slot_sb = A.alloc([NE], F32)
        flo = A.alloc([NE], F32)
        junk64 = A.alloc([NE], F32)
        h2kt = [A.alloc([D], BF16) for _ in range(2)]
        Bh2kt = [Buf("h2kt0"), Buf("h2kt1")]
        Bdf = Buf("dfull")
        for ti in range(16):
            i2 = ti % 2
            tok = slice(ti * 128, (ti + 1) * 128)
            P.dma(h2kt[i2], H2K[tok, :], writes=[Bh2kt[i2]])
            bank = 4 + ti % 2
            for j in range(ti):
                mm(PS[bank][:, 0:NE], ones, smk[:, j, :], j == 0, False, [Bc, Bsmk], [PSB[bank]])
            mm(PS[bank][:, 0:NE], Utri, smk[:, ti, :], ti == 0, True, [Bc, Bsmk], [PSB[bank]])
            cp("dve", slot_sb, PS[bank][:, 0:NE], [PSB[bank]], [Bdf])
            tt("dve", cmp1[:, :, 0:7], slot_sb.unsqueeze(2).to_broadcast([128, NE, 7]),
               thr[:, 1:8].unsqueeze(1).to_broadcast([128, NE, 7]), ALU.is_ge, [Bdf, Be1], [Bdf, Be1])
            P.add("dve", lambda e: e.tensor_reduce(out=flo, in_=cmp1[:, :, 0:7], axis=mybir.AxisListType.X, op=ALU.add), [Bdf, Be1], [Bdf])
            tt("dve", dfull, slot_sb, base, ALU.add, [Bdf, Bbase], [Bdf])
            stt("dve", dfull, flo, 256.0, dfull, ALU.mult, ALU.add, [Bdf], [Bdf])
            for k in range(6):
                P.add("dve", lambda e, ti=ti, k=k: e.scalar_tensor_tensor(
                    out=junk64, in0=sel_all[:, ti, :], scalar=s8_all[:, ti, k:k + 1], in1=dfull, op0=ALU.is_equal, op1=ALU.mult,
                    accum_out=destf[:, ti, k:k + 1]), [Bsel[ti], Bdf], [Bdest[ti], Bdf])
                P.add("dve", lambda e, ti=ti, k=k: e.scalar_tensor_tensor(
                    out=junk64, in0=sel_all[:, ti, :], scalar=s8_all[:, ti, k:k + 1], in1=Wt[:, ti, :], op0=ALU.is_equal, op1=ALU.mult,
                    accum_out=wk[:, ti, k:k + 1]), [Bsel[ti], BWt], [Bdest[ti], Bdf])
            ts("dve", junk64[:, 0:8], destf[:, ti, :], float(NSLOT) - 0.5, 0.0, ALU.is_lt, ALU.add, [Bdest[ti], Bdf], [Bdf])
            tt("dve", wk[:, ti, :], wk[:, ti, :], junk64[:, 0:8], ALU.mult, [Bdest[ti], Bdf], [Bdest[ti]])
            cp("dve", dest_i[:, ti, :], destf[:, ti, :], [Bdest[ti]], [Bdest[ti]])
            for k in range(6):
                P.add("pool", lambda e, ti=ti, k=k, i2=i2: e.indirect_dma_start(
                    out=XG[:, :], out_offset=bass.IndirectOffsetOnAxis(ap=dest_i[:, ti, k:k + 1], axis=0),
                    in_=h2kt[i2], in_offset=None, bounds_check=P.regs[NSLOT - 1], oob_is_err=False),
                    [Bdest[ti], Bh2kt[i2]], [], dma=True)
        P.barrier()
        A.release(mE0)
        if stop_after == "E2":
            return nc, P.emit()

        mE4 = A.mark()
        wg_t = [A.alloc([16, 512], BF16) for _ in range(2)]
        wu_t = [A.alloc([16, 512], BF16) for _ in range(2)]
        wd_t = [A.alloc([4, D], BF16) for _ in range(2)]
        Bweg = [Buf("weg0"), Buf("weg1")]
        Bweu = [Buf("weu0"), Buf("weu1")]
        Bwed = [Buf("wed0"), Buf("wed1")]
        xg_sb = [A.alloc([2, D], BF16) for _ in range(2)]
        Bxg = [Buf("xg0"), Buf("xg1")]
        xgT = [A.alloc([16, 256], BF16) for _ in range(2)]
        BxgT = [Buf("xgT0"), Buf("xgT1")]
        sil = [A.alloc([256], F32) for _ in range(2)]
        Bsil = [Buf("sil0"), Buf("sil1")]
        aT = [A.alloc([4, 256], BF16) for _ in range(2)]
        BaT = [Buf("aT0"), Buf("aT1")]
        yg_sb = [A.alloc([2, D], BF16) for _ in range(2)]
        Byg = [Buf("yg0"), Buf("yg1")]
        XGv = XG.rearrange("(j s p) d -> j p s d", s=2, p=128)
        YGv = YG.rearrange("(j s p) d -> j p s d", s=2, p=128)
        evs = {"c": 0}

        def e4_load(j):
            w2 = j % 2
            for (dst, src, bw) in ((wg_t[w2], w_eg, Bweg[w2]), (wu_t[w2], w_eu, Bweu[w2]), (wd_t[w2], w_ed, Bwed[w2])):
                P.add("pool", lambda e, dst=dst, src=src, j=j: e.indirect_dma_start(
                    out=dst.rearrange("p k n -> p (k n)"), out_offset=None, in_=src[:, :], in_offset=bass.IndirectOffsetOnAxis(ap=idxw[:, j:j + 1], axis=0),
                    bounds_check=P.regs[NE * 128 - 1], oob_is_err=False), [Bidxw], [bw], dma=True)
            P.dma(xg_sb[w2], XGv[j], writes=[Bxg[w2]])

        def e4_T(j):
            w2 = j % 2
            for s_ in range(2):
                for half in range(2):
                    bank = 6 + (s_ * 2 + half) % 2
                    for kk in range(8):
                        k = half * 8 + kk
                        P.add("pe", lambda e, bank=bank, kk=kk, k=k, w2=w2, s_=s_: e.transpose(
                            out=PSbf[bank][:, kk * 128:(kk + 1) * 128], in_=xg_sb[w2][:, s_, k * 128:(k + 1) * 128], identity=ident),
                            [Bxg[w2], Bc], [PSB[bank]])
                    src_ap = PSbf[bank][:, 0:1024].rearrange("p (k n) -> p k n", k=8)
                    dst_ap = xgT[w2][:, half * 8:(half + 1) * 8, s_ * 128:(s_ + 1) * 128]
                    if evs["c"] % 2 == 0:
                        cp("dve", dst_ap, src_ap, [PSB[bank]], [BxgT[w2]])
                    else:
                        act(dst_ap, src_ap, AF.Copy, [PSB[bank]], [BxgT[w2]])
                    evs["c"] += 1

        def e4_GU(j):
            w2 = j % 2
            for m in range(4):
                bg, bu = (0, 1) if m % 2 == 0 else (2, 3)
                for k in range(16):
                    mm(PS[bg][:, 0:256], wg_t[w2][:, k, m * 128:(m + 1) * 128], xgT[w2][:, k, :], k == 0, k == 15, [Bweg[w2], BxgT[w2]], [PSB[bg]])
                for k in range(16):
                    mm(PS[bu][:, 0:256], wu_t[w2][:, k, m * 128:(m + 1) * 128], xgT[w2][:, k, :], k == 0, k == 15, [Bweu[w2], BxgT[w2]], [PSB[bu]])
                s2 = m % 2
                act(sil[s2], PS[bg][:, 0:256], AF.Silu, [PSB[bg]], [Bsil[s2]])
                tt("dve", aT[w2][:, m, :], PS[bu][:, 0:256], sil[s2], ALU.mult, [PSB[bu], Bsil[s2]], [BaT[w2]])

        def e4_D(j):
            w2 = j % 2
            for s_ in range(2):
                for n in range(4):
                    bk = 4 + (s_ * 4 + n) % 2
                    for k in range(4):
                        mm(PS[bk], aT[w2][:, k, s_ * 128:(s_ + 1) * 128], wd_t[w2][:, k, n * 512:(n + 1) * 512], k == 0, k == 3,
                           [BaT[w2], Bwed[w2]], [PSB[bk]])
                    dst_ap = yg_sb[w2][:, s_, n * 512:(n + 1) * 512]
                    if n % 2 == 0:
                        cp("dve", dst_ap, PS[bk], [PSB[bk]], [Byg[w2]])
                    else:
                        act(dst_ap, PS[bk], AF.Copy, [PSB[bk]], [Byg[w2]])
            P.dma(YGv[j], yg_sb[w2], reads=[Byg[w2]])

        e4_load(0)
        e4_load(1)
        e4_T(0)
        e4_GU(0)
        for j in range(NBLK):
            if j + 1 < NBLK:
                e4_T(j + 1)
            e4_D(j)
            if j + 2 < NBLK:
                e4_load(j + 2)
            if j + 1 < NBLK:
                e4_GU(j + 1)
        P.barrier()
        A.release(mE4)

        acc = A.alloc([4, D], F32)
        Bacc = [Buf("acc%d" % i) for i in range(4)]
        sho_t = [A.alloc([D], BF16) for _ in range(2)]
        Bsho_t = [Buf("shot0"), Buf("shot1")]
        NYK = 12
        yk = [A.alloc([D], BF16) for _ in range(NYK)]
        Byk = [Buf("yk%d" % i) for i in range(NYK)]
        x1t = [A.alloc([D], F32) for _ in range(2)]
        Bx1 = [Buf("x1_0"), Buf("x1_1")]
        fo = [A.alloc([D], F32) for _ in range(2)]
        Bfo = [Buf("fo0"), Buf("fo1")]
        junk3 = A.alloc([D], BF16)
        Bjunk3 = Buf("junk3")
        stat3 = A.alloc([16, 4], F32)
        Bst3 = [Buf("st3_%d" % i) for i in range(16)]
        def e3_gather(tile):
            i2 = tile % 2
            tok = slice(tile * 128, (tile + 1) * 128)
            P.dma(x1t[i2], X1[tok, :], writes=[Bx1[i2]])
            P.dma(sho_t[i2], SHO[tok, :], writes=[Bsho_t[i2]])
            for k in range(6):
                y3 = (tile * 6 + k) % NYK
                P.add("pool", lambda e, tile=tile, k=k, y3=y3: e.indirect_dma_start(
                    out=yk[y3], out_offset=None, in_=YG[:, :], in_offset=bass.IndirectOffsetOnAxis(ap=dest_i[:, tile, k:k + 1], axis=0),
                    bounds_check=P.regs[NSLOT - 1], oob_is_err=False), [Bdest[tile]], [Byk[y3]], dma=True)

        def e3_combine(tile):
            tt_ = tile % 4
            i2 = tile % 2
            tok = slice(tile * 128, (tile + 1) * 128)
            for k in range(6):
                y3 = (tile * 6 + k) % NYK
                if k == 0:
                    stt("dve", acc[:, tt_, :], yk[y3], wk[:, tile, k:k + 1], sho_t[i2], ALU.mult, ALU.add,
                        [Byk[y3], Bdest[tile], Bsho_t[i2]], [Bacc[tt_]])
                else:
                    stt("dve", acc[:, tt_, :], yk[y3], wk[:, tile, k:k + 1], acc[:, tt_, :], ALU.mult, ALU.add,
                        [Byk[y3], Bdest[tile], Bacc[tt_]], [Bacc[tt_]])
            act(junk3, acc[:, tt_, :], AF.Square, [Bacc[tt_]], [Bjunk3, Bst3[tile]], accum_out=stat3[:, tile, 0:1])
            rsqrt_chain(stat3[:, tile, 1:2], stat3[:, tile, 0:1], 1.0 / D, [Bst3[tile]], [Bst3[tile]], stat3[:, tile, 2:3])
            stt("dve", fo[i2], acc[:, tt_, :], stat3[:, tile, 1:2], Gab[:, 1, :], ALU.mult, ALU.mult,
                [Bacc[tt_], Bst3[tile], Bg], [Bfo[i2]])
            tt("pool", fo[i2], fo[i2], x1t[i2], ALU.add, [Bfo[i2], Bx1[i2]], [Bfo[i2]])
            P.dma(out_d[tok, :], fo[i2], reads=[Bfo[i2]])

        e3_gather(0)
        for tile in range(16):
            if tile + 1 < 16:
                e3_gather(tile + 1)
            e3_combine(tile)
        A.release(mE)
        stats = P.emit()
    return nc, stats


def _consts():
    cb = np.zeros((128, 640), np.float32)
    cb[:, 512:640] = np.triu(np.ones((128, 128), np.float32), 1)
    cb[:, 0:128] = np.eye(128)
    cb[:, 128:256] = 1.0
    for m in range(64):
        cb[(m + 32) % 64, 256 + m] = 1.0
    for m in range(32):
        cb[(m + 16) % 32, 320 + m] = 1.0
    cf = np.zeros((128, 128), np.float32)
    cf[:, 8:120] = np.arange(112, dtype=np.float32)[None, :]
    cf[:, 120] = np.arange(128, dtype=np.float32)
    fm = (1.0 / (500000.0 ** (np.arange(0, 64, 2, dtype=np.float32) / 64))).astype(np.float32)
    fd = (1.0 / (500000.0 ** (np.arange(0, 32, 2, dtype=np.float32) / 32))).astype(np.float32)
    cf[0:32, 0] = fm
    cf[32:64, 0] = fm
    cf[0:32, 1] = -1.0
    cf[32:64, 1] = 1.0
    cf[0:16, 2] = fd
    cf[16:32, 2] = fd
    cf[0:16, 3] = -1.0
    cf[16:32, 3] = 1.0
    return cb.astype(ml_dtypes.bfloat16), cf


def _elayout(w):
    e, r, n = w.shape
    return np.ascontiguousarray(w.reshape(e, r // 128, 128, n).transpose(0, 2, 1, 3)).reshape(e * 128, (r // 128) * n)


def make_in_maps(x, c, positions, w_ada, b_ada, attn_pre_g, w_in, q_a_norm_g, w_q_up, kv_a_norm_g,
                 w_kv_up, w_mla_o, w_dil_o, w_out, attn_post_g, ffn_pre_g, w_router, router_bias,
                 w_exp_gate, w_exp_up, w_exp_down, w_sh_gate, w_sh_up, w_sh_down, ffn_post_g):
    f = lambda a: np.ascontiguousarray(np.asarray(a, dtype=np.float32))
    x = f(x)
    c = f(c)
    positions = np.asarray(positions).astype(np.int32)
    cb, cf = _consts()
    shared = {
        "cst_bf": cb, "cst_f": cf,
        "w_ada": f(w_ada)[0], "w_in": f(w_in)[0], "w_q_up": f(w_q_up)[0], "w_kv_up": f(w_kv_up)[0],
        "w_mla_o": f(w_mla_o)[0], "w_dil_o": f(w_dil_o)[0], "w_out": f(w_out)[0], "w_router": f(w_router)[0],
        "w_exp_gate": _elayout(f(w_exp_gate)[0]), "w_exp_up": _elayout(f(w_exp_up)[0]), "w_exp_down": _elayout(f(w_exp_down)[0]),
        "w_sh_gate": f(w_sh_gate)[0], "w_sh_up": f(w_sh_up)[0], "w_sh_down": f(w_sh_down)[0],
    }
    b_ada = f(b_ada)[0]

    def colform(v):
        return np.ascontiguousarray(v.reshape(-1, 128).T)

    rowv = np.concatenate([b_ada[2 * D:3 * D], b_ada[5 * D:6 * D], f(attn_post_g)[0], f(ffn_post_g)[0], f(router_bias)[0],
                           b_ada[3 * D:4 * D], b_ada[4 * D:5 * D], f(ffn_pre_g)[0]])[None, :]
    mask_cache = {}
    in_maps = []
    for core in range(8):
        b, p = core // 2, core % 2
        xb = x[b].reshape(32, 128, D)
        pb = positions[b].reshape(32, 128)
        colv = np.zeros((128, 128), np.float32)
        colv[:, 0:16] = colform(c[b])
        colv[:, 16:32] = colform(b_ada[0:D])
        colv[:, 32:48] = colform(b_ada[D:2 * D])
        colv[:, 48:64] = colform(b_ada[3 * D:4 * D])
        colv[:, 64:80] = colform(b_ada[4 * D:5 * D])
        colv[:, 80:96] = colform(f(attn_pre_g)[0])
        colv[:, 96:112] = colform(f(ffn_pre_g)[0])
        colv[:, 112:116] = colform(f(q_a_norm_g)[0])
        colv[:, 116:120] = colform(f(kv_a_norm_g)[0])
        if p not in mask_cache:
            m, _ = mask_table(p)
            mask_cache[p] = np.ascontiguousarray(m.transpose(1, 0, 2)).astype(ml_dtypes.bfloat16)
        d = dict(shared)
        d.update({
            "x_own": np.ascontiguousarray(xb[p::2].reshape(NT, D)),
            "x_oth": np.ascontiguousarray(xb[1 - p::2].reshape(NT, D)),
            "pos": np.ascontiguousarray(np.stack([pb[p::2].reshape(NT), pb[1 - p::2].reshape(NT)])),
            "colv": colv, "rowv": np.ascontiguousarray(rowv), "masks": mask_cache[p],
        })
        in_maps.append(d)
    return in_maps


_NC = None


def kernel(**inputs):
    global _NC
    if _NC is None:
        _NC = build()[0]
    in_maps = make_in_maps(**inputs)
    res = run_bass_kernel_spmd(_NC, in_maps, core_ids=list(range(8)))
    out = np.zeros((4, 32, 128, D), np.float32)
    for core in range(8):
        b, p = core // 2, core % 2
        out[b, p::2] = np.asarray(res.results[core]["out"], dtype=np.float32).reshape(16, 128, D)
    return out.reshape(4, S, D)
```

```python
import math
import numpy as np
import ml_dtypes
from contextlib import ExitStack
import concourse.bass as bass
import concourse.mybir as mybir
from concourse.bass_utils import run_bass_kernel_spmd

F32 = mybir.dt.float32
BF16 = mybir.dt.bfloat16
I32 = mybir.dt.int32
U8 = mybir.dt.uint8
ALU = mybir.AluOpType
AF = mybir.ActivationFunctionType

SAME_ENGINE_SYNC = True

D = 2048
S = 4096
NT = 2048
NE = 64
IN_COLS = 9792
C_QA, C_CKV, C_KR, C_DQ, C_DK, C_DV, C_GA, C_GB = 0, 512, 1024, 1088, 2624, 4160, 5696, 7744
EPS = 1e-6
PI = math.pi
DIL = ((128, 1), (512, 4), (2048, 16))


class Buf:
    __slots__ = ("name", "w", "r", "excl")

    def __init__(self, name="", excl=False):
        self.name = name
        self.w = None
        self.r = {}
        self.excl = excl


class Op:
    __slots__ = ("eng", "fn", "deps", "signal", "semval", "sem", "is_dma", "guard", "idx")

    def __init__(self, eng, fn, is_dma):
        self.eng = eng
        self.fn = fn
        self.deps = []
        self.signal = False
        self.semval = None
        self.sem = None
        self.is_dma = is_dma
        self.guard = None


class Prog:
    ENGS = ("pe", "act", "dve", "pool", "sp")

    def __init__(self, nc, stack):
        self.nc = nc
        self.ops = {e: [] for e in self.ENGS}
        self.last = {e: None for e in self.ENGS}
        self.barrier_deps = []
        self.dma_since_barrier = []
        self.reg_requests = []
        self.regs = {}
        n_dma_sems = {"sp": 24, "pool": 12, "act": 4}
        self.csem = {}
        for e in ("pe", "act", "dve", "pool"):
            self.csem[e] = stack.enter_context(nc.semaphore("c_" + e))
        self.dsem, self.dsem_last, self.dsem_cnt, self.dsem_rr = {}, {}, {}, {}
        for e, n in n_dma_sems.items():
            self.dsem[e] = [stack.enter_context(nc.semaphore("d_%s%d" % (e, i))) for i in range(n)]
            self.dsem_last[e] = [None] * n
            self.dsem_cnt[e] = [0] * n
            self.dsem_rr[e] = 0

    def add(self, eng, fn, reads=(), writes=(), dma=False):
        op = Op(eng, fn, dma)
        op.idx = len(self.ops[eng])
        best = {}
        dmas = {}
        ex = [b for b in reads if b.excl]
        if ex:
            reads = [b for b in reads if not b.excl]
            writes = list(writes) + [b for b in ex if b not in writes]

        def adddep(d):
            if d is None:
                return
            if d.is_dma:
                dmas[id(d)] = d
            else:
                b = best.get(d.eng)
                if b is None or d.idx > b.idx:
                    best[d.eng] = d

        for b in reads:
            adddep(b.w)
        for b in writes:
            adddep(b.w)
            for r in b.r.values():
                adddep(r)
        for d in self.barrier_deps:
            adddep(d)
        fdeps = []
        for d in list(best.values()) + list(dmas.values()):
            if not d.is_dma and d.eng == eng and not dma:
                if eng == "pe" or not SAME_ENGINE_SYNC:
                    continue
            fdeps.append(d)
            d.signal = True
        op.deps = fdeps
        for b in reads:
            b.r[("d", id(op)) if dma else eng] = op
        for b in writes:
            b.w = op
            b.r = {}
        if dma:
            i = self.dsem_rr[eng]
            n = len(self.dsem[eng])
            self.dsem_rr[eng] = (i + 1) % n
            op.sem = self.dsem[eng][i]
            op.guard = self.dsem_last[eng][i]
            self.dsem_cnt[eng][i] += 1
            op.semval = 16 * self.dsem_cnt[eng][i]
            self.dsem_last[eng][i] = op
            op.signal = True
            self.dma_since_barrier.append(op)
        else:
            self.last[eng] = op
        self.ops[eng].append(op)
        return op

    def barrier(self):
        deps = [o for o in self.last.values() if o is not None] + list(self.dma_since_barrier)
        for e in self.dsem:
            for o in self.dsem_last[e]:
                if o is not None and o not in deps:
                    deps.append(o)
        self.barrier_deps = deps
        self.dma_since_barrier = []

    def dma(self, out, in_, reads=(), writes=(), eng="sp"):
        return self.add(eng, lambda e: e.dma_start(out=out, in_=in_), reads, writes, dma=True)

    def emit(self):
        nc = self.nc
        final_deps = [o for o in self.last.values() if o is not None]
        for e in self.dsem:
            for o in self.dsem_last[e]:
                if o is not None:
                    final_deps.append(o)
        for d in final_deps:
            d.signal = True
        for e in ("pe", "act", "dve", "pool"):
            cnt = 0
            for op in self.ops[e]:
                if op.is_dma:
                    continue
                if op.signal:
                    cnt += 1
                    op.semval = cnt
                    op.sem = self.csem[e]
        stats = {}

        def run_engine(ename, eh, final=False):
            waited = {}
            nw = 0

            def wait_for(d):
                nonlocal nw
                key = id(d.sem)
                if waited.get(key, 0) >= d.semval:
                    return
                waited[key] = d.semval
                eh.wait_ge(d.sem, d.semval)
                nw += 1

            if ename == "pool":
                for v in self.reg_requests:
                    self.regs[v] = eh.to_reg(v)
            for op in self.ops[ename]:
                for d in op.deps:
                    wait_for(d)
                if op.guard is not None:
                    wait_for(op.guard)
                ins = op.fn(eh)
                if op.signal:
                    ins.then_inc(op.sem, 16 if op.is_dma else 1)
            if final:
                for d in final_deps:
                    wait_for(d)
            stats[ename] = (len(self.ops[ename]), nw)

        with nc.Block() as block:
            @block.tensor
            def _(t):
                run_engine("pe", t)

            @block.scalar
            def _(s):
                run_engine("act", s)

            @block.vector
            def _(v):
                run_engine("dve", v)

            @block.gpsimd
            def _(g):
                run_engine("pool", g)

            @block.sync
            def _(s):
                run_engine("sp", s, final=True)
        return stats


class StopBuild(Exception):
    pass


class Arena:
    def __init__(self, ap, size):
        self.ap = ap
        self.size = size
        self.off = 0

    def alloc(self, free_shape, dtype, parts=128):
        esz = {F32: 4, BF16: 2, I32: 4, U8: 1}[dtype]
        n = esz
        for s in free_shape:
            n *= s
        off = (self.off + 63) // 64 * 64
        assert off + n <= self.size, ("arena overflow", off, n, self.size)
        self.off = off + n
        a = self.ap[0:parts, off:off + n].bitcast(dtype)
        if len(free_shape) == 2:
            a = a.rearrange("p (a b) -> p a b", a=free_shape[0])
        elif len(free_shape) == 3:
            a = a.rearrange("p (a b c) -> p a b c", a=free_shape[0], b=free_shape[1])
        return a

    def mark(self):
        return self.off

    def release(self, m):
        self.off = m


def mask_table(p):
    masks = []
    idx = {}
    ki = np.arange(128)[:, None]
    qi = np.arange(128)[None, :]

    def add(key, fn):
        m = np.zeros((128, 512), np.float32)
        for a in range(4):
            m[:, a * 128:(a + 1) * 128] = fn(a)
        idx[key] = len(masks)
        masks.append(m)

    for c in range(4):
        add(("m", "own", c), lambda a, c=c: (np.ones((128, 128)) if a > c else ((ki <= qi) if a == c else np.zeros((128, 128)))))
        add(("m", "oth", c), lambda a, c=c: (np.ones((128, 128)) if a > c else (np.full((128, 128), float(p)) if a == c else np.zeros((128, 128)))))
    def dmask(pp, kind, rho, w, d):
        off = 0 if kind == "own" else 128 * (2 * pp - 1)
        ms = []
        for a in range(4):
            delta = 256 * (a + rho) + off + qi - ki
            ms.append(((delta >= 0) & (delta <= w) & (delta % d == 0)).astype(np.float32))
        return np.concatenate(ms, axis=1)

    for g, (w, d) in enumerate(DIL):
        for kind in ("own", "oth"):
            for rho in range(-3, 10):
                m0, m1 = dmask(0, kind, rho, w, d), dmask(1, kind, rho, w, d)
                if m0.any() or m1.any():
                    idx[("d", g, kind, rho)] = len(masks)
                    masks.append(m1 if p == 1 else m0)
    return np.stack(masks), idx


def build(debug=False, stop_after=None):
    nc = bass.Bass("TRN2", target_bir_lowering=False)
    _, midx = mask_table(0)
    _, midx1 = mask_table(1)
    assert midx == midx1
    NM = len(midx)

    def din(name, shape, dt=F32):
        return nc.dram_tensor(name, list(shape), dt, kind="ExternalInput").ap()

    def dscr(name, shape, dt):
        return nc.dram_tensor(name, list(shape), dt, kind=("ExternalOutput" if debug else "Internal")).ap()

    x_own = din("x_own", [NT, D])
    x_oth = din("x_oth", [NT, D])
    pos_in = din("pos", [2, NT], I32)
    colv = din("colv", [128, 128])
    rowv = din("rowv", [1, 7 * D + 64])
    cst_bf = din("cst_bf", [128, 640], BF16)
    cst_f = din("cst_f", [128, 128])
    masks_in = din("masks", [128, NM, 512], BF16)
    w_ada = din("w_ada", [D, 6 * D])
    w_in = din("w_in", [D, IN_COLS])
    w_q_up = din("w_q_up", [512, 1536])
    w_kv_up = din("w_kv_up", [512, 2048])
    w_mla_o = din("w_mla_o", [1024, D])
    w_dil_o = din("w_dil_o", [512, D])
    w_out = din("w_out", [D, D])
    w_router = din("w_router", [D, NE])
    w_eg = din("w_exp_gate", [NE * 128, 8192])
    w_eu = din("w_exp_up", [NE * 128, 8192])
    w_ed = din("w_exp_down", [NE * 128, 8192])
    w_sg = din("w_sh_gate", [D, 512])
    w_su = din("w_sh_up", [D, 512])
    w_sd = din("w_sh_down", [512, D])
    out_d = nc.dram_tensor("out", [NT, D], F32, kind="ExternalOutput").ap()

    QT_m = dscr("QT_m", [8, 192, NT], BF16)
    KT_m = dscr("KT_m", [8, 128, S], BF16)
    KRT = dscr("KRT", [64, S], BF16)
    V_m = dscr("V_m", [8, 128, 32, 128], BF16)
    QT_d = dscr("QT_d", [12, 128, NT], BF16)
    KT_d = dscr("KT_d", [12, 128, S], BF16)
    V_d = dscr("V_d", [12, 128, 32, 128], BF16)
    SG = dscr("SG", [32, 128, NT], BF16)
    OT_m = dscr("OT_m", [8, 128, NT], BF16)
    OT_d = dscr("OT_d", [4, 128, NT], BF16)
    X1 = dscr("X1", [NT, D], F32)
    H2T = dscr("H2T", [128, 16, NT], BF16)
    H2K = dscr("H2K", [NT, D], BF16)
    SHO = dscr("SHO", [NT, D], BF16)
    NBLK = 96
    NSLOT = NBLK * 256
    XG = dscr("XG", [NSLOT, D], BF16)
    YG = dscr("YG", [NSLOT, D], BF16)

    st = ExitStack()
    with st:
        P = Prog(nc, st)
        P.reg_requests = [96 * 256 - 1, NE * 128 - 1]
        ASZ = 206 * 1024
        arena_t = st.enter_context(nc.sbuf_tensor("arena", [128, ASZ], U8))
        A = Arena(arena_t[:, :], ASZ)
        PS = [st.enter_context(nc.psum_tensor("ps%d" % i, [128, 512], F32))[:] for i in range(8)]
        PSB = [Buf("ps%d" % i, excl=True) for i in range(8)]
        PSbf = [p.bitcast(BF16) for p in PS]

        cbf = A.alloc([640], BF16)
        Utri = cbf[:, 512:640]
        ident = cbf[:, 0:128]
        ones = cbf[:, 128:256]
        perm64 = cbf[0:64, 256:320]
        perm32 = cbf[0:32, 320:352]
        cf = A.alloc([128], F32)
        colv_sb = A.alloc([128], F32)
        rbias = A.alloc([NE], F32)
        AB = A.alloc([4, 16], F32)
        Gab = A.alloc([2, D], F32)
        Bc = Buf("consts")
        Bab = Buf("AB")
        Bg = Buf("G")
        P.dma(cbf, cst_bf, writes=[Bc])
        P.dma(cf, cst_f, writes=[Bc])
        P.dma(colv_sb, colv, writes=[Bc])
        P.dma(rbias, rowv[:, 4 * D:4 * D + NE].partition_broadcast(128), writes=[Bc])
        qg = colv_sb[:, 112:116]
        kvg = colv_sb[:, 116:120]

        def act(out, in_, func, reads, writes, **kw):
            return P.add("act", lambda e: e.activation(out=out, in_=in_, func=func, **kw), reads, writes)

        def tt(eng, out, in0, in1, op, reads, writes):
            return P.add(eng, lambda e: e.tensor_tensor(out=out, in0=in0, in1=in1, op=op), reads, writes)

        def ts(eng, out, in0, s1, s2, op0, op1, reads, writes):
            return P.add(eng, lambda e: e.tensor_scalar(out=out, in0=in0, scalar1=s1, scalar2=s2, op0=op0, op1=op1), reads, writes)

        def stt(eng, out, in0, scalar, in1, op0, op1, reads, writes):
            return P.add(eng, lambda e: e.scalar_tensor_tensor(out=out, in0=in0, scalar=scalar, in1=in1, op0=op0, op1=op1), reads, writes)

        def cp(eng, out, in_, reads, writes):
            return P.add(eng, lambda e: e.tensor_copy(out=out, in_=in_), reads, writes)

        def mm(out, lhsT, rhs, start, stop, reads, writes):
            return P.add("pe", lambda e: e.matmul(out, lhsT=lhsT, rhs=rhs, start=start, stop=stop), reads, writes)

        def rsqrt_chain(dst, src, scale, reads, writes, tmp):
            ts("dve", tmp, src, scale, EPS, ALU.mult, ALU.add, reads, writes)
            act(tmp, tmp, AF.Sqrt, writes, writes)
            P.add("dve", lambda e: e.reciprocal(out=dst, in_=tmp), writes, writes)

        mA = A.mark()
        zt = A.alloc([8192], BF16)
        Bz = Buf("zero")
        P.add("pool", lambda e: e.memset(zt, 0.0), [], [Bz])
        XGz = XG.rearrange("(c p r) d -> c p (r d)", p=128, r=4)
        for c_ in range(NSLOT // 512):
            P.dma(XGz[c_], zt, reads=[Bz])
        rows_sb = A.alloc([4 * D], F32)
        Brows = Buf("rows")
        P.dma(rows_sb, rowv[:, 0:4 * D].partition_broadcast(128), writes=[Brows])
        sc = A.alloc([16], BF16)
        scB = A.alloc([16, 128], BF16)
        Bsc = Buf("sc")
        act(sc, colv_sb[:, 0:16], AF.Silu, [Bc], [Bsc])
        for k in range(16):
            cp("dve", scB[:, k, :], sc[:, k:k + 1].to_broadcast([128, 128]), [Bsc], [Bsc])
        wada_v = w_ada.rearrange("(k p) n -> p k n", p=128)
        wa_t = [A.alloc([16, 512], BF16) for _ in range(2)]
        wa_b = [Buf("wa0"), Buf("wa1")]
        modT = A.alloc([64], F32)
        col_segs = [0, 1]
        gi = 0
        for si, seg in enumerate(col_segs):
            for n in range(4):
                wt, wb = wa_t[gi % 2], wa_b[gi % 2]
                gi += 1
                c0 = seg * D + n * 512
                P.dma(wt, wada_v[:, :, c0:c0 + 512], writes=[wb], eng="pool")
                for m in range(4):
                    col = si * 16 + n * 4 + m
                    for k in range(16):
                        mm(PS[0][:, col:col + 1], wt[:, k, m * 128:(m + 1) * 128], sc[:, k:k + 1], k == 0, k == 15,
                           [wb, Bsc], [PSB[0]])
        cp("dve", modT[:, 0:32], PS[0][:, 0:32], [PSB[0]], [Bab])
        tt("dve", modT[:, 0:32], modT[:, 0:32], colv_sb[:, 16:48], ALU.add, [Bab, Bc], [Bab])
        stt("dve", AB[:, 0, :], modT[:, 16:32], 1.0, colv_sb[:, 80:96], ALU.add, ALU.mult, [Bab, Bc], [Bab])
        cp("dve", AB[:, 1, :], modT[:, 0:16], [Bab], [Bab])
        for gsel, seg in enumerate([2, 5]):
            for n in range(4):
                wt, wb = wa_t[gi % 2], wa_b[gi % 2]
                gi += 1
                c0 = seg * D + n * 512
                P.dma(wt, wada_v[:, :, c0:c0 + 512], writes=[wb], eng="pool")
                bank = 1 + (n % 2)
                for k in range(16):
                    mm(PS[bank], scB[:, k, :], wt[:, k, :], k == 0, k == 15, [wb, Bsc], [PSB[bank]])
                dst = Gab[:, gsel, n * 512:(n + 1) * 512]
                tt("dve", dst, PS[bank], rows_sb[:, gsel * D + n * 512: gsel * D + (n + 1) * 512], ALU.add, [PSB[bank], Brows], [Bg])
                tt("dve", dst, dst, rows_sb[:, (2 + gsel) * D + n * 512:(2 + gsel) * D + (n + 1) * 512], ALU.mult, [Bg, Brows], [Bg])
        P.barrier()
        A.release(mA)
        if debug:
            dAB = nc.dram_tensor("dbgAB", [128, 64], F32, kind="ExternalOutput").ap()
            dG = nc.dram_tensor("dbgG", [128, 2 * D], F32, kind="ExternalOutput").ap()
            P.dma(dAB, AB, reads=[Bab])
            P.dma(dG, Gab, reads=[Bg])
        if stop_after == "A":
            return nc, P.emit()

        cosM = A.alloc([NT], BF16, parts=64)
        sinM = A.alloc([NT], BF16, parts=64)
        cosD = A.alloc([NT], BF16, parts=32)
        sinD = A.alloc([NT], BF16, parts=32)
        Brope = Buf("rope")

        hT = A.alloc([16, NT], BF16)
        BhT = [Buf("hT%d" % i) for i in range(16)]
        wq_sb = A.alloc([4, 1536], BF16)
        wkv_sb = A.alloc([4, 2048], BF16)
        Bwq, Bwkv = Buf("wq"), Buf("wkv")
        P.dma(wq_sb, w_q_up.rearrange("(k p) n -> p k n", p=128), writes=[Bwq], eng="pool")
        P.dma(wkv_sb, w_kv_up.rearrange("(k p) n -> p k n", p=128), writes=[Bwkv], eng="pool")
        stat = A.alloc([32, 4], F32)
        Bstat = [Buf("stat%d" % i) for i in range(32)]
        mBov = A.mark()
        xt = [A.alloc([D], F32) for _ in range(2)]
        Bxt = [Buf("xt0"), Buf("xt1")]
        xn = [A.alloc([D], BF16) for _ in range(2)]
        Bxn = [Buf("xn0"), Buf("xn1")]
        junk = A.alloc([D], BF16)
        Bjunk = Buf("junk")
        evt = [A.alloc([8, 128], F32) for _ in range(2)]
        Bevt = [Buf("evt0"), Buf("evt1")]
        posi = A.alloc([NT], I32, parts=64)
        posf = A.alloc([NT], F32, parts=64)
        ang = A.alloc([NT], F32, parts=64)
        posf2 = A.alloc([NT], F32, parts=64)
        Bpos = Buf("pos")
        A.release(mBov)
        wt_t = [A.alloc([16, 512], BF16) for _ in range(2)]
        wt_b = [Buf("wt0"), Buf("wt1")]
        lat = A.alloc([4, NT], BF16)
        Blat = [Buf("lat%d" % i) for i in range(4)]
        sq = [A.alloc([512], BF16) for _ in range(2)]
        Bsq = [Buf("sq0"), Buf("sq1")]
        rbc = A.alloc([512], F32)
        rtmp = A.alloc([512], F32)
        Brbc = Buf("rbc")
        NSTG = 6
        stg = [A.alloc([512], BF16) for _ in range(NSTG)]
        Bstg = [Buf("stg%d" % i) for i in range(NSTG)]
        rt = [A.alloc([512], F32) for _ in range(2)]
        Brt = [Buf("rt0"), Buf("rt1")]
        w_in_v = w_in.rearrange("(k p) n -> p k n", p=128)
        state = {"stg": 0, "wt": 0, "bank": 0, "rt": 0, "sq": 0, "ck": 0}

        def checkpoint():
            state["ck"] += 1
            if stop_after == "B2:%d" % state["ck"]:
                raise StopBuild()

        def next_stg():
            i = state["stg"]
            state["stg"] = (i + 1) % NSTG
            return stg[i], Bstg[i]

        def next_bank(lo=0, hi=6):
            i = state["bank"]
            state["bank"] = i + 1
            b = lo + i % (hi - lo)
            return PS[b], PSB[b], b

        def load_w(c0, ncols):
            i = state["wt"]
            state["wt"] = i + 1
            wt, wb = wt_t[i % 2], wt_b[i % 2]
            P.dma(wt[:, :, 0:ncols], w_in_v[:, :, c0:c0 + ncols], writes=[wb], eng="pool")
            return wt, wb

        def rope_rows(tile, Btile, rows, perm, ctab, stab, tok0):
            ps, pb, _ = next_bank(6, 8)
            mm(ps[0:rows, :], perm, tile[0:rows, :], True, True, [Btile, Bc], [pb])
            i = state["rt"]
            state["rt"] = i + 1
            r1, B1_ = rt[i % 2], Brt[i % 2]
            tt("dve", r1[0:rows, :], ps[0:rows, :], stab[0:rows, tok0:tok0 + 512], ALU.mult, [pb, Brope], [B1_])
            tt("dve", tile[0:rows, :], tile[0:rows, :], ctab[0:rows, tok0:tok0 + 512], ALU.mult, [Btile, Brope], [Btile])
            tt("dve", tile[0:rows, :], tile[0:rows, :], r1[0:rows, :], ALU.add, [Btile, B1_], [Btile])

        try:
          for tsi, xsrc in enumerate((x_own, x_oth)):
            own = tsi == 0
            tokbase = tsi * NT
            P.barrier()
            P.dma(posi, pos_in[tsi:tsi + 1, :].partition_broadcast(64), writes=[Bpos])
            cp("dve", posf, posi, [Bpos], [Bpos])
            for (rows, fcol, scol, ctab, stab) in ((64, 0, 1, cosM, sinM), (32, 2, 3, cosD, sinD)):
                for (shift, tab, signed) in ((0.0, stab, True), (0.5 * PI, ctab, False)):
                    a = ang[0:rows, :]
                    kf = posf2[0:rows, :]
                    ts("dve", a, posf[0:rows, :], cf[0:rows, fcol:fcol + 1], shift, ALU.mult, ALU.add, [Bpos, Bc], [Bpos])
                    ts("dve", kf, a, 1.0 / (2 * PI), 0.0, ALU.mult, ALU.add, [Bpos], [Bpos])
                    cp("dve", posi[0:rows, :], kf, [Bpos], [Bpos])
                    cp("dve", kf, posi[0:rows, :], [Bpos], [Bpos])
                    stt("dve", a, kf, -2 * PI, a, ALU.mult, ALU.add, [Bpos], [Bpos])
                    ts("dve", kf, a, PI, -2 * PI, ALU.is_gt, ALU.mult, [Bpos], [Bpos])
                    tt("dve", a, a, kf, ALU.add, [Bpos], [Bpos])
                    ts("dve", kf, a, -PI, 2 * PI, ALU.is_lt, ALU.mult, [Bpos], [Bpos])
                    tt("dve", a, a, kf, ALU.add, [Bpos], [Bpos])
                    ts("dve", a, a, 3.141592, -3.141592, ALU.min, ALU.max, [Bpos], [Bpos])
                    act(a, a, AF.Sin, [Bpos], [Bpos])
                    dst = tab[0:rows, :]
                    if signed:
                        ts("dve", dst, a, cf[0:rows, scol:scol + 1], 0.0, ALU.mult, ALU.add, [Bpos, Bc], [Brope])
                    else:
                        cp("dve", dst, a, [Bpos], [Brope])
            for ti in range(16):
                i2 = ti % 2
                sidx = tsi * 16 + ti
                P.dma(xt[i2], xsrc[ti * 128:(ti + 1) * 128, :], writes=[Bxt[i2]])
                act(junk, xt[i2], AF.Square, [Bxt[i2]], [Bjunk, Bstat[sidx]], accum_out=stat[:, sidx, 0:1])
                rsqrt_chain(stat[:, sidx, 1:2], stat[:, sidx, 0:1], 1.0 / D, [Bstat[sidx]], [Bstat[sidx]], stat[:, sidx, 2:3])
                act(xn[i2], xt[i2], AF.Copy, [Bxt[i2], Bstat[sidx]], [Bxn[i2]], scale=stat[:, sidx, 1:2])
                for half in range(2):
                    bank = 6 + half
                    for kk in range(8):
                        k = half * 8 + kk
                        P.add("pe", lambda e, bank=bank, kk=kk, k=k, i2=i2: e.transpose(
                            out=PSbf[bank][:, kk * 128:(kk + 1) * 128], in_=xn[i2][:, k * 128:(k + 1) * 128], identity=ident),
                            [Bxn[i2], Bc], [PSB[bank]])
                    src = PSbf[bank][:, 0:1024].rearrange("p (k n) -> p k n", k=8)
                    a1 = AB[:, 0, half * 8:(half + 1) * 8].unsqueeze(2).to_broadcast([128, 8, 128])
                    b1 = AB[:, 1, half * 8:(half + 1) * 8].unsqueeze(2).to_broadcast([128, 8, 128])
                    tt("dve", evt[half], src, a1, ALU.mult, [PSB[bank], Bab], [Bevt[half]])
                    tt("dve", hT[:, half * 8:(half + 1) * 8, ti * 128:(ti + 1) * 128], evt[half], b1, ALU.add,
                       [Bevt[half], Bab], [BhT[ti]])

            P.barrier()
            if debug and own:
                dH = nc.dram_tensor("dbgH", [128, 16, NT], BF16, kind="ExternalOutput").ap()
                dR = nc.dram_tensor("dbgR", [64, 4 * NT], BF16, kind="ExternalOutput").ap()
                P.dma(dH, hT, reads=BhT)
                P.dma(dR[:, 0:NT], cosM, reads=[Brope])
                P.dma(dR[:, NT:2 * NT], sinM, reads=[Brope])
                P.dma(dR[0:32, 2 * NT:3 * NT], cosD, reads=[Brope])
                P.dma(dR[0:32, 3 * NT:4 * NT], sinD, reads=[Brope])
            if stop_after == "B1":
                return nc, P.emit()
            def latent_segment(c0, gcol, is_q):
                wt, wb = load_w(c0, 512)
                for tg in range(4):
                    tsl = slice(tg * 512, (tg + 1) * 512)
                    hreads = [BhT[tg * 4 + j] for j in range(4)]
                    for m in range(4):
                        ps, pb, _ = next_bank()
                        for k in range(16):
                            mm(ps, wt[:, k, m * 128:(m + 1) * 128], hT[:, k, tsl], k == 0, k == 15, hreads + [wb], [pb])
                        i = state["sq"]
                        state["sq"] = i + 1
                        s_, bs_ = sq[i % 2], Bsq[i % 2]
                        act(s_, ps, AF.Square, [pb], [bs_])
                        ts("dve", lat[:, m, tsl], ps, gcol[:, m:m + 1], 0.0, ALU.mult, ALU.add, [pb, Bc], [Blat[tg]])
                        mm(PS[6], ones, s_, m == 0, m == 3, [bs_, Bc], [PSB[6]])
                    rsqrt_chain(rbc, PS[6], 1.0 / 512, [PSB[6]], [Brbc], rtmp)
                    for m in range(4):
                        tt("dve", lat[:, m, tsl], lat[:, m, tsl], rbc, ALU.mult, [Blat[tg], Brbc], [Blat[tg]])

            def up_fm(wsb, Bw, col0, M, tg, dst_ap, rope=None):
                tsl = slice(tg * 512, (tg + 1) * 512)
                ps, pb, _ = next_bank()
                for k in range(4):
                    mm(ps[0:M, :], wsb[:, k, col0:col0 + M], lat[:, k, tsl], k == 0, k == 3, [Bw, Blat[tg]], [pb])
                sg_, bsg = next_stg()
                act(sg_[0:M, :], ps[0:M, :], AF.Copy, [pb], [bsg])
                if rope is not None:
                    rope_rows(sg_, bsg, *rope, tg * 512)
                P.dma(dst_ap, sg_[0:M, :], reads=[bsg])

            tcols = slice(tokbase, tokbase + NT)
            if own:
                latent_segment(C_QA, qg, True)
                checkpoint()
                for tg in range(4):
                    c512 = slice(tg * 512, (tg + 1) * 512)
                    for h in range(8):
                        up_fm(wq_sb, Bwq, h * 192, 128, tg, QT_m[h, 0:128, c512])
                        up_fm(wq_sb, Bwq, h * 192 + 128, 64, tg, QT_m[h, 128:192, c512], rope=(64, perm64, cosM, sinM))
            checkpoint()
            latent_segment(C_CKV, kvg, False)
            for tg in range(4):
                c512 = slice(tokbase + tg * 512, tokbase + (tg + 1) * 512)
                for h in range(8):
                    up_fm(wkv_sb, Bwkv, h * 256, 128, tg, KT_m[h, :, c512])
                if tg == 0:
                    checkpoint()
                wkv_h = wkv_sb.rearrange("p k (h c) -> p k h c", c=256)
                for tt_ in range(4):
                    tile = tsi * 16 + tg * 4 + tt_
                    tok = slice(tg * 512 + tt_ * 128, tg * 512 + (tt_ + 1) * 128)
                    for hh in range(2):
                        ps, pb, _ = next_bank()
                        for k in range(4):
                            mm(ps.rearrange("p (h c) -> p h c", h=4), lat[:, k, tok], wkv_h[:, k, hh * 4:(hh + 1) * 4, 128:256],
                               k == 0, k == 3, [Bwkv, Blat[tg]], [pb])
                        sg_, bsg = next_stg()
                        act(sg_, ps, AF.Copy, [pb], [bsg])
                        P.dma(V_m[hh * 4:(hh + 1) * 4, :, tile, :].rearrange("h p d -> p h d"),
                              sg_.rearrange("p (h d) -> p h d", h=4), reads=[bsg])
            checkpoint()
            wt, wb = load_w(C_KR, 64)
            for tg in range(4):
                tsl = slice(tg * 512, (tg + 1) * 512)
                hreads = [BhT[tg * 4 + j] for j in range(4)]
                ps, pb, _ = next_bank()
                for k in range(16):
                    mm(ps[0:64, :], wt[:, k, 0:64], hT[:, k, tsl], k == 0, k == 15, hreads + [wb], [pb])
                sg_, bsg = next_stg()
                act(sg_[0:64, :], ps[0:64, :], AF.Copy, [pb], [bsg])
                rope_rows(sg_, bsg, 64, perm64, cosM, sinM, tg * 512)
                P.dma(KRT[:, tokbase + tg * 512: tokbase + (tg + 1) * 512], sg_[0:64, :], reads=[bsg])

            def dil_fm(cbase, dstT, ncol_tok_base):
                for cg in range(3):
                    wt, wb = load_w(cbase + cg * 512, 512)
                    for tg in range(4):
                        tsl = slice(tg * 512, (tg + 1) * 512)
                        hreads = [BhT[tg * 4 + j] for j in range(4)]
                        for m in range(4):
                            h = cg * 4 + m
                            ps, pb, _ = next_bank()
                            for k in range(16):
                                mm(ps, wt[:, k, m * 128:(m + 1) * 128], hT[:, k, tsl], k == 0, k == 15, hreads + [wb], [pb])
                            sg_, bsg = next_stg()
                            act(sg_, ps, AF.Copy, [pb], [bsg])
                            rope_rows(sg_, bsg, 32, perm32, cosD, sinD, tg * 512)
                            P.dma(dstT[h, :, ncol_tok_base + tg * 512: ncol_tok_base + (tg + 1) * 512], sg_, reads=[bsg])

            checkpoint()
            if own:
                dil_fm(C_DQ, QT_d, 0)
            checkpoint()
            dil_fm(C_DK, KT_d, tokbase)
            checkpoint()
            for cg in range(3):
                wt, wb = load_w(C_DV + cg * 512, 512)
                for ti in range(16):
                    tile = tsi * 16 + ti
                    ps, pb, _ = next_bank()
                    for k in range(16):
                        mm(ps, hT[:, k, ti * 128:(ti + 1) * 128], wt[:, k, :], k == 0, k == 15, [BhT[ti], wb], [pb])
                    sg_, bsg = next_stg()
                    act(sg_, ps, AF.Copy, [pb], [bsg])
                    P.dma(V_d[cg * 4:(cg + 1) * 4, :, tile, :].rearrange("h p d -> p h d"),
                          sg_.rearrange("p (h d) -> p h d", h=4), reads=[bsg])
            checkpoint()
            if own:
                for cg in range(8):
                    wt, wb = load_w(C_GA + cg * 512, 512)
                    for tg in range(4):
                        tsl = slice(tg * 512, (tg + 1) * 512)
                        hreads = [BhT[tg * 4 + j] for j in range(4)]
                        for m in range(4):
                            ps, pb, _ = next_bank()
                            for k in range(16):
                                mm(ps, wt[:, k, m * 128:(m + 1) * 128], hT[:, k, tsl], k == 0, k == 15, hreads + [wb], [pb])
                            sg_, bsg = next_stg()
                            act(sg_, ps, AF.Sigmoid, [pb], [bsg])
                            P.dma(SG[cg * 4 + m, :, tsl], sg_, reads=[bsg])
        except StopBuild:
            return nc, P.emit()
        P.barrier()
        A.release(mA)
        if stop_after == "B":
            return nc, P.emit()

        mC = A.mark()
        masks_sb = A.alloc([NM, 512], BF16)
        Bmask = Buf("masks")
        for i0 in range(0, NM, 16):
            i1 = min(NM, i0 + 16)
            P.dma(masks_sb[:, i0:i1, :], masks_in[:, i0:i1, :], writes=[Bmask])
        krt_sb = A.alloc([S], BF16, parts=64)
        Bkrt = Buf("krt")
        P.dma(krt_sb, KRT, writes=[Bkrt])
        NHB = 4
        hb_k = [A.alloc([S], BF16) for _ in range(NHB)]
        hb_v = [A.alloc([32, 128], BF16) for _ in range(NHB)]
        hb_q = [A.alloc([NT], BF16) for _ in range(NHB)]
        hb_qr = [A.alloc([NT], BF16, parts=64) for _ in range(2)]
        Bhk = [Buf("hk%d" % i) for i in range(NHB)]
        Bhv = [Buf("hv%d" % i) for i in range(NHB)]
        Bhq = [Buf("hq%d" % i) for i in range(NHB)]
        Bqr = [Buf("qr0"), Buf("qr1")]
        NPT = 6
        pT = [A.alloc([512], BF16) for _ in range(NPT)]
        BpT = [Buf("pT%d" % i) for i in range(NPT)]
        rden = A.alloc([512], F32)
        Brden = Buf("rden")
        ost = [A.alloc([512], BF16) for _ in range(2)]
        Bost = [Buf("ost0"), Buf("ost1")]
        cnt = {"pt": 0, "s": 0, "o": 0, "hb": 0}

        tiles = []

        def emit_attention():
            LA = 3
            flat = []
            for ti_, (pre, blocks, scale, dst) in enumerate(tiles):
                for bi in range(len(blocks)):
                    flat.append((ti_, bi))
            ptinfo = {}

            def s_stage(f):
                ti_, bi = flat[f]
                pre, blocks, scale, dst = tiles[ti_]
                if bi == 0:
                    for fn in pre:
                        fn()
                qparts, kparts, v_ap, vbufs, mi = blocks[bi]
                sb = f % 4
                npart = len(qparts)
                for pi in range(npart):
                    qa, qrows, qb = qparts[pi]
                    ka, krows, kb = kparts[pi]
                    mm(PS[sb], ka, qa, pi == 0, pi == npart - 1, qb + kb, [PSB[sb]])
                pt, bpt = pT[f % NPT], BpT[f % NPT]
                act(pt, PS[sb], AF.Exp, [PSB[sb]], [bpt], scale=scale)
                if mi is not None:
                    tt("pool" if f % 3 != 2 else "dve", pt, pt, masks_sb[:, mi, :], ALU.mult, [bpt, Bmask], [bpt])

            def pv_stage(f):
                ti_, bi = flat[f]
                pre, blocks, scale, dst = tiles[ti_]
                nb = len(blocks)
                qparts, kparts, v_ap, vbufs, mi = blocks[bi]
                ob, db = 4 + 2 * (ti_ % 2), 5 + 2 * (ti_ % 2)
                pt, bpt = pT[f % NPT], BpT[f % NPT]
                mm(PS[ob], v_ap, pt, bi == 0, bi == nb - 1, vbufs + [bpt], [PSB[ob]])
                mm(PS[db], ones, pt, bi == 0, bi == nb - 1, [bpt, Bc], [PSB[db]])
                if bi == nb - 1:
                    P.add("dve", lambda e: e.reciprocal(out=rden, in_=PS[db]), [PSB[db]], [Brden])
                    o_, bo_ = ost[ti_ % 2], Bost[ti_ % 2]
                    tt("dve", o_, PS[ob], rden, ALU.mult, [PSB[ob], Brden], [bo_])
                    P.dma(dst, o_, reads=[bo_], eng="pool")

            n = len(flat)
            for f in range(min(LA, n)):
                s_stage(f)
            for f in range(n):
                if f + LA < n:
                    s_stage(f + LA)
                pv_stage(f)

        sc_m = 192 ** -0.5

        def mla_loads(h):
            hi = h % NHB
            qi_ = h % 2

            def fn():
                P.dma(hb_k[hi], KT_m[h], writes=[Bhk[hi]])
                P.dma(hb_v[hi], V_m[h], writes=[Bhv[hi]])
                P.dma(hb_q[hi], QT_m[h, 0:128, :], writes=[Bhq[hi]])
                P.dma(hb_qr[qi_], QT_m[h, 128:192, :], writes=[Bqr[qi_]])
            return fn

        def dil_loads(slot_idx, g):
            hi = (8 + slot_idx * 3 + g) % NHB
            h = 4 * g + slot_idx

            def fn():
                P.dma(hb_k[hi], KT_d[h], writes=[Bhk[hi]])
                P.dma(hb_v[hi], V_d[h], writes=[Bhv[hi]])
                P.dma(hb_q[hi], QT_d[h], writes=[Bhq[hi]])
            return fn

        for h in range(8):
            hi = h % NHB
            qi_ = h % 2
            for t in range(4):
                pre = []
                if h == 0 and t == 0:
                    pre = [mla_loads(0), mla_loads(1)]
                elif t == 0:
                    pre = [mla_loads(h + 1)] if h + 1 < 8 else [dil_loads(0, 0)]
                qsl = slice(t * 512, (t + 1) * 512)
                blocks = []
                for kind, base in (("own", 0), ("oth", 1)):
                    for jk in range(4 * t + 4):
                        ksl = slice(base * NT + jk * 128, base * NT + (jk + 1) * 128)
                        mi = midx[("m", kind, jk - 4 * t)] if jk >= 4 * t else None
                        blocks.append((
                            [(hb_q[hi][:, qsl], 128, [Bhq[hi]]), (hb_qr[qi_][:, qsl], 64, [Bqr[qi_]])],
                            [(hb_k[hi][:, ksl], 128, [Bhk[hi]]), (krt_sb[:, ksl], 64, [Bkrt])],
                            hb_v[hi][:, base * 16 + jk, :], [Bhv[hi]], mi))
                tiles.append((pre, blocks, sc_m, OT_m[h, :, qsl]))
        sc_d = 128 ** -0.5
        for s_ in range(4):
            for t in range(4):
                pre = []
                if t == 0:
                    pre = [dil_loads(s_, 1), dil_loads(s_, 2)]
                    if s_ > 0:
                        pre = [dil_loads(s_, 0)] + pre
                qsl = slice(t * 512, (t + 1) * 512)
                blocks = []
                for g in range(3):
                    hi = (8 + s_ * 3 + g) % NHB
                    for kind, base in (("own", 0), ("oth", 1)):
                        for rho in range(-3, 10):
                            jk = 4 * t - rho
                            key = ("d", g, kind, rho)
                            if jk < 0 or jk > 15 or key not in midx:
                                continue
                            ksl = slice(base * NT + jk * 128, base * NT + (jk + 1) * 128)
                            blocks.append((
                                [(hb_q[hi][:, qsl], 128, [Bhq[hi]])],
                                [(hb_k[hi][:, ksl], 128, [Bhk[hi]])],
                                hb_v[hi][:, base * 16 + jk, :], [Bhv[hi]], midx[key]))
                tiles.append((pre, blocks, sc_d, OT_d[s_, :, qsl]))
        emit_attention()
        P.barrier()
        A.release(mC)
        if stop_after == "C":
            return nc, P.emit()

        mD = A.mark()
        merged = A.alloc([16, NT], BF16)
        Bmg = [Buf("mg%d" % i) for i in range(4)]
        mD1 = A.mark()
        wmo = A.alloc([8, D], BF16)
        wdo = A.alloc([4, D], BF16)
        Bwmo, Bwdo = Buf("wmo"), Buf("wdo")
        P.dma(wmo, w_mla_o.rearrange("(k p) n -> p k n", p=128), writes=[Bwmo], eng="pool")
        P.dma(wdo, w_dil_o.rearrange("(k p) n -> p k n", p=128), writes=[Bwdo], eng="pool")
        otm = [A.alloc([8, 512], BF16) for _ in range(2)]
        otd = [A.alloc([4, 512], BF16) for _ in range(2)]
        Bot = [Buf("ot0"), Buf("ot1")]
        Botd = [Buf("otd0"), Buf("otd1")]
        sga = [A.alloc([512], BF16) for _ in range(3)]
        sgb = [A.alloc([512], BF16) for _ in range(3)]
        Bsgt = [Buf("sg%d" % i) for i in range(3)]
        Bsgtb = [Buf("sgb%d" % i) for i in range(3)]
        t1 = [A.alloc([512], F32) for _ in range(2)]
        t2 = [A.alloc([512], F32) for _ in range(2)]
        Bt12 = [Buf("t12_0"), Buf("t12_1")]
        ci = 0
        for tg in range(4):
            tsl = slice(tg * 512, (tg + 1) * 512)
            o2 = tg % 2
            P.dma(otm[o2], OT_m[:, :, tsl].rearrange("h p t -> p h t"), writes=[Bot[o2]])
            P.dma(otd[o2], OT_d[:, :, tsl].rearrange("h p t -> p h t"), writes=[Botd[o2]])
            for m in range(16):
                s3 = ci % 3
                c2 = ci % 2
                ci += 1
                P.dma(sga[s3], SG[m, :, tsl], writes=[Bsgt[s3]])
                P.dma(sgb[s3], SG[16 + m, :, tsl], writes=[Bsgtb[s3]])
                ba, bb = (0, 1) if c2 == 0 else (2, 3)
                for k in range(8):
                    mm(PS[ba], wmo[:, k, m * 128:(m + 1) * 128], otm[o2][:, k, :], k == 0, k == 7, [Bwmo, Bot[o2]], [PSB[ba]])
                for k in range(4):
                    mm(PS[bb], wdo[:, k, m * 128:(m + 1) * 128], otd[o2][:, k, :], k == 0, k == 3, [Bwdo, Botd[o2]], [PSB[bb]])
                tt("dve", t1[c2], PS[ba], sga[s3], ALU.mult, [PSB[ba], Bsgt[s3]], [Bt12[c2]])
                tt("dve", t2[c2], PS[bb], sgb[s3], ALU.mult, [PSB[bb], Bsgtb[s3]], [Bt12[c2]])
                tt("pool", merged[:, m, tsl], t1[c2], t2[c2], ALU.add, [Bt12[c2]], [Bmg[tg]])
        A.release(mD1)

        P.barrier()
        AB2r = A.alloc([2, D], BF16)
        BAB2 = Buf("AB2r")
        mD2a = A.mark()
        rows2 = A.alloc([3 * D], F32)
        Brows2 = Buf("rows2")
        P.dma(rows2, rowv[:, 4 * D + 64:7 * D + 64].partition_broadcast(128), writes=[Brows2])
        sc2 = A.alloc([16], BF16)
        scB2 = A.alloc([16, 128], BF16)
        Bsc2 = Buf("sc2")
        act(sc2, colv_sb[:, 0:16], AF.Silu, [Bc], [Bsc2])
        for k in range(16):
            cp("dve", scB2[:, k, :], sc2[:, k:k + 1].to_broadcast([128, 128]), [Bsc2], [Bsc2])
        wa2 = [A.alloc([16, 512], BF16) for _ in range(2)]
        Bwa2 = [Buf("wa2_0"), Buf("wa2_1")]
        tmpr = A.alloc([512], F32)
        Btmpr = Buf("tmpr")
        wada_v2 = w_ada.rearrange("(k p) n -> p k n", p=128)
        gi2 = 0
        for which, seg in ((1, 3), (0, 4)):
            for n in range(4):
                wt2, wb2 = wa2[gi2 % 2], Bwa2[gi2 % 2]
                gi2 += 1
                c0 = seg * D + n * 512
                P.dma(wt2, wada_v2[:, :, c0:c0 + 512], writes=[wb2], eng="pool")
                bank = 4 + (n % 2)
                for k in range(16):
                    mm(PS[bank], scB2[:, k, :], wt2[:, k, :], k == 0, k == 15, [wb2, Bsc2], [PSB[bank]])
                nsl = slice(n * 512, (n + 1) * 512)
                if which == 1:
                    tt("dve", AB2r[:, 1, nsl], PS[bank], rows2[:, n * 512:(n + 1) * 512], ALU.add, [PSB[bank], Brows2], [BAB2])
                else:
                    tt("dve", tmpr, PS[bank], rows2[:, D + n * 512:D + (n + 1) * 512], ALU.add, [PSB[bank], Brows2], [Btmpr])
                    stt("dve", AB2r[:, 0, nsl], tmpr, 1.0, rows2[:, 2 * D + n * 512:2 * D + (n + 1) * 512], ALU.add, ALU.mult,
                        [Btmpr, Brows2], [BAB2])
        P.barrier()
        A.release(mD2a)
        wo = A.alloc([16, D], BF16)
        Bwo = Buf("wo")
        P.dma(wo, w_out.rearrange("(k p) n -> p k n", p=128), writes=[Bwo], eng="pool")
        xt2 = [A.alloc([D], F32)] * 2
        Bxt2 = [Buf("xt2_0")] * 2
        yt = [A.alloc([D], F32) for _ in range(2)]
        Byt = [Buf("yt0"), Buf("yt1")]
        h2k = [A.alloc([D], BF16) for _ in range(2)]
        Bh2k = [Buf("h2k0"), Buf("h2k1")]
        junk2 = A.alloc([D], BF16)
        Bjunk2 = Buf("junk2")
        stat2 = A.alloc([16, 12], F32)
        Bst2 = [Buf("st2_%d" % i) for i in range(16)]
        h2t = [A.alloc([16, 128], BF16) for _ in range(2)]
        Bh2t = [Buf("h2t0"), Buf("h2t1")]
        def d2_part1(ti):
            i2 = ti % 2
            tg = ti // 4
            tok = slice(ti * 128, (ti + 1) * 128)
            P.dma(xt2[i2], x_own[tok, :], writes=[Bxt2[i2]])
            for n in range(4):
                for k in range(16):
                    mm(PS[n], merged[:, k, tok], wo[:, k, n * 512:(n + 1) * 512], k == 0, k == 15, [Bmg[tg], Bwo], [PSB[n]])
            for n in range(4):
                act(junk2[:, 0:512], PS[n], AF.Square, [PSB[n]], [Bjunk2, Bst2[ti]], accum_out=stat2[:, ti, n:n + 1])
            P.add("dve", lambda e, ti=ti: e.tensor_reduce(out=stat2[:, ti, 4:5], in_=stat2[:, ti, 0:4], axis=mybir.AxisListType.X, op=ALU.add),
                  [Bst2[ti]], [Bst2[ti]])
            rsqrt_chain(stat2[:, ti, 5:6], stat2[:, ti, 4:5], 1.0 / D, [Bst2[ti]], [Bst2[ti]], stat2[:, ti, 6:7])
            for n in range(4):
                nsl = slice(n * 512, (n + 1) * 512)
                stt("dve", yt[i2][:, nsl], PS[n], stat2[:, ti, 5:6], Gab[:, 0, nsl], ALU.mult, ALU.mult,
                    [PSB[n], Bst2[ti], Bg], [Byt[i2]])
            tt("pool", yt[i2], yt[i2], xt2[i2], ALU.add, [Byt[i2], Bxt2[i2]], [Byt[i2]])
            P.dma(X1[tok, :], yt[i2], reads=[Byt[i2]])
            act(junk2, yt[i2], AF.Square, [Byt[i2]], [Bjunk2, Bst2[ti]], accum_out=stat2[:, ti, 7:8])
            rsqrt_chain(stat2[:, ti, 8:9], stat2[:, ti, 7:8], 1.0 / D, [Bst2[ti]], [Bst2[ti]], stat2[:, ti, 9:10])
            stt("dve", yt[i2], yt[i2], stat2[:, ti, 8:9], AB2r[:, 0, :], ALU.mult, ALU.mult, [Byt[i2], Bst2[ti], BAB2], [Byt[i2]])
            tt("pool", h2k[i2], yt[i2], AB2r[:, 1, :], ALU.add, [Byt[i2], BAB2], [Bh2k[i2]])
            P.dma(H2K[tok, :], h2k[i2], reads=[Bh2k[i2]])

        def d2_part2(ti):
            i2 = ti % 2
            tok = slice(ti * 128, (ti + 1) * 128)
            for half in range(2):
                bank = 6 + half
                for kk in range(8):
                    k = half * 8 + kk
                    P.add("pe", lambda e, bank=bank, kk=kk, k=k, i2=i2: e.transpose(
                        out=PSbf[bank][:, kk * 128:(kk + 1) * 128], in_=h2k[i2][:, k * 128:(k + 1) * 128], identity=ident),
                        [Bh2k[i2], Bc], [PSB[bank]])
                src = PSbf[bank][:, 0:1024].rearrange("p (k n) -> p k n", k=8)
                if half == 0:
                    cp("dve", h2t[i2][:, 0:8, :], src, [PSB[bank]], [Bh2t[i2]])
                else:
                    act(h2t[i2][:, 8:16, :], src, AF.Copy, [PSB[bank]], [Bh2t[i2]])
            P.dma(H2T[:, :, tok], h2t[i2], reads=[Bh2t[i2]])

        d2_part1(0)
        for ti in range(16):
            if ti + 1 < 16:
                d2_part1(ti + 1)
            d2_part2(ti)
        P.barrier()
        A.release(mD)
        if stop_after == "D":
            return nc, P.emit()

        mE = A.mark()
        Wt = A.alloc([16, NE], F32)
        sel_all = A.alloc([16, NE], F32)
        s8_all = A.alloc([16, 8], F32)
        smk = A.alloc([16, NE], BF16)
        destf = A.alloc([16, 8], F32)
        wk = A.alloc([16, 8], F32)
        dest_i = A.alloc([16, 8], I32)
        idxw = A.alloc([NBLK], I32)
        BWt = Buf("Wt")
        Bsel = [Buf("sel%d" % i) for i in range(16)]
        Bsmk = Buf("smk")
        Bdest = [Buf("dest%d" % i) for i in range(16)]
        Bidxw = Buf("idxw")
        mE0 = A.mark()
        wr_sb = A.alloc([16, NE], BF16)
        Bwr = Buf("wr")
        P.dma(wr_sb, w_router.rearrange("(k p) n -> p k n", p=128), writes=[Bwr], eng="pool")
        h2 = [A.alloc([16, 512], BF16) for _ in range(2)]
        Bh2 = [Buf("h2_0"), Buf("h2_1")]
        scs = A.alloc([NE], F32)
        bia = A.alloc([NE], F32)
        top8 = A.alloc([8, 8], F32)
        gsc = A.alloc([8], F32)
        g8 = A.alloc([8], F32)
        gm = A.alloc([8], F32)
        smask = A.alloc([NE], F32)
        wsum = A.alloc([4], F32)
        Brt_ = Buf("route")
        P.add("dve", lambda e: e.memset(destf, 0.0), [], Bdest)
        P.add("dve", lambda e: e.memset(wk, 0.0), [], Bdest)

        def route(ti, h2tile, bh2, col0):
            R = [Brt_]
            sel = sel_all[:, ti, :]
            s8 = s8_all[:, ti, :]
            for k in range(16):
                mm(PS[7][:, 0:NE], h2tile[:, k, col0:col0 + 128], wr_sb[:, k, :], k == 0, k == 15, [bh2, Bwr], [PSB[7]])
            act(scs, PS[7][:, 0:NE], AF.Sigmoid, [PSB[7]], R)
            tt("dve", bia, scs, rbias, ALU.add, R + [Bc], R)
            for g in range(8):
                P.add("dve", lambda e, g=g: e.max(out=top8[:, g, :], in_=bia[:, g * 8:(g + 1) * 8]), R, R)
            tt("dve", gsc, top8[:, :, 0], top8[:, :, 1], ALU.add, R, R)
            P.add("dve", lambda e: e.max(out=g8, in_=gsc), R, R)
            ts("dve", gm, gsc, g8[:, 3:4], 0.0, ALU.is_ge, ALU.add, R, R)
            gmb = gm.unsqueeze(2).to_broadcast([128, 8, 8])
            tt("dve", sel.rearrange("p (g c) -> p g c", g=8), bia.rearrange("p (g c) -> p g c", g=8), gmb, ALU.mult, R, R + [Bsel[ti]])
            ts("dve", gm, gm, -1.0, 4.0, ALU.add, ALU.mult, R, R)
            tt("dve", sel.rearrange("p (g c) -> p g c", g=8), sel.rearrange("p (g c) -> p g c", g=8),
               gm.unsqueeze(2).to_broadcast([128, 8, 8]), ALU.add, R + [Bsel[ti]], R + [Bsel[ti]])
            P.add("dve", lambda e: e.max(out=s8, in_=sel), R + [Bsel[ti]], R + [Bsel[ti]])
            ts("dve", smask, sel, s8[:, 5:6], 0.0, ALU.is_ge, ALU.add, R + [Bsel[ti]], R)
            cp("dve", smk[:, ti, :], smask, R, R + [Bsmk])
            tt("dve", smask, smask, scs, ALU.mult, R, R)
            P.add("dve", lambda e: e.tensor_reduce(out=wsum[:, 0:1], in_=smask, axis=mybir.AxisListType.X, op=ALU.add), R, R)
            ts("dve", wsum[:, 1:2], wsum[:, 0:1], 1e-20, 0.4, ALU.add, ALU.mult, R, R)
            P.add("dve", lambda e: e.reciprocal(out=wsum[:, 2:3], in_=wsum[:, 1:2]), R, R)
            ts("dve", Wt[:, ti, :], smask, wsum[:, 2:3], 0.0, ALU.mult, ALU.add, R, R + [BWt])

        wsg = A.alloc([16, 512], BF16)
        wsu = A.alloc([16, 512], BF16)
        wsd = A.alloc([4, D], BF16)
        Bws = Buf("ws")
        P.dma(wsg, w_sg.rearrange("(k p) n -> p k n", p=128), writes=[Bws], eng="pool")
        P.dma(wsu, w_su.rearrange("(k p) n -> p k n", p=128), writes=[Bws], eng="pool")
        P.dma(wsd, w_sd.rearrange("(k p) n -> p k n", p=128), writes=[Bws], eng="pool")
        sil3 = [A.alloc([512], F32) for _ in range(2)]
        Bsil3 = [Buf("sil3_0"), Buf("sil3_1")]
        aT3 = A.alloc([4, 512], BF16)
        BaT3 = Buf("aT3")
        sho_sb = [A.alloc([D], BF16) for _ in range(2)]
        Bsho = [Buf("sho0"), Buf("sho1")]
        for tg in range(4):
            hsel = tg % 2
            P.dma(h2[hsel], H2T[:, :, tg * 512:(tg + 1) * 512], writes=[Bh2[hsel]])
            for m in range(4):
                bg, bu = (0, 1) if m % 2 == 0 else (2, 3)
                for k in range(16):
                    mm(PS[bg], wsg[:, k, m * 128:(m + 1) * 128], h2[hsel][:, k, :], k == 0, k == 15, [Bws, Bh2[hsel]], [PSB[bg]])
                for k in range(16):
                    mm(PS[bu], wsu[:, k, m * 128:(m + 1) * 128], h2[hsel][:, k, :], k == 0, k == 15, [Bws, Bh2[hsel]], [PSB[bu]])
                s2 = m % 2
                act(sil3[s2], PS[bg], AF.Silu, [PSB[bg]], [Bsil3[s2]])
                tt("pool" if False else "dve", aT3[:, m, :], PS[bu], sil3[s2], ALU.mult, [PSB[bu], Bsil3[s2]], [BaT3])
            for tt_ in range(4):
                tile = tg * 4 + tt_
                i2 = tile % 2
                for n in range(4):
                    bk = 4 + (tt_ * 4 + n) % 3
                    for k in range(4):
                        mm(PS[bk], aT3[:, k, tt_ * 128:(tt_ + 1) * 128], wsd[:, k, n * 512:(n + 1) * 512], k == 0, k == 3,
                           [BaT3, Bws], [PSB[bk]])
                    act(sho_sb[i2][:, n * 512:(n + 1) * 512], PS[bk], AF.Copy, [PSB[bk]], [Bsho[i2]])
                P.dma(SHO[tile * 128:(tile + 1) * 128, :], sho_sb[i2], reads=[Bsho[i2]])
            for tt_ in range(4):
                route(tg * 4 + tt_, h2[hsel], Bh2[hsel], tt_ * 128)
        cnt = A.alloc([NE], F32)
        pcnt = A.alloc([NE], F32)
        ca = A.alloc([NE], F32)
        cb = A.alloc([NE], F32)
        base = A.alloc([NE], F32)
        thr = A.alloc([8], F32)
        cmp1 = A.alloc([NE, 8], F32)
        cmp2 = A.alloc([NBLK, NE], F32)
        blke = A.alloc([NBLK], F32)
        usedm = A.alloc([NBLK], F32)
        Be1 = Buf("e1")
        Bbase = Buf("base")
        E1 = [Be1]
        for ti in range(16):
            mm(PS[6][:, 0:NE], ones, smk[:, ti, :], ti == 0, ti == 15, [Bc, Bsmk], [PSB[6]])
        cp("dve", cnt, PS[6][:, 0:NE], [PSB[6]], E1)
        for j in range(8):
            P.add("dve", lambda e, j=j: e.memset(thr[:, j:j + 1], 256.0 * j), E1, E1)
        tt("dve", cmp1, cnt.unsqueeze(2).to_broadcast([128, NE, 8]), thr.unsqueeze(1).to_broadcast([128, NE, 8]), ALU.is_gt, E1, E1)
        P.add("dve", lambda e: e.tensor_reduce(out=pcnt, in_=cmp1, axis=mybir.AxisListType.X, op=ALU.add), E1, E1)
        cp("dve", ca, pcnt, E1, E1)
        src_, dst_ = ca, cb
        for sft in (1, 2, 4, 8, 16, 32):
            cp("dve", dst_[:, 0:sft], src_[:, 0:sft], E1, E1)
            tt("dve", dst_[:, sft:NE], src_[:, sft:NE], src_[:, 0:NE - sft], ALU.add, E1, E1)
            src_, dst_ = dst_, src_
        pends = src_
        tt("dve", base, pends, pcnt, ALU.subtract, E1, E1 + [Bbase])
        ts("dve", base, base, 256.0, 0.0, ALU.mult, ALU.add, E1 + [Bbase], E1 + [Bbase])
        jgrid = cf[:, 8:8 + NBLK]
        tt("dve", cmp2, pends.unsqueeze(1).to_broadcast([128, NBLK, NE]), jgrid.unsqueeze(2).to_broadcast([128, NBLK, NE]), ALU.is_le, E1 + [Bc], E1)
        P.add("dve", lambda e: e.tensor_reduce(out=blke, in_=cmp2, axis=mybir.AxisListType.X, op=ALU.add), E1, E1)
        ts("dve", blke, blke, 63.0, 128.0, ALU.min, ALU.mult, E1, E1)
        tt("dve", blke, blke, cf[:, 120:121].to_broadcast([128, NBLK]), ALU.add, E1 + [Bc], E1)
        ts("dve", usedm, jgrid, pends[:, NE - 1:NE], 1.0e6, ALU.is_ge, ALU.mult, E1 + [Bc], E1)
        tt("dve", blke, blke, usedm, ALU.add, E1, E1)
        cp("dve", idxw, blke, E1, [Bidxw])
        dfull = A.alloc([NE], F32)
        junk64 = A.alloc([NE], F32)
        h2kt = [A.alloc([D], BF16) for _ in range(2)]
        Bh2kt = [Buf("h2kt0"), Buf("h2kt1")]
        Bdf = Buf("dfull")
        for ti in range(16):
            i2 = ti % 2
            tok = slice(ti * 128, (ti + 1) * 128)
            P.dma(h2kt[i2], H2K[tok, :], writes=[Bh2kt[i2]])
            bank = 4 + ti % 2
            for j in range(ti):
                mm(PS[bank][:, 0:NE], ones, smk[:, j, :], j == 0, False, [Bc, Bsmk], [PSB[bank]])
            mm(PS[bank][:, 0:NE], Utri, smk[:, ti, :], ti == 0, True, [Bc, Bsmk], [PSB[bank]])
            tt("dve", dfull, PS[bank][:, 0:NE], base, ALU.add, [PSB[bank], Bbase], [Bdf])
            for k in range(6):
                P.add("dve", lambda e, ti=ti, k=k: e.scalar_tensor_tensor(
                    out=junk64, in0=sel_all[:, ti, :], scalar=s8_all[:, ti, k:k + 1], in1=dfull, op0=ALU.is_equal, op1=ALU.mult,
                    accum_out=destf[:, ti, k:k + 1]), [Bsel[ti], Bdf], [Bdest[ti], Bdf])
                P.add("dve", lambda e, ti=ti, k=k: e.scalar_tensor_tensor(
                    out=junk64, in0=sel_all[:, ti, :], scalar=s8_all[:, ti, k:k + 1], in1=Wt[:, ti, :], op0=ALU.is_equal, op1=ALU.mult,
                    accum_out=wk[:, ti, k:k + 1]), [Bsel[ti], BWt], [Bdest[ti], Bdf])
            ts("dve", junk64[:, 0:8], destf[:, ti, :], float(NSLOT) - 0.5, 0.0, ALU.is_lt, ALU.add, [Bdest[ti], Bdf], [Bdf])
            tt("dve", wk[:, ti, :], wk[:, ti, :], junk64[:, 0:8], ALU.mult, [Bdest[ti], Bdf], [Bdest[ti]])
            cp("dve", dest_i[:, ti, :], destf[:, ti, :], [Bdest[ti]], [Bdest[ti]])
            for k in range(6):
                P.add("pool", lambda e, ti=ti, k=k, i2=i2: e.indirect_dma_start(
                    out=XG[:, :], out_offset=bass.IndirectOffsetOnAxis(ap=dest_i[:, ti, k:k + 1], axis=0),
                    in_=h2kt[i2], in_offset=None, bounds_check=P.regs[NSLOT - 1], oob_is_err=False),
                    [Bdest[ti], Bh2kt[i2]], [], dma=True)
        P.barrier()
        A.release(mE0)
        if stop_after == "E2":
            return nc, P.emit()

        mE4 = A.mark()
        wg_t = [A.alloc([16, 512], BF16) for _ in range(2)]
        wu_t = [A.alloc([16, 512], BF16) for _ in range(2)]
        wd_t = [A.alloc([4, D], BF16) for _ in range(2)]
        Bweg = [Buf("weg0"), Buf("weg1")]
        Bweu = [Buf("weu0"), Buf("weu1")]
        Bwed = [Buf("wed0"), Buf("wed1")]
        xg_sb = [A.alloc([2, D], BF16) for _ in range(2)]
        Bxg = [Buf("xg0"), Buf("xg1")]
        xgT = [A.alloc([16, 256], BF16) for _ in range(2)]
        BxgT = [Buf("xgT0"), Buf("xgT1")]
        sil = [A.alloc([256], F32) for _ in range(2)]
        Bsil = [Buf("sil0"), Buf("sil1")]
        aT = [A.alloc([4, 256], BF16) for _ in range(2)]
        BaT = [Buf("aT0"), Buf("aT1")]
        yg_sb = [A.alloc([2, D], BF16) for _ in range(2)]
        Byg = [Buf("yg0"), Buf("yg1")]
        XGv = XG.rearrange("(j s p) d -> j p s d", s=2, p=128)
        YGv = YG.rearrange("(j s p) d -> j p s d", s=2, p=128)
        evs = {"c": 0}

        def e4_load(j):
            w2 = j % 2
            for (dst, src, bw) in ((wg_t[w2], w_eg, Bweg[w2]), (wu_t[w2], w_eu, Bweu[w2]), (wd_t[w2], w_ed, Bwed[w2])):
                P.add("pool", lambda e, dst=dst, src=src, j=j: e.indirect_dma_start(
                    out=dst.rearrange("p k n -> p (k n)"), out_offset=None, in_=src[:, :], in_offset=bass.IndirectOffsetOnAxis(ap=idxw[:, j:j + 1], axis=0),
                    bounds_check=P.regs[NE * 128 - 1], oob_is_err=False), [Bidxw], [bw], dma=True)
            P.dma(xg_sb[w2], XGv[j], writes=[Bxg[w2]])

        def e4_T(j):
            w2 = j % 2
            for s_ in range(2):
                for half in range(2):
                    bank = 6 + (s_ * 2 + half) % 2
                    for kk in range(8):
                        k = half * 8 + kk
                        P.add("pe", lambda e, bank=bank, kk=kk, k=k, w2=w2, s_=s_: e.transpose(
                            out=PSbf[bank][:, kk * 128:(kk + 1) * 128], in_=xg_sb[w2][:, s_, k * 128:(k + 1) * 128], identity=ident),
                            [Bxg[w2], Bc], [PSB[bank]])
                    src_ap = PSbf[bank][:, 0:1024].rearrange("p (k n) -> p k n", k=8)
                    dst_ap = xgT[w2][:, half * 8:(half + 1) * 8, s_ * 128:(s_ + 1) * 128]
                    if evs["c"] % 2 == 0:
                        cp("dve", dst_ap, src_ap, [PSB[bank]], [BxgT[w2]])
                    else:
                        act(dst_ap, src_ap, AF.Copy, [PSB[bank]], [BxgT[w2]])
                    evs["c"] += 1

        def e4_GU(j):
            w2 = j % 2
            for m in range(4):
                bg, bu = (0, 1) if m % 2 == 0 else (2, 3)
                for k in range(16):
                    mm(PS[bg][:, 0:256], wg_t[w2][:, k, m * 128:(m + 1) * 128], xgT[w2][:, k, :], k == 0, k == 15, [Bweg[w2], BxgT[w2]], [PSB[bg]])
                for k in range(16):
                    mm(PS[bu][:, 0:256], wu_t[w2][:, k, m * 128:(m + 1) * 128], xgT[w2][:, k, :], k == 0, k == 15, [Bweu[w2], BxgT[w2]], [PSB[bu]])
                s2 = m % 2
                act(sil[s2], PS[bg][:, 0:256], AF.Silu, [PSB[bg]], [Bsil[s2]])
                tt("dve", aT[w2][:, m, :], PS[bu][:, 0:256], sil[s2], ALU.mult, [PSB[bu], Bsil[s2]], [BaT[w2]])

        def e4_D(j):
            w2 = j % 2
            for s_ in range(2):
                for n in range(4):
                    bk = 4 + (s_ * 4 + n) % 2
                    for k in range(4):
                        mm(PS[bk], aT[w2][:, k, s_ * 128:(s_ + 1) * 128], wd_t[w2][:, k, n * 512:(n + 1) * 512], k == 0, k == 3,
                           [BaT[w2], Bwed[w2]], [PSB[bk]])
                    dst_ap = yg_sb[w2][:, s_, n * 512:(n + 1) * 512]
                    if n % 2 == 0:
                        cp("dve", dst_ap, PS[bk], [PSB[bk]], [Byg[w2]])
                    else:
                        act(dst_ap, PS[bk], AF.Copy, [PSB[bk]], [Byg[w2]])
            P.dma(YGv[j], yg_sb[w2], reads=[Byg[w2]])

        e4_load(0)
        e4_load(1)
        e4_T(0)
        e4_GU(0)
        for j in range(NBLK):
            if j + 1 < NBLK:
                e4_T(j + 1)
            e4_D(j)
            if j + 2 < NBLK:
                e4_load(j + 2)
            if j + 1 < NBLK:
                e4_GU(j + 1)
        P.barrier()
        A.release(mE4)

        acc = A.alloc([4, D], F32)
        Bacc = [Buf("acc%d" % i) for i in range(4)]
        sho_t = [A.alloc([D], BF16) for _ in range(2)]
        Bsho_t = [Buf("shot0"), Buf("shot1")]
        NYK = 12
        yk = [A.alloc([D], BF16) for _ in range(NYK)]
        Byk = [Buf("yk%d" % i) for i in range(NYK)]
        x1t = [A.alloc([D], F32) for _ in range(2)]
        Bx1 = [Buf("x1_0"), Buf("x1_1")]
        fo = [A.alloc([D], F32) for _ in range(2)]
        Bfo = [Buf("fo0"), Buf("fo1")]
        junk3 = A.alloc([D], BF16)
        Bjunk3 = Buf("junk3")
        stat3 = A.alloc([16, 4], F32)
        Bst3 = [Buf("st3_%d" % i) for i in range(16)]
        def e3_gather(tile):
            i2 = tile % 2
            tok = slice(tile * 128, (tile + 1) * 128)
            P.dma(x1t[i2], X1[tok, :], writes=[Bx1[i2]])
            P.dma(sho_t[i2], SHO[tok, :], writes=[Bsho_t[i2]])
            for k in range(6):
                y3 = (tile * 6 + k) % NYK
                P.add("pool", lambda e, tile=tile, k=k, y3=y3: e.indirect_dma_start(
                    out=yk[y3], out_offset=None, in_=YG[:, :], in_offset=bass.IndirectOffsetOnAxis(ap=dest_i[:, tile, k:k + 1], axis=0),
                    bounds_check=P.regs[NSLOT - 1], oob_is_err=False), [Bdest[tile]], [Byk[y3]], dma=True)

        def e3_combine(tile):
            tt_ = tile % 4
            i2 = tile % 2
            tok = slice(tile * 128, (tile + 1) * 128)
            for k in range(6):
                y3 = (tile * 6 + k) % NYK
                if k == 0:
                    stt("dve", acc[:, tt_, :], yk[y3], wk[:, tile, k:k + 1], sho_t[i2], ALU.mult, ALU.add,
                        [Byk[y3], Bdest[tile], Bsho_t[i2]], [Bacc[tt_]])
                else:
                    stt("dve", acc[:, tt_, :], yk[y3], wk[:, tile, k:k + 1], acc[:, tt_, :], ALU.mult, ALU.add,
                        [Byk[y3], Bdest[tile], Bacc[tt_]], [Bacc[tt_]])
            act(junk3, acc[:, tt_, :], AF.Square, [Bacc[tt_]], [Bjunk3, Bst3[tile]], accum_out=stat3[:, tile, 0:1])
            rsqrt_chain(stat3[:, tile, 1:2], stat3[:, tile, 0:1], 1.0 / D, [Bst3[tile]], [Bst3[tile]], stat3[:, tile, 2:3])
            stt("dve", fo[i2], acc[:, tt_, :], stat3[:, tile, 1:2], Gab[:, 1, :], ALU.mult, ALU.mult,
                [Bacc[tt_], Bst3[tile], Bg], [Bfo[i2]])
            tt("pool", fo[i2], fo[i2], x1t[i2], ALU.add, [Bfo[i2], Bx1[i2]], [Bfo[i2]])
            P.dma(out_d[tok, :], fo[i2], reads=[Bfo[i2]])

        e3_gather(0)
        for tile in range(16):
            if tile + 1 < 16:
                e3_gather(tile + 1)
            e3_combine(tile)
        A.release(mE)
        stats = P.emit()
    return nc, stats


def _consts():
    cb = np.zeros((128, 640), np.float32)
    cb[:, 512:640] = np.triu(np.ones((128, 128), np.float32), 1)
    cb[:, 0:128] = np.eye(128)
    cb[:, 128:256] = 1.0
    for m in range(64):
        cb[(m + 32) % 64, 256 + m] = 1.0
    for m in range(32):
        cb[(m + 16) % 32, 320 + m] = 1.0
    cf = np.zeros((128, 128), np.float32)
    cf[:, 8:120] = np.arange(112, dtype=np.float32)[None, :]
    cf[:, 120] = np.arange(128, dtype=np.float32)
    fm = (1.0 / (500000.0 ** (np.arange(0, 64, 2, dtype=np.float32) / 64))).astype(np.float32)
    fd = (1.0 / (500000.0 ** (np.arange(0, 32, 2, dtype=np.float32) / 32))).astype(np.float32)
    cf[0:32, 0] = fm
    cf[32:64, 0] = fm
    cf[0:32, 1] = -1.0
    cf[32:64, 1] = 1.0
    cf[0:16, 2] = fd
    cf[16:32, 2] = fd
    cf[0:16, 3] = -1.0
    cf[16:32, 3] = 1.0
    return cb.astype(ml_dtypes.bfloat16), cf


def _elayout(w):
    e, r, n = w.shape
    return np.ascontiguousarray(w.reshape(e, r // 128, 128, n).transpose(0, 2, 1, 3)).reshape(e * 128, (r // 128) * n)


def make_in_maps(x, c, positions, w_ada, b_ada, attn_pre_g, w_in, q_a_norm_g, w_q_up, kv_a_norm_g,
                 w_kv_up, w_mla_o, w_dil_o, w_out, attn_post_g, ffn_pre_g, w_router, router_bias,
                 w_exp_gate, w_exp_up, w_exp_down, w_sh_gate, w_sh_up, w_sh_down, ffn_post_g):
    f = lambda a: np.ascontiguousarray(np.asarray(a, dtype=np.float32))
    x = f(x)
    c = f(c)
    positions = np.asarray(positions).astype(np.int32)
    cb, cf = _consts()
    shared = {
        "cst_bf": cb, "cst_f": cf,
        "w_ada": f(w_ada)[0], "w_in": f(w_in)[0], "w_q_up": f(w_q_up)[0], "w_kv_up": f(w_kv_up)[0],
        "w_mla_o": f(w_mla_o)[0], "w_dil_o": f(w_dil_o)[0], "w_out": f(w_out)[0], "w_router": f(w_router)[0],
        "w_exp_gate": _elayout(f(w_exp_gate)[0]), "w_exp_up": _elayout(f(w_exp_up)[0]), "w_exp_down": _elayout(f(w_exp_down)[0]),
        "w_sh_gate": f(w_sh_gate)[0], "w_sh_up": f(w_sh_up)[0], "w_sh_down": f(w_sh_down)[0],
    }
    b_ada = f(b_ada)[0]

    def colform(v):
        return np.ascontiguousarray(v.reshape(-1, 128).T)

    rowv = np.concatenate([b_ada[2 * D:3 * D], b_ada[5 * D:6 * D], f(attn_post_g)[0], f(ffn_post_g)[0], f(router_bias)[0],
                           b_ada[3 * D:4 * D], b_ada[4 * D:5 * D], f(ffn_pre_g)[0]])[None, :]
    mask_cache = {}
    in_maps = []
    for core in range(8):
        b, p = core // 2, core % 2
        xb = x[b].reshape(32, 128, D)
        pb = positions[b].reshape(32, 128)
        colv = np.zeros((128, 128), np.float32)
        colv[:, 0:16] = colform(c[b])
        colv[:, 16:32] = colform(b_ada[0:D])
        colv[:, 32:48] = colform(b_ada[D:2 * D])
        colv[:, 48:64] = colform(b_ada[3 * D:4 * D])
        colv[:, 64:80] = colform(b_ada[4 * D:5 * D])
        colv[:, 80:96] = colform(f(attn_pre_g)[0])
        colv[:, 96:112] = colform(f(ffn_pre_g)[0])
        colv[:, 112:116] = colform(f(q_a_norm_g)[0])
        colv[:, 116:120] = colform(f(kv_a_norm_g)[0])
        if p not in mask_cache:
            m, _ = mask_table(p)
            mask_cache[p] = np.ascontiguousarray(m.transpose(1, 0, 2)).astype(ml_dtypes.bfloat16)
        d = dict(shared)
        d.update({
            "x_own": np.ascontiguousarray(xb[p::2].reshape(NT, D)),
            "x_oth": np.ascontiguousarray(xb[1 - p::2].reshape(NT, D)),
            "pos": np.ascontiguousarray(np.stack([pb[p::2].reshape(NT), pb[1 - p::2].reshape(NT)])),
            "colv": colv, "rowv": np.ascontiguousarray(rowv), "masks": mask_cache[p],
        })
        in_maps.append(d)
    return in_maps


_NC = None


def kernel(**inputs):
    global _NC
    if _NC is None:
        _NC = build()[0]
    in_maps = make_in_maps(**inputs)
    res = run_bass_kernel_spmd(_NC, in_maps, core_ids=list(range(8)))
    out = np.zeros((4, 32, 128, D), np.float32)
    for core in range(8):
        b, p = core // 2, core % 2
        out[b, p::2] = np.asarray(res.results[core]["out"], dtype=np.float32).reshape(16, 128, D)
    return out.reshape(4, S, D)
```
